# Optimizing a Trainium2 kernel written in Bass

```python
import jax, jax.numpy as jnp
from jax import lax
import numpy as np

D_MODEL = 1024
BATCH = 8
SEQ = 4096
DEPTH = 4

N_MIXERS = 2
ROPE_THETA = 10000.0
NORM_EPS = 1e-6

A_HEADS = 16
A_HEAD_DIM = 64
A_WIDTH = A_HEADS * A_HEAD_DIM
A_GROUPS = ((128, 1), (512, 4), (2048, 16))
A_N_GROUPS = len(A_GROUPS)
A_QKV_COLS = A_N_GROUPS * 3 * A_WIDTH
A_IN_COLS = A_QKV_COLS + A_WIDTH

B_HEADS = 16
B_KV_HEADS = 4
B_HEAD_DIM = 64
B_WIDTH = B_HEADS * B_HEAD_DIM
B_KV_WIDTH = B_KV_HEADS * B_HEAD_DIM
IDX_HEADS = 8
IDX_DIM = 64
TOPK_MAX = 256
Q_BLOCK = 128
B_SPLITS = (B_WIDTH, B_KV_WIDTH, B_KV_WIDTH, IDX_HEADS * IDX_DIM, IDX_DIM, IDX_HEADS, B_WIDTH)
B_IN_COLS = sum(B_SPLITS)

N_A_LAYERS = (DEPTH + 1) // 2
N_B_LAYERS = DEPTH // 2

kernel_name = 'hybrid_dilated_dsa_adaln_trunk'


def rms_norm(t, g):
    t32 = t.astype(jnp.float32)
    t32 = t32 * lax.rsqrt(jnp.mean(t32 * t32, axis=-1, keepdims=True) + NORM_EPS)
    return t32.astype(t.dtype) * g


def rope(t, pos):
    half = t.shape[-1] // 2
    inv_freq = ROPE_THETA ** (-jnp.arange(half, dtype=jnp.float32) / half)
    ang = pos.astype(jnp.float32)[..., None] * inv_freq
    cos = jnp.cos(ang)[:, :, None, :]
    sin = jnp.sin(ang)[:, :, None, :]
    t32 = t.astype(jnp.float32)
    t1, t2 = t32[..., :half], t32[..., half:]
    return jnp.concatenate([t1 * cos - t2 * sin, t2 * cos + t1 * sin], axis=-1).astype(t.dtype)


def banded_causal_attention(q, k, v, n_back):
    N, n, H, Dh = q.shape
    blk = n_back
    nb = -(-n // blk)
    pad = nb * blk - n
    qb = jnp.pad(q, ((0, 0), (0, pad), (0, 0), (0, 0))).reshape(N, nb, blk, H, Dh)
    kp = jnp.pad(k, ((0, 0), (blk, pad), (0, 0), (0, 0))).reshape(N, nb + 1, blk, H, Dh)
    vp = jnp.pad(v, ((0, 0), (blk, pad), (0, 0), (0, 0))).reshape(N, nb + 1, blk, H, Dh)
    kb = jnp.concatenate([kp[:, :-1], kp[:, 1:]], axis=2)
    vb = jnp.concatenate([vp[:, :-1], vp[:, 1:]], axis=2)
    s = jnp.einsum('nbqhd,nbkhd->nbhqk', qb, kb).astype(jnp.float32) * (Dh ** -0.5)
    qi = jnp.arange(blk)[:, None]
    kj = jnp.arange(2 * blk)[None, :]
    dist = blk + qi - kj
    key_pos = jnp.arange(nb)[:, None, None] * blk - blk + kj[None]
    mask = (dist >= 0) & (dist <= n_back) & (key_pos >= 0)
    s = jnp.where(mask[None, :, None], s, -jnp.inf)
    m = jnp.max(s, axis=-1, keepdims=True)
    e = jnp.exp(s - m)
    den = jnp.sum(e, axis=-1, keepdims=True)
    lse = (m + jnp.log(den))[..., 0]
    o = jnp.einsum('nbhqk,nbkhd->nbqhd', (e / den).astype(v.dtype), vb)
    o = o.reshape(N, nb * blk, H, Dh)[:, :n]
    lse = lse.transpose(0, 1, 3, 2).reshape(N, nb * blk, H)[:, :n]
    return o, lse


def dilated_group_attention(q, k, v, dilation, n_back):
    B, S, H, Dh = q.shape
    n = S // dilation

    def to_res(t):
        return t.reshape(B, n, dilation, H, Dh).transpose(0, 2, 1, 3, 4).reshape(B * dilation, n, H, Dh)

    o, lse = banded_causal_attention(to_res(q), to_res(k), to_res(v), n_back)
    o = o.reshape(B, dilation, n, H, Dh).transpose(0, 2, 1, 3, 4).reshape(B, S, H, Dh)
    lse = lse.reshape(B, dilation, n, H).transpose(0, 2, 1, 3).reshape(B, S, H)
    return o, lse


def dilated_mixer(h, pos, w_in, w_out):
    B, S, _ = h.shape
    proj = h @ w_in
    qkv = proj[..., :A_QKV_COLS].reshape(B, S, A_N_GROUPS, 3, A_HEADS, A_HEAD_DIM)
    gate = proj[..., A_QKV_COLS:]
    outs, lses = [], []
    for g, (window, dilation) in enumerate(A_GROUPS):
        q = rope(qkv[:, :, g, 0], pos)
        k = rope(qkv[:, :, g, 1], pos)
        v = qkv[:, :, g, 2]
        o, lse = dilated_group_attention(q, k, v, dilation, window // dilation)
        outs.append(o)
        lses.append(lse)
    o = jnp.stack(outs, axis=0)
    alpha = jax.nn.softmax(jnp.stack(lses, axis=0), axis=0)
    y = jnp.sum(alpha[..., None].astype(o.dtype) * o, axis=0).reshape(B, S, A_WIDTH)
    return (y * jax.nn.silu(gate)) @ w_out


def dsa_mixer(h, pos, w_in, w_out):
    B, S, _ = h.shape
    proj = h @ w_in
    cuts = list(np.cumsum(B_SPLITS)[:-1])
    q, k, v, qi, ki, wi, gate = jnp.split(proj, cuts, axis=-1)
    group = B_HEADS // B_KV_HEADS
    q = rope(q.reshape(B, S, B_HEADS, B_HEAD_DIM), pos).reshape(B, S, B_KV_HEADS, group, B_HEAD_DIM)
    k = rope(k.reshape(B, S, B_KV_HEADS, B_HEAD_DIM), pos)
    v = v.reshape(B, S, B_KV_HEADS, B_HEAD_DIM)
    qi = rope(qi.reshape(B, S, IDX_HEADS, IDX_DIM), pos)
    ki = rope(ki.reshape(B, S, 1, IDX_DIM), pos)[:, :, 0].astype(jnp.float32)
    wi = wi * (IDX_HEADS ** -0.5)
    k_sel = min(TOPK_MAX, S // 4)
    n_blk = S // Q_BLOCK
    key_idx = jnp.arange(S)

    def to_blocks(t):
        return t.reshape(B, n_blk, Q_BLOCK, *t.shape[2:]).swapaxes(0, 1)

    def block_fn(args):
        qb, qib, wib, start = args
        q_pos = start + jnp.arange(Q_BLOCK)
        sc = jnp.einsum('bqhd,bsd->bqhs', qib.astype(jnp.float32), ki) * (IDX_DIM ** -0.5)
        score = jnp.einsum('bqhs,bqh->bqs', jax.nn.relu(sc), wib.astype(jnp.float32))
        causal = key_idx[None, :] <= q_pos[:, None]
        score = jnp.where(causal[None], score, -jnp.inf)
        _, idx = lax.top_k(score, k_sel)
        valid = idx <= q_pos[None, :, None]
        ks = jax.vmap(lambda kb_, ib_: kb_[ib_])(k, idx)
        vs = jax.vmap(lambda vb_, ib_: vb_[ib_])(v, idx)
        s = jnp.einsum('bqgrd,bqkgd->bqgrk', qb, ks).astype(jnp.float32) * (B_HEAD_DIM ** -0.5)
        s = jnp.where(valid[:, :, None, None, :], s, -jnp.inf)
        p = jax.nn.softmax(s, axis=-1).astype(vs.dtype)
        o = jnp.einsum('bqgrk,bqkgd->bqgrd', p, vs)
        return o.reshape(B, Q_BLOCK, B_WIDTH)

    starts = jnp.arange(n_blk) * Q_BLOCK
    o = lax.map(block_fn, (to_blocks(q), to_blocks(qi), to_blocks(wi), starts))
    o = o.swapaxes(0, 1).reshape(B, S, B_WIDTH)
    return (o * jax.nn.silu(gate)) @ w_out


def setup_inputs(seed: int = 0) -> dict:
    key = jax.random.key(seed)
    ks = jax.random.split(key, 12)
    f32 = jnp.float32
    x = jax.random.normal(ks[0], (BATCH, SEQ, D_MODEL), f32)
    c = jax.random.normal(ks[1], (BATCH, D_MODEL), f32)
    offset = jax.random.randint(ks[2], (BATCH, 1), 0, 4096, dtype=jnp.int32)
    positions = (offset + jnp.arange(SEQ, dtype=jnp.int32)[None, :]).astype(jnp.int32)
    norm_g = 1.0 + 0.02 * jax.random.normal(ks[3], (DEPTH, D_MODEL), f32)
    ada_w = 0.02 * jax.random.normal(ks[4], (DEPTH, D_MODEL, 3 * D_MODEL), f32)
    ada_b = 0.02 * jax.random.normal(ks[5], (DEPTH, 3 * D_MODEL), f32)
    a_w_in = jax.random.normal(ks[6], (N_A_LAYERS, D_MODEL, A_IN_COLS), f32) * (D_MODEL ** -0.5)
    a_w_out = jax.random.normal(ks[7], (N_A_LAYERS, A_WIDTH, D_MODEL), f32) * (A_WIDTH ** -0.5)
    b_w_in = jax.random.normal(ks[8], (N_B_LAYERS, D_MODEL, B_IN_COLS), f32) * (D_MODEL ** -0.5)
    b_w_out = jax.random.normal(ks[9], (N_B_LAYERS, B_WIDTH, D_MODEL), f32) * (B_WIDTH ** -0.5)
    final_g = 1.0 + 0.02 * jax.random.normal(ks[10], (D_MODEL,), f32)
    return {'x': x, 'c': c, 'positions': positions, 'norm_g': norm_g, 'ada_w': ada_w,
            'ada_b': ada_b, 'a_w_in': a_w_in, 'a_w_out': a_w_out, 'b_w_in': b_w_in,
            'b_w_out': b_w_out, 'final_g': final_g}


def reference(x, c, positions, norm_g, ada_w, ada_b, a_w_in, a_w_out, b_w_in, b_w_out, final_g):
    h = x
    c_act = jax.nn.silu(c)
    for i in range(DEPTH):
        mod = c_act @ ada_w[i] + ada_b[i]
        shift, scale, gate = jnp.split(mod, 3, axis=-1)
        u = rms_norm(h, norm_g[i]) * (1.0 + scale[:, None, :]) + shift[:, None, :]
        if i % N_MIXERS == 0:
            y = dilated_mixer(u, positions, a_w_in[i // N_MIXERS], a_w_out[i // N_MIXERS])
        else:
            y = dsa_mixer(u, positions, b_w_in[i // N_MIXERS], b_w_out[i // N_MIXERS])
        h = h + gate[:, None, :] * y
    return rms_norm(h, final_g)
```

```python
import numpy as np
import ml_dtypes
import concourse.bass as bass
import concourse.mybir as mybir
from concourse.bass_utils import run_bass_kernel_spmd

F32 = mybir.dt.float32
BF16 = mybir.dt.bfloat16
I32 = mybir.dt.int32
ALU = mybir.AluOpType
AF = mybir.ActivationFunctionType

S = 4096
D = 1024
NT = S // 128
EPS = 1e-6
A_GROUPS = ((128, 1), (512, 4), (2048, 16))
TOPK = 256


class Sched:
    ENGS = ("tensor", "scalar", "vector", "gpsimd", "sync")

    def __init__(self):
        self.ops = []

    def add(self, eng, fn, r=(), w=(), dma=0):
        self.ops.append(dict(eng=eng, fn=fn, r=list(r), w=list(w), dma=dma))

    def pe(self, fn, r=(), w=()):
        self.add("tensor", fn, r, w)

    def act(self, fn, r=(), w=()):
        self.add("scalar", fn, r, w)

    def dve(self, fn, r=(), w=()):
        self.add("vector", fn, r, w)

    def pool(self, fn, r=(), w=()):
        import os
        self.add(os.environ.get("KPOOL", "gpsimd"), fn, r, w)

    def dma(self, fn, r=(), w=(), n=1, q="sync"):
        self.add(q, fn, r, w, dma=n)

    def finalize(self, nc, n_dma_sems=16):
        ops = self.ops
        state = {}

        def touch(key):
            name, sub = key
            d = state.setdefault(name, {})
            if sub == "*":
                d.setdefault("*", [None, []])
                return list(d.keys())
            d.setdefault(sub, [None, []])
            return [sub, "*"] if "*" in d else [sub]

        for i, op in enumerate(ops):
            deps = set()
            for key in op["r"]:
                d = state.setdefault(key[0], {})
                for sub in touch(key):
                    wv = d[sub][0]
                    if wv is not None:
                        deps.add(wv)
            for key in op["w"]:
                d = state.setdefault(key[0], {})
                for sub in touch(key):
                    wv, rd = d[sub]
                    if wv is not None:
                        deps.add(wv)
                    deps.update(rd)
            for key in op["r"]:
                d = state[key[0]]
                subs = [key[1]] if key[1] != "*" else list(d.keys())
                for sub in subs:
                    rl = d[sub][1]
                    if not op["dma"]:
                        rl[:] = [j for j in rl if ops[j]["dma"] or ops[j]["eng"] != op["eng"]]
                    rl.append(i)
            for key in op["w"]:
                d = state[key[0]]
                subs = [key[1]] if key[1] != "*" else list(d.keys())
                for sub in subs:
                    d[sub][0] = i
                    d[sub][1] = []
            deps.discard(i)
            op["deps"] = deps

        pos = {}
        cnt = {e: 0 for e in self.ENGS}
        for i, op in enumerate(ops):
            pos[i] = cnt[op["eng"]]
            cnt[op["eng"]] += 1

        for i, op in enumerate(ops):
            keep = set()
            for dix in op["deps"]:
                p = ops[dix]
                if p["eng"] == op["eng"] and not p["dma"]:
                    if op["eng"] == "tensor" and not op["dma"]:
                        continue
                keep.add(dix)
            op["deps"] = keep

        dma_ops = [i for i, op in enumerate(ops) if op["dma"]]
        sem_tot = [0] * n_dma_sems
        last_on_sem = [None] * n_dma_sems
        qcount = {"sync": 0, "scalar": 0, "gpsimd": 0}
        qrange = {"sync": (0, n_dma_sems - 6), "scalar": (n_dma_sems - 6, n_dma_sems), "gpsimd": (n_dma_sems - 6, n_dma_sems)}
        for i in dma_ops:
            op = ops[i]
            lo_, hi_ = qrange[op["eng"]]
            s = lo_ + qcount[op["eng"]] % (hi_ - lo_)
            qcount[op["eng"]] += 1
            if last_on_sem[s] is not None:
                op["deps"].add(last_on_sem[s])
            sem_tot[s] += 16 * op["dma"]
            op["sig"] = ("dma", s, sem_tot[s])
            last_on_sem[s] = i

        needed = set()
        for op in ops:
            needed.update(op["deps"])
        sig_cnt = {e: 0 for e in self.ENGS}
        for i, op in enumerate(ops):
            if op["dma"]:
                continue
            if i in needed:
                sig_cnt[op["eng"]] += 1
                op["sig"] = ("eng", op["eng"], sig_cnt[op["eng"]])
            else:
                op["sig"] = None

        self.by_eng = {e: [op for op in ops if op["eng"] == e] for e in self.ENGS}
        self.final_dma = [(s, sem_tot[s]) for s in range(n_dma_sems) if sem_tot[s] > 0]
        self.n_dma_sems = n_dma_sems

    def emit(self, nc, block, sems, dma_sems):
        ops = self.ops

        def run_engine(e, name):
            known = {}
            for op in self.by_eng[name]:
                waits = {}
                for dix in op["deps"]:
                    sg = ops[dix]["sig"]
                    if sg[0] == "dma":
                        key = ("dma", sg[1])
                    else:
                        key = ("eng", sg[1])
                    waits[key] = max(waits.get(key, 0), sg[2])
                for key, val in waits.items():
                    if known.get(key, 0) >= val:
                        continue
                    known[key] = val
                    sem = dma_sems[key[1]] if key[0] == "dma" else sems[key[1]]
                    e.wait_ge(sem, val)
                inst = op["fn"](e)
                sg = op["sig"]
                if sg is not None:
                    if sg[0] == "dma":
                        insts = inst if isinstance(inst, (list, tuple)) else [inst]
                        assert len(insts) == op["dma"], (len(insts), op["dma"])
                        for ins in insts:
                            ins.then_inc(dma_sems[sg[1]], 16)
                    else:
                        inst.then_inc(sems[name], 1)
            if name in ("sync", "scalar"):
                for s, tot in self.final_dma:
                    if known.get(("dma", s), 0) < tot:
                        e.wait_ge(dma_sems[s], tot)

        @block.tensor
        def _(e):
            run_engine(e, "tensor")

        @block.scalar
        def _(e):
            run_engine(e, "scalar")

        @block.vector
        def _(e):
            run_engine(e, "vector")

        @block.gpsimd
        def _(e):
            run_engine(e, "gpsimd")

        @block.sync
        def _(e):
            run_engine(e, "sync")


NEG_BIG = -1.0e30
IDX_SCALE = float((8 ** -0.5) * (64 ** -0.5))
TWO_PI = float(2 * np.pi)
CW1 = 6.28125
CW2 = float(2 * np.pi - 6.28125)
NIT = 22
BIS_LO = -64.0
BIS_STEP0 = 64.0

CB_ID = 0
CB_SWAP = 128
CB_BAND = 256
CB_ONES = 512
CB_N = 640
CF_INVF = 0
CF_SIGN = 1
CF_CAUS = 2
CF_ONES = 130
CF_N = 258


def make_consts():
    cb = np.zeros((128, CB_N), np.float32)
    cb[:, CB_ID:CB_ID + 128] = np.eye(128, dtype=np.float32)
    for m in range(128):
        k = m + 32 if (m % 64) < 32 else m - 32
        cb[k, CB_SWAP + m] = 1.0
    kj = np.arange(128)[:, None]
    c = np.arange(128)[None, :]
    cb[:, CB_BAND:CB_BAND + 128] = (kj <= c)
    cb[:, CB_BAND + 128:CB_BAND + 256] = (kj >= c)
    cb[:, CB_ONES:CB_ONES + 128] = 1.0
    cf = np.zeros((128, CF_N), np.float32)
    half = 32
    inv_freq = (10000.0 ** (-np.arange(half, dtype=np.float32) / half)).astype(np.float32)
    p = np.arange(128)
    cf[:, CF_INVF] = inv_freq[p % 32]
    cf[:, CF_SIGN] = np.where((p % 64) < 32, -1.0, 1.0)
    t = np.arange(128)[:, None]
    s = np.arange(128)[None, :]
    cf[:, CF_CAUS:CF_CAUS + 128] = np.where(s <= t, 0.0, NEG_BIG)
    cf[:, CF_ONES:CF_ONES + 128] = 1.0
    return cb.astype(ml_dtypes.bfloat16), cf


def a_piece_cols():
    out = []
    for hp in range(8):
        pcs = []
        heads = (2 * hp, 2 * hp + 1)
        for g in range(3):
            for t in range(3):
                pcs.append([g * 3072 + t * 1024 + h * 64 + d for h in heads for d in range(64)])
        pcs.append([9216 + h * 64 + d for h in heads for d in range(64)])
        out.append(pcs)
    return np.array(out, np.int64)


B_OFF = dict(q=0, k=1024, v=1280, qi=1536, ki=2048, wi=2112, gate=2120)
BP_Q = 0
BP_K2 = 8
BP_V = 12
BP_QI = 14
BP_WREP = 18
BP_KI2 = 22
BP_GATE = 23
BP_WI = 31
BP_N = 32


def b_piece_cols():
    pcs = []
    for p in range(8):
        pcs.append([B_OFF["q"] + p * 128 + j for j in range(128)])
    for g in range(4):
        pcs.append([B_OFF["k"] + g * 64 + d for _ in range(2) for d in range(64)])
    for p in range(2):
        pcs.append([B_OFF["v"] + p * 128 + j for j in range(128)])
    for p in range(4):
        pcs.append([B_OFF["qi"] + p * 128 + j for j in range(128)])
    for p in range(4):
        pcs.append([B_OFF["wi"] + 2 * p + e for e in range(2) for _ in range(64)])
    pcs.append([B_OFF["ki"] + d for _ in range(2) for d in range(64)])
    for p in range(8):
        pcs.append([B_OFF["gate"] + p * 128 + j for j in range(128)])
    pcs.append([B_OFF["wi"] + (j % 8) for j in range(128)])
    return np.array(pcs, np.int64)


def pieces_layout(w, cols):
    g = w[:, cols.reshape(-1)]
    npc = cols.size // 128
    g = g.reshape(8, 128, npc, 128)
    g = np.ascontiguousarray(g.transpose(2, 1, 0, 3))
    return g.reshape(cols.shape[:-1] + (128, 8, 128))


from contextlib import ExitStack


def pk(bank, sub="*"):
    return ("ps%d" % bank, sub)


def f32v(ap):
    return ap.bitcast(F32)


class Prog:
    def __init__(self, layers=(0, 1, 2, 3), final_norm=True, debug_out=None):
        self.layers = tuple(layers)
        self.final_norm = final_norm
        self.debug_out = debug_out
        self.nc = bass.Bass("TRN2", target_bir_lowering=False)
        self.sc = Sched()
        self.rr = {}

    def ring(self, name, n):
        i = self.rr.get(name, 0)
        self.rr[name] = i + 1
        return i % n

    def build(self):
        nc = self.nc
        sc = self.sc
        dt = nc.dram_tensor
        self.x = dt("x", [S, D], F32, kind="ExternalInput").ap()
        self.cT = dt("cT", [128, 8], F32, kind="ExternalInput").ap()
        self.pos = dt("pos", [1, S], I32, kind="ExternalInput").ap()
        self.ngT = dt("ngT", [128, 32], F32, kind="ExternalInput").ap()
        self.fg = dt("fg", [1, D], F32, kind="ExternalInput").ap()
        self.ada_w = dt("ada_w", [4, D, 3 * D], F32, kind="ExternalInput").ap()
        self.adabT = dt("adabT", [128, 96], F32, kind="ExternalInput").ap()
        self.adab = dt("adab", [4, 3 * D], F32, kind="ExternalInput").ap()
        self.awin = dt("awin", [2, 8, 10, 128, 8, 128], F32, kind="ExternalInput").ap()
        self.awout = dt("awout", [2, D, D], F32, kind="ExternalInput").ap()
        self.bwin = dt("bwin", [2, BP_N, 128, 8, 128], F32, kind="ExternalInput").ap()
        self.bwout = dt("bwout", [2, D, D], F32, kind="ExternalInput").ap()
        self.cbd = dt("cb", [128, CB_N], BF16, kind="ExternalInput").ap()
        self.cfd = dt("cf", [128, CF_N], F32, kind="ExternalInput").ap()
        self.out = dt("out", [S, D], F32, kind="ExternalOutput").ap()
        self.yTd = dt("yT_scr", [D, S], BF16, kind="Internal").ap()
        self.gbd = dt("gb_scr", [4, 128, D], F32, kind="Internal").ap()
        self.QTd = dt("QT_scr", [D, S], BF16, kind="Internal").ap()
        self.QiTd = dt("QiT_scr", [512, S], BF16, kind="Internal").ap()
        self.GTd = dt("GT_scr", [D, S], BF16, kind="Internal").ap()

        with ExitStack() as es:
            def sb(name, shape, dtype):
                return es.enter_context(nc.sbuf_tensor(name, shape, dtype))

            self.UT = sb("UT", [128, 32768], BF16)
            self.TAB = sb("TAB", [128, 8192], BF16)
            self.RX = sb("RX", [128, 16384], BF16)
            self.RY = sb("RY", [128, 8704], BF16)
            self.RZ = sb("RZ", [128, 6656], BF16)
            self.RW = sb("RW", [128, 12288], BF16)
            self.RT = sb("RT", [128, 12288], BF16)
            self.cb = sb("cbs", [128, CB_N], BF16)
            self.cf = sb("cfs", [128, CF_N], F32)
            self.sm = sb("small", [128, 512], F32)
            self.wtok = sb("wtok", [128, 3 * 256], F32)
            self.dummy = sb("dummyt", [128, 8], F32)
            self.ps = [es.enter_context(nc.psum_tensor(f"ps{i}", [128, 512], F32)) for i in range(8)]
            self.sems = {e: es.enter_context(nc.semaphore(f"s_{e}")) for e in Sched.ENGS}
            NDS = 18
            self.dsems = [es.enter_context(nc.semaphore(f"d_{i}")) for i in range(NDS)]
            block = es.enter_context(nc.Block())

            self.define()
            import os
            if os.environ.get("KTRUNC"):
                sc.ops = sc.ops[:int(os.environ["KTRUNC"])]
            sc.finalize(nc, n_dma_sems=NDS)
            sc.emit(nc, block, self.sems, self.dsems)
        return nc

    def smv(self, a, b):
        return self.sm[:, a:b]

    def fence(self, regions):
        d = self.dummy
        self.sc.dve(lambda e: e.memset(d[:, 0:1], 0.0), w=[(r, "*") for r in regions] + [("dummy", 0)])

    def define(self):
        sc = self.sc
        cb, cf = self.cb, self.cf
        sc.dma(lambda e: e.dma_start(out=cb[:, :], in_=self.cbd[:, :]), w=[("cb", 0)])
        sc.dma(lambda e: e.dma_start(out=cf[:, :], in_=self.cfd[:, :]), w=[("cf", 0)])
        self.p0_tables()
        self.p0_mods()
        src = self.x
        for l in self.layers:
            self.p1_norm(l, src)
            if l % 2 == 0:
                self.p2_a(l)
            else:
                self.p2_b(l)
            self.p3_out(l, src)
            src = self.out
        if self.final_norm:
            self.p4_final(src)

    def p0_tables(self):
        sc = self.sc
        UT, cf = self.UT, self.cf
        posi = UT[:, 0:8192].bitcast(I32)
        ang = f32v(UT[:, 8192:16384])
        tk = f32v(UT[:, 16384:24576])
        rr_ = f32v(UT[:, 24576:32768])
        tki = UT[:, 16384:24576].bitcast(I32)
        C = self.TAB[:, 0:4096]
        Sg = self.TAB[:, 4096:8192]
        K = [("UT", "p0")]
        sc.dma(lambda e: e.dma_start(out=posi, in_=self.pos.partition_broadcast(128)), w=K)
        sc.dve(lambda e: e.tensor_copy(out=rr_, in_=posi), r=K, w=K)
        sc.dve(lambda e: e.tensor_scalar(out=ang, in0=rr_, scalar1=cf[:, CF_INVF:CF_INVF + 1], scalar2=None,
                                         op0=ALU.mult), r=K + [("cf", 0)], w=K)

        def reduce_and_sin(shift, dst, mul_sign):
            sc.dve(lambda e: e.tensor_scalar(out=tk, in0=ang, scalar1=shift, scalar2=1.0 / TWO_PI,
                                             op0=ALU.add, op1=ALU.mult), r=K, w=K)
            sc.dve(lambda e: e.tensor_copy(out=tki, in_=tk), r=K, w=K)
            sc.dve(lambda e: e.tensor_copy(out=tk, in_=tki), r=K, w=K)
            sc.dve(lambda e: e.tensor_scalar(out=rr_, in0=ang, scalar1=shift, scalar2=None, op0=ALU.add), r=K, w=K)
            sc.dve(lambda e: e.scalar_tensor_tensor(out=rr_, in0=tk, scalar=-CW1, in1=rr_, op0=ALU.mult, op1=ALU.add),
                   r=K, w=K)
            sc.dve(lambda e: e.scalar_tensor_tensor(out=rr_, in0=tk, scalar=-CW2, in1=rr_, op0=ALU.mult, op1=ALU.add),
                   r=K, w=K)
            sc.dve(lambda e: e.tensor_scalar(out=tk, in0=rr_, scalar1=float(np.pi), scalar2=-TWO_PI,
                                             op0=ALU.is_gt, op1=ALU.mult), r=K, w=K)
            sc.dve(lambda e: e.tensor_tensor(out=rr_, in0=rr_, in1=tk, op=ALU.add), r=K, w=K)
            sc.dve(lambda e: e.tensor_scalar(out=tk, in0=rr_, scalar1=float(-np.pi), scalar2=TWO_PI,
                                             op0=ALU.is_lt, op1=ALU.mult), r=K, w=K)
            sc.dve(lambda e: e.tensor_tensor(out=rr_, in0=rr_, in1=tk, op=ALU.add), r=K, w=K)
            sc.dve(lambda e: e.tensor_scalar(out=rr_, in0=rr_, scalar1=3.1415925, scalar2=-3.1415925,
                                             op0=ALU.min, op1=ALU.max), r=K, w=K)
            if mul_sign:
                sc.act(lambda e: e.activation(out=tk, in_=rr_, func=AF.Sin), r=K, w=K)
                sc.dve(lambda e: e.tensor_scalar(out=dst, in0=tk, scalar1=cf[:, CF_SIGN:CF_SIGN + 1], scalar2=None,
                                                 op0=ALU.mult), r=K, w=[("TAB", 0)])
            else:
                sc.act(lambda e: e.activation(out=dst, in_=rr_, func=AF.Sin), r=K, w=[("TAB", 0)])

        reduce_and_sin(0.0, Sg, True)
        reduce_and_sin(float(np.pi / 2), C, False)
        self.fence(["UT"])

    def p0_mods(self):
        sc = self.sc
        UT, RX, cf, cb, sm = self.UT, self.RX, self.cf, self.cb, self.sm
        c_sb = sm[:, 392:400]
        cact = sm[:, 384:392]
        modT = sm[:, 192:288]
        adabT = sm[:, 288:384]
        ng = sm[:, 160:192]
        gs = sm[:, 96:128]
        sh = sm[:, 128:160]
        stage = [f32v(UT[:, 0:8192]).rearrange("p (k n) -> p k n", k=8),
                 f32v(UT[:, 8192:16384]).rearrange("p (k n) -> p k n", k=8)]
        cbc = f32v(UT[:, 16384:18432]).rearrange("p (k n) -> p k n", k=8)
        gbt = f32v(UT[:, 18432:20480])
        abb = f32v(UT[:, 20480:22528])
        cbh = UT[:, 22528:23552].rearrange("p (k n) -> p k n", k=8)
        cbl = UT[:, 23552:24576].rearrange("p (k n) -> p k n", k=8)
        tmp = f32v(UT[:, 24576:25600]).rearrange("p (j n) -> p j n", j=4)
        whi = [RX[:, 0:4096].rearrange("p (k n) -> p k n", k=8), RX[:, 4096:8192].rearrange("p (k n) -> p k n", k=8)]
        wlo = [RX[:, 8192:12288].rearrange("p (k n) -> p k n", k=8),
               RX[:, 12288:16384].rearrange("p (k n) -> p k n", k=8)]
        ident = cb[:, CB_ID:CB_ID + 128]
        sc.dma(lambda e: e.dma_start(out=c_sb, in_=self.cT[:, :]), w=[("sm", "c")])
        sc.dma(lambda e: e.dma_start(out=adabT, in_=self.adabT[:, :]), w=[("sm", "adabT")])
        sc.dma(lambda e: e.dma_start(out=ng, in_=self.ngT[:, :]), w=[("sm", "ng")])
        sc.act(lambda e: e.activation(out=cact, in_=c_sb, func=AF.Silu), r=[("sm", "c")], w=[("sm", "cact")])
        for kc in range(8):
            sc.dve(lambda e, kc=kc: e.tensor_scalar(out=cbc[:, kc, :], in0=cf[:, CF_ONES:CF_ONES + 128],
                                                    scalar1=cact[:, kc:kc + 1], scalar2=None, op0=ALU.mult),
                   r=[("sm", "cact"), ("cf", 0)], w=[("UT", "cbc")])
        sc.dve(lambda e: e.tensor_copy(out=cbh, in_=cbc), r=[("UT", "cbc")], w=[("UT", "cbh")])
        sc.dve(lambda e: e.tensor_tensor(out=cbl, in0=cbc, in1=cbh, op=ALU.subtract),
               r=[("UT", "cbc"), ("UT", "cbh")], w=[("UT", "cbl")])
        for l in range(4):
            if l not in self.layers:
                continue
            sc.dma(lambda e, l=l: e.dma_start(out=abb, in_=self.adab[l:l + 1, 2048:3072].partition_broadcast(128)),
                   w=[("UT", "abb")])
            for cc in range(6):
                si = self.ring("adast", 2)
                st = stage[si]
                src_ = self.ada_w[l].rearrange("(k p) n -> p k n", p=128)[:, :, cc * 512:(cc + 1) * 512]
                sc.dma(lambda e, st=st, src_=src_: e.dma_start(out=st, in_=src_), w=[("UT", ("adast", si))])
                wh, wl = whi[si], wlo[si]
                sc.act(lambda e, st=st, wh=wh: e.activation(out=wh, in_=st, func=AF.Copy),
                       r=[("UT", ("adast", si))], w=[("RX", ("whi", si))])
                sc.dve(lambda e, st=st, wh=wh, wl=wl: e.tensor_tensor(out=wl, in0=st, in1=wh, op=ALU.subtract),
                       r=[("UT", ("adast", si)), ("RX", ("whi", si))], w=[("RX", ("wlo", si))])
                pi = self.ring("p0ps", 2)
                pp = self.ps[pi]
                combos = [(cbh, wh, "cbh", "whi"), (cbh, wl, "cbh", "wlo"), (cbl, wh, "cbl", "whi")]
                n = 0
                for (la_, ra_, lk, rk) in combos:
                    for kc in range(8):
                        sc.pe(lambda e, la_=la_, ra_=ra_, kc=kc, pp=pp, n=n: e.matmul(
                            pp[:, :], lhsT=la_[:, kc, :], rhs=ra_[:, kc, :], start=(n == 0), stop=(n == 23)),
                            r=[("UT", lk), ("RX", (rk, si))], w=[pk(pi)])
                        n += 1
                sc.dve(lambda e, pp=pp: e.tensor_tensor(
                    out=tmp, in0=pp[:, :].rearrange("p (j n) -> p j n", j=4),
                    in1=ident.unsqueeze(1).to_broadcast([128, 4, 128]), op=ALU.mult),
                    r=[pk(pi), ("cb", 0)], w=[("UT", "tmp")])
                c0 = l * 24 + cc * 4
                sc.dve(lambda e, c0=c0: e.tensor_reduce(out=modT[:, c0:c0 + 4], in_=tmp, axis=mybir.AxisListType.X,
                                                        op=ALU.add),
                       r=[("UT", "tmp")], w=[("sm", ("modTraw", l, cc))])
                if cc >= 4:
                    half = cc - 4
                    sc.dve(lambda e, half=half, pp=pp: e.tensor_tensor(
                        out=gbt[:, half * 512:(half + 1) * 512], in0=pp[:, :],
                        in1=abb[:, half * 512:(half + 1) * 512], op=ALU.add),
                        r=[pk(pi), ("UT", "abb")], w=[("UT", ("gbt", half))])
            sc.dma(lambda e, l=l: e.dma_start(out=self.gbd[l], in_=gbt), r=[("UT", ("gbt", 0)), ("UT", ("gbt", 1))],
                   w=[("gbd", l)], q="scalar")
        for l in self.layers:
            sc.dve(lambda e, l=l: e.tensor_tensor(out=modT[:, l * 24:(l + 1) * 24], in0=modT[:, l * 24:(l + 1) * 24],
                                                  in1=adabT[:, l * 24:(l + 1) * 24], op=ALU.add),
                   r=[("sm", ("modTraw", l, cc)) for cc in range(6)] + [("sm", "adabT")], w=[("sm", "modT")])
        for l in self.layers:
            sc.dve(lambda e, l=l: e.scalar_tensor_tensor(out=gs[:, l * 8:(l + 1) * 8], in0=modT[:, l * 24 + 8:l * 24 + 16],
                                                         scalar=1.0, in1=ng[:, l * 8:(l + 1) * 8], op0=ALU.add,
                                                         op1=ALU.mult),
                   r=[("sm", "modT"), ("sm", "ng")], w=[("sm", "gs")])
            sc.dve(lambda e, l=l: e.tensor_copy(out=sh[:, l * 8:(l + 1) * 8], in_=modT[:, l * 24:l * 24 + 8]),
                   r=[("sm", "modT")], w=[("sm", "sh")])
        self.fence(["UT", "RX"])

    def p1_norm(self, l, src):
        sc = self.sc
        RW, sm, cb = self.RW, self.sm, self.cb
        UT3 = self.UT[:, :].rearrange("p (k t) -> p k t", k=8)
        ss, rstd, ms = sm[:, 0:32], sm[:, 32:64], sm[:, 64:96]
        gs, sh = sm[:, 96:128], sm[:, 128:160]
        ht = [f32v(RW[:, 0:2048]), f32v(RW[:, 2048:4096])]
        xh = [RW[:, 4096:5120], RW[:, 5120:6144]]
        ident = cb[:, CB_ID:CB_ID + 128]
        self.fence(["RW"])
        for tt in range(NT):
            bi = self.ring("p1ht", 2)
            h_, x_ = ht[bi], xh[bi]
            hk = [("hd", tt)] if src is self.out else []
            sc.dma(lambda e, h_=h_, tt=tt: e.dma_start(out=h_, in_=src[tt * 128:(tt + 1) * 128, :]),
                   r=hk, w=[("RW", ("ht", bi))])
            sc.act(lambda e, h_=h_, x_=x_, tt=tt: e.activation(out=x_, in_=h_, func=AF.Square,
                                                               accum_out=ss[:, tt:tt + 1]),
                   r=[("RW", ("ht", bi))], w=[("RW", ("xh", bi)), ("sm", ("ss", tt))])
            sc.dve(lambda e, tt=tt: e.tensor_scalar(out=ms[:, tt:tt + 1], in0=ss[:, tt:tt + 1], scalar1=1.0 / D,
                                                    scalar2=EPS, op0=ALU.mult, op1=ALU.add),
                   r=[("sm", ("ss", tt))], w=[("sm", ("ms", tt))])
            sc.act(lambda e, tt=tt: e.activation(out=ms[:, tt:tt + 1], in_=ms[:, tt:tt + 1], func=AF.Sqrt),
                   r=[("sm", ("ms", tt))], w=[("sm", ("ms", tt))])
            sc.dve(lambda e, tt=tt: e.reciprocal(out=rstd[:, tt:tt + 1], in_=ms[:, tt:tt + 1]),
                   r=[("sm", ("ms", tt))], w=[("sm", ("rstd", tt))])
            sc.dve(lambda e, h_=h_, x_=x_, tt=tt: e.tensor_scalar(out=x_, in0=h_, scalar1=rstd[:, tt:tt + 1],
                                                                  scalar2=None, op0=ALU.mult),
                   r=[("RW", ("ht", bi)), ("sm", ("rstd", tt))], w=[("RW", ("xh", bi))])
            pi = 6 + self.ring("p1ps", 2)
            pst = self.ps[pi][:, :].bitcast(BF16).rearrange("p (k t) -> p k t", k=8)
            for kc in range(8):
                sc.pe(lambda e, x_=x_, kc=kc, pst=pst: e.transpose(pst[:, kc, :], x_[:, kc * 128:(kc + 1) * 128], ident),
                      r=[("RW", ("xh", bi)), ("cb", 0)], w=[pk(pi)])
            for kc in range(8):
                o = UT3[:, kc, tt * 128:(tt + 1) * 128]
                col = l * 8 + kc
                if True:
                    sc.act(lambda e, o=o, kc=kc, pst=pst, col=col: e.activation(
                        out=o, in_=pst[:, kc, :], func=AF.Identity, bias=sh[:, col:col + 1], scale=gs[:, col:col + 1]),
                        r=[pk(pi), ("sm", "gs"), ("sm", "sh")], w=[("UT", (tt, kc))])
                else:
                    sc.dve(lambda e, o=o, kc=kc, pst=pst, col=col: e.tensor_scalar(
                        out=o, in0=pst[:, kc, :], scalar1=gs[:, col:col + 1], scalar2=sh[:, col:col + 1],
                        op0=ALU.mult, op1=ALU.add),
                        r=[pk(pi), ("sm", "gs"), ("sm", "sh")], w=[("UT", (tt, kc))])

    def ut_keys(self, tiles, kc):
        return [("UT", (t, kc)) for t in tiles]

    def p4_final(self, src):
        sc = self.sc
        self.fence(["UT", "RW"])
        UT, sm = self.UT, self.sm
        fgb = f32v(UT[:, 0:2048])
        ht = [f32v(UT[:, 2048:4096]), f32v(UT[:, 4096:6144])]
        ho = [f32v(UT[:, 6144:8192]), f32v(UT[:, 8192:10240])]
        junk = f32v(UT[:, 10240:12288])
        ss, rstd, ms = sm[:, 0:32], sm[:, 32:64], sm[:, 64:96]
        sc.dma(lambda e: e.dma_start(out=fgb, in_=self.fg.partition_broadcast(128)), w=[("UT", "fgb")])
        for tt in range(NT):
            bi = self.ring("p4", 2)
            h_, o_ = ht[bi], ho[bi]
            hk = [("hd", tt)] if src is self.out else []
            sc.dma(lambda e, h_=h_, tt=tt: e.dma_start(out=h_, in_=src[tt * 128:(tt + 1) * 128, :]),
                   r=hk, w=[("UT", ("ht", bi))])
            sc.act(lambda e, h_=h_, tt=tt: e.activation(out=junk, in_=h_, func=AF.Square, accum_out=ss[:, tt:tt + 1]),
                   r=[("UT", ("ht", bi))], w=[("UT", "junk"), ("sm", ("ss", tt))])
            sc.dve(lambda e, tt=tt: e.tensor_scalar(out=ms[:, tt:tt + 1], in0=ss[:, tt:tt + 1], scalar1=1.0 / D,
                                                    scalar2=EPS, op0=ALU.mult, op1=ALU.add),
                   r=[("sm", ("ss", tt))], w=[("sm", ("ms", tt))])
            sc.act(lambda e, tt=tt: e.activation(out=ms[:, tt:tt + 1], in_=ms[:, tt:tt + 1], func=AF.Sqrt),
                   r=[("sm", ("ms", tt))], w=[("sm", ("ms", tt))])
            sc.dve(lambda e, tt=tt: e.reciprocal(out=rstd[:, tt:tt + 1], in_=ms[:, tt:tt + 1]),
                   r=[("sm", ("ms", tt))], w=[("sm", ("rstd", tt))])
            sc.dve(lambda e, h_=h_, o_=o_, tt=tt: e.scalar_tensor_tensor(
                out=o_, in0=h_, scalar=rstd[:, tt:tt + 1], in1=fgb, op0=ALU.mult, op1=ALU.mult),
                r=[("UT", ("ht", bi)), ("sm", ("rstd", tt)), ("UT", "fgb")], w=[("UT", ("ho", bi))])
            sc.dma(lambda e, o_=o_, tt=tt: e.dma_start(out=self.out[tt * 128:(tt + 1) * 128, :], in_=o_),
                   r=[("UT", ("ho", bi))], w=[("hd", tt)], q="scalar")

    def dump(self, name, ap, keys):
        shape = list(ap.shape)
        d = self.nc.dram_tensor("dbg_" + name, shape, ap.dtype, kind="ExternalOutput").ap()
        self.sc.dma(lambda e: e.dma_start(out=d, in_=ap), r=keys, w=[("dbg", name)], q="scalar")

    def p3_out(self, l, src):
        sc = self.sc
        self.fence(["UT"])
        UT = self.UT
        wd = (self.awout if l % 2 == 0 else self.bwout)[l // 2].rearrange("(k p) n -> p k n", p=128)
        wst = [f32v(UT[:, 0:4096]).rearrange("p (k n) -> p k n", k=2)]
        wob = UT[:, 4096:12288].rearrange("p (k n) -> p k n", k=8)
        gb = f32v(UT[:, 12288:14336])
        ych = [UT[:, 14336:18432].rearrange("p (k t) -> p k t", k=8),
               UT[:, 18432:22528].rearrange("p (k t) -> p k t", k=8)]
        ht = [f32v(UT[:, 22528:24576]), f32v(UT[:, 24576:26624])]
        hn = [f32v(UT[:, 26624:28672]), f32v(UT[:, 28672:30720])]
        sc.dma(lambda e: e.dma_start(out=gb, in_=self.gbd[l]), r=[("gbd", l)], w=[("UT", "gb")])
        for q4 in range(4):
            sc.dma(lambda e, q4=q4: e.dma_start(out=wst[0], in_=wd[:, 2 * q4:2 * q4 + 2, :]), w=[("UT", "wst")])
            for k2 in range(2):
                kc = 2 * q4 + k2
                sc.dve(lambda e, kc=kc, k2=k2: e.tensor_tensor(out=wob[:, kc, :], in0=wst[0][:, k2, :], in1=gb, op=ALU.mult),
                       r=[("UT", "wst"), ("UT", "gb")], w=[("UT", ("wob", kc))])
        for tc in range(8):
            yi = self.ring("p3y", 2)
            yc = ych[yi]
            srcy = self.yTd.rearrange("(k p) t -> p k t", p=128)[:, :, tc * 512:(tc + 1) * 512]
            sc.dma(lambda e, yc=yc, srcy=srcy: e.dma_start(out=yc, in_=srcy),
                   r=[("yT", (h, tc)) for h in range(16)], w=[("UT", ("ych", yi))])
            for t4 in range(4):
                tt = tc * 4 + t4
                bi = self.ring("p3h", 2)
                h_, n_ = ht[bi], hn[bi]
                hk = [("hd", tt)] if src is self.out else []
                sc.dma(lambda e, h_=h_, tt=tt: e.dma_start(out=h_, in_=src[tt * 128:(tt + 1) * 128, :]),
                       r=hk, w=[("UT", ("ht", bi))])
                for half in range(2):
                    pi = 4 + self.ring("p3ps", 4)
                    pp = self.ps[pi]
                    for kc in range(8):
                        sc.pe(lambda e, yc=yc, kc=kc, t4=t4, half=half, pp=pp: e.matmul(
                            pp[:, :], lhsT=yc[:, kc, t4 * 128:(t4 + 1) * 128], rhs=wob[:, kc, half * 512:(half + 1) * 512],
                            start=(kc == 0), stop=(kc == 7)),
                            r=[("UT", ("ych", yi)), ("UT", ("wob", kc))], w=[pk(pi)])
                    sc.dve(lambda e, h_=h_, n_=n_, half=half, pp=pp: e.tensor_tensor(
                        out=n_[:, half * 512:(half + 1) * 512], in0=pp[:, :], in1=h_[:, half * 512:(half + 1) * 512],
                        op=ALU.add),
                        r=[pk(pi), ("UT", ("ht", bi))], w=[("UT", ("hn", bi, half))])
                sc.dma(lambda e, n_=n_, tt=tt: e.dma_start(out=self.out[tt * 128:(tt + 1) * 128, :], in_=n_),
                       r=[("UT", ("hn", bi, 0)), ("UT", ("hn", bi, 1))], w=[("hd", tt)], q="scalar")
        self.fence(["UT"])

    def load_piece(self, dram_ap):
        sc = self.sc
        RW = self.RW
        si = self.ring("wst", 3)
        bi = self.ring("wbf", 4)
        st = f32v(RW[:, si * 2048:(si + 1) * 2048]).rearrange("p (k n) -> p k n", k=8)
        wb = RW[:, 6144 + bi * 1024:6144 + (bi + 1) * 1024].rearrange("p (k n) -> p k n", k=8)
        sc.dma(lambda e: e.dma_start(out=st, in_=dram_ap), w=[("RW", ("wst", si))])
        sc.pool(lambda e: e.tensor_copy(out=wb, in_=st), r=[("RW", ("wst", si))], w=[("RW", ("wbf", bi))])
        return wb, ("RW", ("wbf", bi))

    def proj_rope(self, wb, wkey, sink, pre_chunk=None):
        sc = self.sc
        RT, cb = self.RT, self.cb
        UT3 = self.UT[:, :].rearrange("p (k t) -> p k t", k=8)
        C = self.TAB[:, 0:4096]
        Sg = self.TAB[:, 4096:8192]
        swap = cb[:, CB_SWAP:CB_SWAP + 128]
        qb = [RT[:, 0:512], RT[:, 512:1024]]
        t1 = [f32v(RT[:, 1024:2048]), f32v(RT[:, 2048:3072])]
        t2 = [f32v(RT[:, 3072:4096]), f32v(RT[:, 4096:5120])]
        pend = None

        def do_swap(p):
            tc, bi, pa = p
            pb = 2 + self.ring("psB", 2)
            sc.pe(lambda e: e.matmul(self.ps[pb][:, :], lhsT=swap, rhs=qb[bi], start=True, stop=True),
                  r=[("RT", ("qb", bi)), ("cb", 0)], w=[pk(pb)])
            sl = slice(tc * 512, (tc + 1) * 512)
            sc.dve(lambda e: e.tensor_tensor(out=t1[bi], in0=self.ps[pb][:, :], in1=Sg[:, sl], op=ALU.mult),
                   r=[pk(pb), ("TAB", 0)], w=[("RT", ("t1", bi))])
            sc.pool(lambda e: e.tensor_tensor(out=t2[bi], in0=qb[bi], in1=C[:, sl], op=ALU.mult),
                    r=[("RT", ("qb", bi)), ("TAB", 0)], w=[("RT", ("t2", bi))])
            sink(tc, t1[bi], t2[bi], [("RT", ("t1", bi)), ("RT", ("t2", bi))])

        for tc in range(8):
            if pre_chunk is not None:
                pre_chunk(tc)
            pa = self.ring("psA", 2)
            bi = self.ring("qb", 2)
            for kc in range(8):
                sc.pe(lambda e, kc=kc, tc=tc, pa=pa: e.matmul(
                    self.ps[pa][:, :], lhsT=wb[:, kc, :], rhs=UT3[:, kc, tc * 512:(tc + 1) * 512],
                    start=(kc == 0), stop=(kc == 7)),
                    r=[wkey] + self.ut_keys(range(tc * 4, tc * 4 + 4), kc), w=[pk(pa)])
            sc.act(lambda e, pa=pa, bi=bi: e.activation(out=qb[bi], in_=self.ps[pa][:, :], func=AF.Copy),
                   r=[pk(pa)], w=[("RT", ("qb", bi))])
            if pend is not None:
                do_swap(pend)
            pend = (tc, bi, pa)
        do_swap(pend)

    def p2_a(self, l):
        sc = self.sc
        la = l // 2
        RX, RY, RZ, RT, cb, cf = self.RX, self.RY, self.RZ, self.RT, self.cb, self.cf
        UT3 = self.UT[:, :].rearrange("p (k t) -> p k t", k=8)
        acc = [f32v(RX[:, 0:8192]), f32v(RX[:, 8192:16384])]
        QT, KT = RY[:, 0:4096], RY[:, 4096:8192]
        Vp = RZ[:, 0:4160].rearrange("p (t e c) -> p t e c", t=32, e=2)
        band = cb[:, CB_BAND:CB_BAND + 256]
        self.fence(["RX", "RY", "RZ", "RW", "RT"])
        sc.pool(lambda e: e.memset(RZ[:, 0:4160], 1.0), w=[("RZ", "*")])
        PTs = [RT[:, 5120 + i * 512:5120 + (i + 1) * 512].rearrange("p (e q) -> p e q", e=2) for i in range(3)]
        th = f32v(RT[:, 6656:7680])
        sg = f32v(RT[:, 7680:8704])
        tq = f32v(RT[:, 8704:9728])
        rd = f32v(RT[:, 9728:10752])
        yb = [RT[:, 10752:11264], RT[:, 11264:11776]]

        def sink_to(dst, dkey, d, eng):
            def sink(tc, t1, t2, keys):
                dv = dst.rearrange("p (r i) -> p r i", r=d)[:, :, tc * 512 // d:(tc + 1) * 512 // d]
                a = t1.rearrange("p (i r) -> p r i", r=d)
                b = t2.rearrange("p (i r) -> p r i", r=d)
                sc.add(eng, lambda e: e.tensor_tensor(out=dv, in0=a, in1=b, op=ALU.add), r=keys, w=[dkey])
            return sink

        for hp in range(8):
            for g, (win, d) in enumerate(A_GROUPS):
                nb = 32 // d
                wb, wk = self.load_piece(self.awin[la, hp, 3 * g + 0])
                self.proj_rope(wb, wk, sink_to(QT, ("RY", "QT"), d, "vector"))
                wb, wk = self.load_piece(self.awin[la, hp, 3 * g + 1])
                self.proj_rope(wb, wk, sink_to(KT, ("RY", "KT"), d, "vector"))
                wb, wk = self.load_piece(self.awin[la, hp, 3 * g + 2])
                UTd = UT3.rearrange("p k (i r) -> p k r i", r=d)
                for J0 in range(0, 32, 4):
                    pv = 2 + self.ring("psB", 2)
                    for jj in range(4):
                        J = J0 + jj
                        r_, b_ = J // nb, J % nb
                        tiles = range(d * b_, d * b_ + d)
                        for kc in range(8):
                            sc.pe(lambda e, kc=kc, jj=jj, r_=r_, b_=b_, pv=pv, wb=wb, UTd=UTd: e.matmul(
                                self.ps[pv][:, jj * 128:(jj + 1) * 128], lhsT=UTd[:, kc, r_, b_ * 128:(b_ + 1) * 128],
                                rhs=wb[:, kc, :], start=(kc == 0), stop=(kc == 7)),
                                r=[wk] + self.ut_keys(tiles, kc), w=[pk(pv)])
                    sc.act(lambda e, pv=pv, J0=J0: e.activation(
                        out=Vp[:, J0:J0 + 4, :, 0:64],
                        in_=self.ps[pv][:, :].rearrange("p (j e c) -> p j e c", j=4, e=2), func=AF.Copy),
                        r=[pk(pv)], w=[("RZ", ("V", J0))])
                def blk(J, nb=nb):
                    first = (J % nb == 0)
                    has_off = (J + 1 < 32) and ((J + 1) % nb != 0)
                    return first, has_off, (256 if has_off else 128)

                def sbank(J, e_):
                    return (4 + e_) if J % 2 == 0 else e_

                def emit_S(J):
                    _, _, N = blk(J)
                    for e_ in range(2):
                        bS = sbank(J, e_)
                        sc.pe(lambda e, e_=e_, J=J, N=N, bS=bS: e.matmul(
                            self.ps[bS][:, 0:N], lhsT=KT[64 * e_:64 * e_ + 64, J * 128:(J + 1) * 128],
                            rhs=QT[64 * e_:64 * e_ + 64, J * 128:J * 128 + N], start=True, stop=True),
                            r=[("RY", "QT"), ("RY", "KT")], w=[pk(bS)])

                emit_S(0)
                for J in range(32):
                    first, has_off, N = blk(J)
                    if J + 1 < 32:
                        emit_S(J + 1)
                    pti = self.ring("PT", 3)
                    PT = PTs[pti]
                    for e_ in range(2):
                        bS = sbank(J, e_)
                        sc.act(lambda e, PT=PT, e_=e_, N=N, bS=bS: e.activation(out=PT[:, e_, 0:N], in_=self.ps[bS][:, 0:N],
                                                                              func=AF.Exp, scale=0.125),
                               r=[pk(bS)], w=[("RT", ("PT", pti))])
                    sc.dve(lambda e, PT=PT, N=N: e.tensor_tensor(
                        out=PT[:, :, 0:N], in0=PT[:, :, 0:N],
                        in1=band[:, 0:N].unsqueeze(1).to_broadcast([128, 2, N]), op=ALU.mult),
                        r=[("RT", ("PT", pti)), ("cb", 0)], w=[("RT", ("PT", pti))])
                    bk = 6 + (J % 2)
                    bk2 = 6 + ((J + 1) % 2)
                    for e_ in range(2):
                        sc.pe(lambda e, e_=e_, J=J, PT=PT, bk=bk, first=first: e.matmul(
                            self.ps[bk][0:65, e_ * 128:(e_ + 1) * 128], lhsT=Vp[:, J, e_, :], rhs=PT[:, e_, 0:128],
                            start=(first and e_ == 0), stop=True, skip_group_check=True),
                            r=[("RZ", ("V", (J // 4) * 4)), ("RT", ("PT", pti))], w=[pk(bk)])
                    if has_off:
                        for e_ in range(2):
                            sc.pe(lambda e, e_=e_, J=J, PT=PT, bk2=bk2: e.matmul(
                                self.ps[bk2][0:65, e_ * 128:(e_ + 1) * 128], lhsT=Vp[:, J, e_, :], rhs=PT[:, e_, 128:256],
                                start=(e_ == 0), stop=False, skip_group_check=True),
                                r=[("RZ", ("V", (J // 4) * 4)), ("RT", ("PT", pti))], w=[pk(bk2)])
                    r_, b_ = J // nb, J % nb
                    tiles = range(d * b_, d * b_ + d)
                    for e_ in range(2):
                        pO = self.ps[bk]
                        av = acc[e_].rearrange("p (i r) -> p r i", r=d)[0:65, r_, b_ * 128:(b_ + 1) * 128]
                        akeys = [("RX", ("acc", e_, t)) for t in tiles]
                        if g == 0:
                            sc.dve(lambda e, av=av, pO=pO, e_=e_: e.tensor_copy(
                                out=av, in_=pO[0:65, e_ * 128:(e_ + 1) * 128]),
                                r=[pk(bk)], w=akeys)
                        else:
                            sc.dve(lambda e, av=av, pO=pO, e_=e_: e.tensor_tensor(
                                out=av, in0=pO[0:65, e_ * 128:(e_ + 1) * 128], in1=av, op=ALU.add),
                                r=[pk(bk)] + akeys, w=akeys)
            wbg, wkg = self.load_piece(self.awin[la, hp, 9])
            units = []
            for e_ in range(2):
                for tc in range(8):
                    units.append(dict(h=2 * hp + e_, tc=tc, acc=acc[e_][:, tc * 512:(tc + 1) * 512],
                                      akeys=[("RX", ("acc", e_, t)) for t in range(tc * 4, tc * 4 + 4)],
                                      gate=("proj", wbg, wkg, e_)))
            self.finalize_units(units)

    def finalize_units(self, units):
        sc = self.sc
        RT = self.RT
        UT3 = self.UT[:, :].rearrange("p (k t) -> p k t", k=8)
        th = f32v(RT[:, 6656:7680])
        sg = f32v(RT[:, 7680:8704])
        tq = f32v(RT[:, 8704:9728])
        yb = [RT[:, 10752:11264], RT[:, 11264:11776]]
        rdf = [f32v(RT[:, 9728:10752])[64:65, :], f32v(RT[64:65, 10752:11776])]
        rdh = [RT[64:65, 11776:12288], RT[64:65, 7680:8192]]
        rdl = [RT[64:65, 6656:7168], RT[64:65, 8704:9216]]
        onesb = self.cb[64:65, CB_ONES:CB_ONES + 64]
        st = {}

        def stage1(i):
            u = units[i]
            ri = i % 2
            acc_ap, akeys = u["acc"], u["akeys"]
            sc.dve(lambda e: e.tensor_scalar(out=rdf[ri], in0=acc_ap[64:65, :], scalar1=2.0, scalar2=None, op0=ALU.mult),
                   r=akeys, w=[("RT", ("rdf", ri))])
            sc.dve(lambda e: e.reciprocal(out=rdf[ri], in_=rdf[ri]), r=[("RT", ("rdf", ri))], w=[("RT", ("rdf", ri))])
            sc.act(lambda e: e.activation(out=rdh[ri], in_=rdf[ri], func=AF.Copy),
                   r=[("RT", ("rdf", ri))], w=[("RT", ("rdh", ri))])
            sc.dve(lambda e: e.tensor_tensor(out=rdl[ri], in0=rdf[ri], in1=rdh[ri], op=ALU.subtract),
                   r=[("RT", ("rdf", ri)), ("RT", ("rdh", ri))], w=[("RT", ("rdl", ri))])
            if u["gate"][0] == "dram":
                u["gate"][3]()
            if u["gate"][0] == "proj":
                _, wbg, wkg, e_ = u["gate"]
                tc = u["tc"]
                pg = 2 + ri
                for kc in range(8):
                    sc.pe(lambda e, kc=kc: e.matmul(
                        self.ps[pg][0:64, :], lhsT=wbg[:, kc, 64 * e_:64 * e_ + 64],
                        rhs=UT3[:, kc, tc * 512:(tc + 1) * 512], start=(kc == 0), stop=(kc == 7)),
                        r=[wkg] + self.ut_keys(range(tc * 4, tc * 4 + 4), kc), w=[pk(pg)])

        def stage2(i):
            u = units[i]
            ri = i % 2
            acc_ap, akeys = u["acc"], u["akeys"]
            pb = ri
            sc.pe(lambda e: e.matmul(self.ps[pb][0:64, :], lhsT=onesb, rhs=rdh[ri], start=True, stop=False),
                  r=[("RT", ("rdh", ri)), ("cb", 0)], w=[pk(pb)])
            sc.pe(lambda e: e.matmul(self.ps[pb][0:64, :], lhsT=onesb, rhs=rdl[ri], start=False, stop=True),
                  r=[("RT", ("rdl", ri)), ("cb", 0)], w=[pk(pb)])
            if u["gate"][0] == "proj":
                pg = 2 + ri
                sc.act(lambda e: e.activation(out=th[0:64, :], in_=self.ps[pg][0:64, :], func=AF.Tanh, scale=0.5),
                       r=[pk(pg)], w=[("RT", "th")])
                sc.dve(lambda e: e.scalar_tensor_tensor(out=sg[0:64, :], in0=th[0:64, :], scalar=1.0,
                                                        in1=self.ps[pg][0:64, :], op0=ALU.add, op1=ALU.mult),
                       r=[pk(pg), ("RT", "th")], w=[("RT", "sg")])
                gsrc, gkeys = sg[0:64, :], [("RT", "sg")]
            else:
                gsrc, gkeys = u["gate"][1], u["gate"][2]
            sc.dve(lambda e: e.tensor_tensor(out=tq[0:64, :], in0=acc_ap[0:64, :], in1=self.ps[pb][0:64, :], op=ALU.mult),
                   r=akeys + [pk(pb)], w=[("RT", "tq")])
            yi = self.ring("yb", 2)
            y_ = yb[yi]
            sc.pool(lambda e: e.tensor_tensor(out=y_[0:64, :], in0=tq[0:64, :], in1=gsrc, op=ALU.mult),
                    r=[("RT", "tq")] + gkeys, w=[("RT", ("yb", yi))])
            h, tc = u["h"], u["tc"]
            sc.dma(lambda e: e.dma_start(out=self.yTd[h * 64:(h + 1) * 64, tc * 512:(tc + 1) * 512], in_=y_[0:64, :]),
                   r=[("RT", ("yb", yi))], w=[("yT", (h, tc))], q="scalar")

        stage1(0)
        for i in range(len(units)):
            if i + 1 < len(units):
                stage1(i + 1)
            stage2(i)

    def p2_b(self, l):
        sc = self.sc
        lb = l // 2
        UT, RX, RY, RZ, RW, RT, cb, cf, sm = self.UT, self.RX, self.RY, self.RZ, self.RW, self.RT, self.cb, self.cf, self.sm
        UT3 = UT[:, :].rearrange("p (k t) -> p k t", k=8)
        KT2 = RX[:, :].rearrange("p (g t) -> p g t", g=4)
        Vp = RY[:, 0:8320].rearrange("p (t g c) -> p t g c", t=32, g=4)
        KiT2 = RZ[:, 0:4096]
        wtok = self.wtok[:, 0:256]
        lo_t = self.wtok[:, 256:512]
        hi_t = self.wtok[:, 512:768]
        bw = self.bwin[lb]
        self.fence(["RX", "RY", "RZ", "RW", "RT"])
        sc.pool(lambda e: e.memset(RY[:, 0:8320], 1.0), w=[("RY", "*")])
        ob = [RT[:, 5120:5632], RT[:, 5632:6144]]
        th = f32v(RT[:, 6656:7680])
        sg = f32v(RT[:, 7680:8704])
        tq = f32v(RT[:, 8704:9728])
        rd = f32v(RT[:, 9728:10752])
        yb = [RT[:, 10752:11264], RT[:, 11264:11776]]
        wr = [th, sg]

        def sink_sb(dst3, g, dkey):
            def sink(tc, t1, t2, keys):
                sc.dve(lambda e: e.tensor_tensor(out=dst3[:, g, tc * 512:(tc + 1) * 512], in0=t1, in1=t2, op=ALU.add),
                       r=keys, w=[dkey])
            return sink

        def sink_dram(dram_rows, tag, mul=None):
            def sink(tc, t1, t2, keys):
                oi = self.ring("ob", 2)
                o_ = ob[oi]
                if mul is None:
                    sc.dve(lambda e: e.tensor_tensor(out=o_, in0=t1, in1=t2, op=ALU.add), r=keys, w=[("RT", ("ob", oi))])
                else:
                    wv, wkeys = mul(tc)
                    sc.dve(lambda e: e.tensor_tensor(out=t1, in0=t1, in1=t2, op=ALU.add), r=keys, w=[keys[0]])
                    sc.pool(lambda e: e.tensor_tensor(out=o_, in0=t1, in1=wv, op=ALU.mult),
                            r=[keys[0]] + wkeys, w=[("RT", ("ob", oi))])
                sc.dma(lambda e: e.dma_start(out=dram_rows[:, tc * 512:(tc + 1) * 512], in_=o_),
                       r=[("RT", ("ob", oi))], w=[(tag, tc)], q="scalar")
            return sink

        for g in range(4):
            wb, wk = self.load_piece(bw[BP_K2 + g])
            self.proj_rope(wb, wk, sink_sb(KT2, g, ("RX", ("KT2", g))))
        wb, wk = self.load_piece(bw[BP_KI2])
        KiT3 = RZ[:, 0:4096].rearrange("p (g t) -> p g t", g=1)
        self.proj_rope(wb, wk, sink_sb(KiT3, 0, ("RZ", "KiT2")))
        for p in range(2):
            wb, wk = self.load_piece(bw[BP_V + p])
            for J0 in range(0, 32, 4):
                pv = 2 + self.ring("psB", 2)
                for jj in range(4):
                    J = J0 + jj
                    for kc in range(8):
                        sc.pe(lambda e, kc=kc, jj=jj, J=J, pv=pv, wb=wb: e.matmul(
                            self.ps[pv][:, jj * 128:(jj + 1) * 128], lhsT=UT3[:, kc, J * 128:(J + 1) * 128],
                            rhs=wb[:, kc, :], start=(kc == 0), stop=(kc == 7)),
                            r=[wk] + self.ut_keys([J], kc), w=[pk(pv)])
                sc.act(lambda e, pv=pv, J0=J0, p=p: e.activation(
                    out=Vp[:, J0:J0 + 4, 2 * p:2 * p + 2, 0:64],
                    in_=self.ps[pv][:, :].rearrange("p (j e c) -> p j e c", j=4, e=2), func=AF.Copy),
                    r=[pk(pv)], w=[("RY", ("V", J0, p))])
        wb, wk = self.load_piece(bw[BP_WI])
        pw = 2 + self.ring("psB", 2)
        for J in range(32):
            for kc in range(8):
                sc.pe(lambda e, kc=kc, J=J, wb=wb: e.matmul(
                    self.ps[pw][:, J * 8:(J + 1) * 8], lhsT=UT3[:, kc, J * 128:(J + 1) * 128], rhs=wb[:, kc, 0:8],
                    start=(kc == 0), stop=(kc == 7)), r=[wk] + self.ut_keys([J], kc), w=[pk(pw)])
        sc.act(lambda e: e.activation(out=wtok, in_=self.ps[pw][:, 0:256], func=AF.Copy), r=[pk(pw)], w=[("wtok", "w")])
        sc.dve(lambda e: e.tensor_scalar(out=lo_t, in0=wtok, scalar1=0.0, scalar2=2.0, op0=ALU.is_ge, op1=ALU.mult),
               r=[("wtok", "w")], w=[("wtok", "lo")])
        sc.dve(lambda e: e.tensor_scalar(out=lo_t, in0=lo_t, scalar1=-1.0, scalar2=None, op0=ALU.add),
               r=[("wtok", "lo")], w=[("wtok", "lo")])
        for p in range(4):
            wbr, wkr = self.load_piece(bw[BP_WREP + p])
            wbq, wkq = self.load_piece(bw[BP_QI + p])

            def pre_chunk(tc, wbr=wbr, wkr=wkr):
                pa = self.ring("psA", 2)
                wi_ = tc % 2
                for kc in range(8):
                    sc.pe(lambda e, kc=kc, pa=pa: e.matmul(
                        self.ps[pa][:, :], lhsT=wbr[:, kc, :], rhs=UT3[:, kc, tc * 512:(tc + 1) * 512],
                        start=(kc == 0), stop=(kc == 7)),
                        r=[wkr] + self.ut_keys(range(tc * 4, tc * 4 + 4), kc), w=[pk(pa)])
                sc.act(lambda e, pa=pa, wi_=wi_: e.activation(out=wr[wi_], in_=self.ps[pa][:, :], func=AF.Abs,
                                                             scale=IDX_SCALE),
                       r=[pk(pa)], w=[("RT", ("wr", wi_))])

            def mul(tc):
                return wr[tc % 2], [("RT", ("wr", tc % 2))]

            self.proj_rope(wbq, wkq, sink_dram(self.QiTd[p * 128:(p + 1) * 128, :], ("QiT", p), mul=mul),
                           pre_chunk=pre_chunk)
        for p in range(8):
            wb, wk = self.load_piece(bw[BP_Q + p])
            self.proj_rope(wb, wk, sink_dram(self.QTd[p * 128:(p + 1) * 128, :], ("QT", p)))
        for p in range(8):
            wb, wk = self.load_piece(bw[BP_GATE + p])
            for e_ in range(2):
                h = 2 * p + e_
                for tc in range(8):
                    pg = 2 + self.ring("psB", 2)
                    for kc in range(8):
                        sc.pe(lambda e, kc=kc, pg=pg, wb=wb, e_=e_, tc=tc: e.matmul(
                            self.ps[pg][0:64, :], lhsT=wb[:, kc, 64 * e_:64 * e_ + 64],
                            rhs=UT3[:, kc, tc * 512:(tc + 1) * 512], start=(kc == 0), stop=(kc == 7)),
                            r=[wk] + self.ut_keys(range(tc * 4, tc * 4 + 4), kc), w=[pk(pg)])
                    sc.act(lambda e, pg=pg: e.activation(out=tq[0:64, :], in_=self.ps[pg][0:64, :], func=AF.Tanh, scale=0.5),
                           r=[pk(pg)], w=[("RT", "tq")])
                    oi = self.ring("ob", 2)
                    o_ = ob[oi]
                    sc.dve(lambda e, pg=pg, o_=o_: e.scalar_tensor_tensor(
                        out=o_[0:64, :], in0=tq[0:64, :], scalar=1.0, in1=self.ps[pg][0:64, :], op0=ALU.add, op1=ALU.mult),
                        r=[pk(pg), ("RT", "tq")], w=[("RT", ("ob", oi))])
                    sc.dma(lambda e, o_=o_, h=h, tc=tc: e.dma_start(
                        out=self.GTd[h * 64:(h + 1) * 64, tc * 512:(tc + 1) * 512], in_=o_[0:64, :]),
                        r=[("RT", ("ob", oi))], w=[(("GT", h), tc)], q="scalar")

        self.fence(["UT", "RW", "RT"])
        accB = f32v(UT[:, 0:16384]).rearrange("p (h q) -> p h q", h=16)
        score = f32v(UT[:, 16384:24576])
        maskb = UT[:, 24576:28672]
        maskT = UT[:, 28672:32768].rearrange("p (j q) -> p j q", j=32)
        QTc = RW[:, 0:4096].rearrange("p (a q) -> p a q", a=8)
        QiTc = RW[:, 4096:6144].rearrange("p (a q) -> p a q", a=4)
        tmpS = [f32v(RW[:, 6144:7168]), f32v(RW[:, 7168:8192])]
        GTc = [RW[:, 8192:8704], RW[:, 8704:9216]]
        PTs = [RT[:, 5120 + i * 512:5120 + (i + 1) * 512] for i in range(3)]
        ident = cb[:, CB_ID:CB_ID + 128]
        caus = cf[:, CF_CAUS:CF_CAUS + 128]
        for qt in range(32):
            sc4, q0 = qt // 4, (qt % 4) * 128
            csl = slice(sc4 * 512, (sc4 + 1) * 512)
            if qt % 4 == 0:
                sc.dma(lambda e, csl=csl: e.dma_start(out=QTc, in_=self.QTd.rearrange("(a p) t -> p a t", p=128)[:, :, csl]),
                       r=[(("QT", p), sc4) for p in range(8)], w=[("RW", "QTc")])
                sc.dma(lambda e, csl=csl: e.dma_start(out=QiTc, in_=self.QiTd.rearrange("(a p) t -> p a t", p=128)[:, :, csl]),
                       r=[(("QiT", p), sc4) for p in range(4)], w=[("RW", "QiTc")])
            nk = qt + 1
            ncols = nk * 128
            for c0 in range(0, ncols, 512):
                cw = min(512, ncols - c0)
                for hh in range(8):
                    par = hh % 2
                    sc.pe(lambda e, hh=hh, par=par, c0=c0, cw=cw, q0=q0: e.matmul(
                        self.ps[2 + par][:, 0:cw], lhsT=QiTc[64 * par:64 * par + 64, hh // 2, q0:q0 + 128],
                        rhs=KiT2[64 * par:64 * par + 64, c0:c0 + cw], start=True, stop=True),
                        r=[("RW", "QiTc"), ("RZ", "KiT2")], w=[pk(2 + par)])
                    col = qt * 8 + hh
                    ti = self.ring("tmpS", 2)
                    t_ = tmpS[ti]
                    sc.act(lambda e, par=par, cw=cw, t_=t_: e.activation(out=t_[:, 0:cw], in_=self.ps[2 + par][:, 0:cw],
                                                                       func=AF.Relu),
                           r=[pk(2 + par)], w=[("RW", ("tmpS", ti))])
                    if hh == 0:
                        sc.dve(lambda e, c0=c0, cw=cw, col=col, t_=t_: e.tensor_scalar(
                            out=score[:, c0:c0 + cw], in0=t_[:, 0:cw], scalar1=lo_t[:, col:col + 1], scalar2=None,
                            op0=ALU.mult),
                            r=[("RW", ("tmpS", ti)), ("wtok", "lo")], w=[("UT", "score")])
                    else:
                        sc.dve(lambda e, c0=c0, cw=cw, col=col, t_=t_: e.scalar_tensor_tensor(
                            out=score[:, c0:c0 + cw], in0=t_[:, 0:cw], scalar=lo_t[:, col:col + 1],
                            in1=score[:, c0:c0 + cw], op0=ALU.mult, op1=ALU.add),
                            r=[("RW", ("tmpS", ti)), ("wtok", "lo"), ("UT", "score")], w=[("UT", "score")])
            sc.dve(lambda e, qt=qt: e.tensor_tensor(out=score[:, qt * 128:(qt + 1) * 128],
                                                    in0=score[:, qt * 128:(qt + 1) * 128], in1=caus, op=ALU.add),
                   r=[("UT", "score"), ("cf", 0)], w=[("UT", "score")])
            if qt >= 2:
                mid = sm[:, 400:401]
                cnt = sm[:, 401:402]
                ind = sm[:, 402:403]
                thr = sm[:, 403:404]
                sc.dve(lambda e: e.memset(mid, BIS_LO + BIS_STEP0), w=[("sm", "mid")])
                sgn = sm[:, 404:405]
                tot = sm[:, 405:406]
                nD = (nk // 2) * 128 if nk >= 8 else ncols
                nA = ncols - nD
                for it in range(NIT):
                    step = BIS_STEP0 / (2.0 ** it)
                    sc.dve(lambda e, nD=nD: e.tensor_scalar(
                        out=maskb[:, 0:nD], in0=score[:, 0:nD], scalar1=mid, scalar2=None, op0=ALU.is_gt,
                        op1=ALU.add, accum_out=cnt),
                        r=[("UT", "score"), ("sm", "mid")], w=[("UT", "maskb"), ("sm", "cnt")])
                    if nA > 0:
                        sc.act(lambda e, nD=nD, ncols=ncols: e.activation(
                            out=maskb[:, nD:ncols], in_=score[:, nD:ncols], func=AF.Sign, bias=mid, scale=-1.0,
                            accum_out=sgn),
                            r=[("UT", "score"), ("sm", "mid")], w=[("UT", "maskbA"), ("sm", ("sgn", True))])
                        sc.dve(lambda e: e.scalar_tensor_tensor(out=tot, in0=sgn, scalar=-0.5, in1=cnt, op0=ALU.mult,
                                                                op1=ALU.add),
                               r=[("sm", ("sgn", True)), ("sm", "cnt")], w=[("sm", "tot")])
                        thr_c = TOPK - 0.5 - nA / 2.0
                        sc.dve(lambda e, step=step, thr_c=thr_c: e.tensor_scalar(out=ind, in0=tot, scalar1=thr_c, scalar2=step,
                                                                                 op0=ALU.is_gt, op1=ALU.mult),
                               r=[("sm", "tot")], w=[("sm", "ind")])
                    else:
                        sc.dve(lambda e, step=step: e.tensor_scalar(out=ind, in0=cnt, scalar1=TOPK - 0.5, scalar2=step,
                                                                   op0=ALU.is_gt, op1=ALU.mult),
                               r=[("sm", "cnt")], w=[("sm", "ind")])
                    sc.dve(lambda e, step=step: e.scalar_tensor_tensor(out=mid, in0=ind, scalar=-step / 2.0, in1=mid,
                                                                       op0=ALU.add, op1=ALU.add),
                           r=[("sm", "ind"), ("sm", "mid")], w=[("sm", "mid")])
                fstep = BIS_STEP0 / (2.0 ** NIT)
                sc.dve(lambda e: e.tensor_scalar(out=thr, in0=mid, scalar1=-fstep, scalar2=None, op0=ALU.add),
                       r=[("sm", "mid")], w=[("sm", "thr")])
                sc.dve(lambda e, ncols=ncols: e.tensor_scalar(out=maskb[:, 0:ncols], in0=score[:, 0:ncols], scalar1=thr,
                                                              scalar2=None, op0=ALU.is_gt),
                       r=[("UT", "score"), ("sm", "thr")], w=[("UT", "maskb"), ("UT", "maskbA")])
            else:
                sc.dve(lambda e, ncols=ncols: e.tensor_scalar(out=maskb[:, 0:ncols], in0=score[:, 0:ncols],
                                                              scalar1=-1.0e29, scalar2=None, op0=ALU.is_gt),
                       r=[("UT", "score")], w=[("UT", "maskb")])
            for j0 in range(0, nk, 8):
                m = min(8, nk - j0)
                pt_ = 2 + self.ring("psB", 2)
                ptv = self.ps[pt_][:, :].bitcast(BF16).rearrange("p (j q) -> p j q", j=8)
                for jj in range(m):
                    sc.pe(lambda e, jj=jj, j0=j0, ptv=ptv: e.transpose(
                        ptv[:, jj, :], maskb[:, (j0 + jj) * 128:(j0 + jj + 1) * 128], ident),
                        r=[("UT", "maskb"), ("UT", "maskbA"), ("cb", 0)], w=[pk(pt_)])
                sc.act(lambda e, ptv=ptv, j0=j0, m=m: e.activation(out=maskT[:, j0:j0 + m, :], in_=ptv[:, 0:m, :],
                                                                  func=AF.Copy),
                       r=[pk(pt_)], w=[("UT", "maskT")])
            steps = [(gk, j) for gk in range(4) for j in range(nk)]
            pos_ = {}
            for gk in range(4):
                pos_[gk] = 6 + self.ring("psO", 2)

            def sbankb(i, par):
                return (4 + par) if i % 2 == 0 else par

            def emit_Sb(i, q0=q0):
                gk, j = steps[i]
                for r_ in range(4):
                    par, r2 = r_ % 2, r_ // 2
                    bS = sbankb(i, par)
                    sc.pe(lambda e, par=par, r2=r2, gk=gk, j=j, q0=q0, bS=bS: e.matmul(
                        self.ps[bS][:, r2 * 128:(r2 + 1) * 128],
                        lhsT=KT2[64 * par:64 * par + 64, gk, j * 128:(j + 1) * 128],
                        rhs=QTc[64 * par:64 * par + 64, 2 * gk + r2, q0:q0 + 128], start=True, stop=True),
                        r=[("RX", ("KT2", gk)), ("RW", "QTc")], w=[pk(bS)])

            emit_Sb(0)
            for i, (gk, j) in enumerate(steps):
                po = pos_[gk]
                if i + 1 < len(steps):
                    emit_Sb(i + 1)
                pti = self.ring("PT", 3)
                PT4 = PTs[pti].rearrange("p (a b q) -> p a b q", a=2, b=2)
                for par in range(2):
                    bS = sbankb(i, par)
                    sc.act(lambda e, par=par, PT4=PT4, bS=bS: e.activation(
                        out=PT4[:, par, :, :], in_=self.ps[bS][:, 0:256].rearrange("p (b q) -> p b q", b=2),
                        func=AF.Exp, scale=0.125), r=[pk(bS)], w=[("RT", ("PT", pti))])
                PTf = PTs[pti].rearrange("p (a q) -> p a q", a=4)
                sc.dve(lambda e, PTf=PTf, j=j: e.tensor_tensor(
                    out=PTf, in0=PTf, in1=maskT[:, j, :].unsqueeze(1).to_broadcast([128, 4, 128]), op=ALU.mult),
                    r=[("RT", ("PT", pti)), ("UT", "maskT")], w=[("RT", ("PT", pti))])
                for r_ in range(4):
                    par, r2 = r_ % 2, r_ // 2
                    sc.pe(lambda e, par=par, r2=r2, r_=r_, gk=gk, j=j, PT4=PT4, po=po: e.matmul(
                        self.ps[po][0:65, r_ * 128:(r_ + 1) * 128], lhsT=Vp[:, j, gk, :], rhs=PT4[:, par, r2, :],
                        start=(j == 0 and r_ == 0), stop=(j == nk - 1), skip_group_check=True),
                        r=[("RY", ("V", (j // 4) * 4, gk // 2)), ("RT", ("PT", pti))], w=[pk(po)])
                if j == nk - 1:
                    sc.act(lambda e, gk=gk, po=po, q0=q0: e.activation(
                        out=accB[0:65, 4 * gk:4 * gk + 4, q0:q0 + 128],
                        in_=self.ps[po][0:65, :].rearrange("p (r q) -> p r q", r=4), func=AF.Copy),
                        r=[pk(po)], w=[("UT", ("accB", gk))])
            if qt % 4 == 3:
                units = []
                for h in range(16):
                    gi = h % 2
                    g_ = GTc[gi]

                    def loader(g_=g_, h=h, csl=csl, gi=gi, sc4=sc4):
                        sc.dma(lambda e: e.dma_start(out=g_[0:64, :], in_=self.GTd[h * 64:(h + 1) * 64, csl]),
                               r=[(("GT", h), sc4)], w=[("RW", ("GTc", gi))])

                    units.append(dict(h=h, tc=sc4, acc=accB[:, h, :], akeys=[("UT", ("accB", h // 4))],
                                      gate=("dram", g_[0:64, :], [("RW", ("GTc", gi))], loader)))
                self.finalize_units(units)


_SHARED = {}


def prep_shared(inputs):
    cbv, cfv = make_consts()
    norm_g = np.asarray(inputs["norm_g"], np.float32)
    ada_b = np.asarray(inputs["ada_b"], np.float32)
    sh = dict(
        ngT=np.ascontiguousarray(norm_g.reshape(4, 8, 128).transpose(2, 0, 1).reshape(128, 32)),
        fg=np.ascontiguousarray(np.asarray(inputs["final_g"], np.float32).reshape(1, D)),
        ada_w=np.ascontiguousarray(np.asarray(inputs["ada_w"], np.float32)),
        adabT=np.ascontiguousarray(ada_b.reshape(4, 24, 128).transpose(2, 0, 1).reshape(128, 96)),
        adab=np.ascontiguousarray(ada_b),
        awout=np.ascontiguousarray(np.asarray(inputs["a_w_out"], np.float32)),
        bwout=np.ascontiguousarray(np.asarray(inputs["b_w_out"], np.float32)),
        cb=cbv, cf=cfv,
    )
    acols = a_piece_cols()
    a_w_in = np.asarray(inputs["a_w_in"], np.float32)
    sh["awin"] = np.stack([pieces_layout(a_w_in[i], acols) for i in range(2)])
    bcols = b_piece_cols()
    b_w_in = np.asarray(inputs["b_w_in"], np.float32)
    sh["bwin"] = np.stack([pieces_layout(b_w_in[i], bcols) for i in range(2)])
    return sh


def core_inputs(inputs, b, shared):
    m = dict(shared)
    m["x"] = np.ascontiguousarray(np.asarray(inputs["x"][b], np.float32))
    m["cT"] = np.ascontiguousarray(np.asarray(inputs["c"][b], np.float32).reshape(8, 128).T)
    m["pos"] = np.ascontiguousarray(np.asarray(inputs["positions"][b], np.int32).reshape(1, S))
    return m


_NC_CACHE = {}


def kernel(x, c, positions, norm_g, ada_w, ada_b, a_w_in, a_w_out, b_w_in, b_w_out, final_g):
    inputs = dict(x=x, c=c, positions=positions, norm_g=norm_g, ada_w=ada_w, ada_b=ada_b, a_w_in=a_w_in,
                  a_w_out=a_w_out, b_w_in=b_w_in, b_w_out=b_w_out, final_g=final_g)
    shared = prep_shared(inputs)
    nc = Prog().build()
    in_maps = [core_inputs(inputs, b, shared) for b in range(8)]
    res = run_bass_kernel_spmd(nc, in_maps, core_ids=list(range(8)))
    return np.stack([np.asarray(r["out"], np.float32) for r in res.results], axis=0)
```

```python
import numpy as np
import ml_dtypes
import concourse.bass as bass
import concourse.mybir as mybir
from concourse.bass_utils import run_bass_kernel_spmd

F32 = mybir.dt.float32
BF16 = mybir.dt.bfloat16
I32 = mybir.dt.int32
ALU = mybir.AluOpType
AF = mybir.ActivationFunctionType

S = 4096
D = 1024
NT = S // 128
EPS = 1e-6
A_GROUPS = ((128, 1), (512, 4), (2048, 16))
TOPK = 256


class Sched:
    ENGS = ("tensor", "scalar", "vector", "gpsimd", "sync")

    def __init__(self):
        self.ops = []

    def add(self, eng, fn, r=(), w=(), dma=0):
        self.ops.append(dict(eng=eng, fn=fn, r=list(r), w=list(w), dma=dma))

    def pe(self, fn, r=(), w=()):
        self.add("tensor", fn, r, w)

    def act(self, fn, r=(), w=()):
        self.add("scalar", fn, r, w)

    def dve(self, fn, r=(), w=()):
        self.add("vector", fn, r, w)

    def pool(self, fn, r=(), w=()):
        import os
        self.add(os.environ.get("KPOOL", "gpsimd"), fn, r, w)

    def dma(self, fn, r=(), w=(), n=1, q="sync"):
        self.add(q, fn, r, w, dma=n)

    def finalize(self, nc, n_dma_sems=16):
        ops = self.ops
        state = {}

        def touch(key):
            name, sub = key
            d = state.setdefault(name, {})
            if sub == "*":
                d.setdefault("*", [None, []])
                return list(d.keys())
            d.setdefault(sub, [None, []])
            return [sub, "*"] if "*" in d else [sub]

        for i, op in enumerate(ops):
            deps = set()
            for key in op["r"]:
                d = state.setdefault(key[0], {})
                for sub in touch(key):
                    wv = d[sub][0]
                    if wv is not None:
                        deps.add(wv)
            for key in op["w"]:
                d = state.setdefault(key[0], {})
                for sub in touch(key):
                    wv, rd = d[sub]
                    if wv is not None:
                        deps.add(wv)
                    deps.update(rd)
            for key in op["r"]:
                d = state[key[0]]
                subs = [key[1]] if key[1] != "*" else list(d.keys())
                for sub in subs:
                    rl = d[sub][1]
                    if not op["dma"]:
                        rl[:] = [j for j in rl if ops[j]["dma"] or ops[j]["eng"] != op["eng"]]
                    rl.append(i)
            for key in op["w"]:
                d = state[key[0]]
                subs = [key[1]] if key[1] != "*" else list(d.keys())
                for sub in subs:
                    d[sub][0] = i
                    d[sub][1] = []
            deps.discard(i)
            op["deps"] = deps

        pos = {}
        cnt = {e: 0 for e in self.ENGS}
        for i, op in enumerate(ops):
            pos[i] = cnt[op["eng"]]
            cnt[op["eng"]] += 1

        for i, op in enumerate(ops):
            keep = set()
            for dix in op["deps"]:
                p = ops[dix]
                if p["eng"] == op["eng"] and not p["dma"]:
                    if op["eng"] == "tensor" and not op["dma"]:
                        continue
                keep.add(dix)
            op["deps"] = keep

        dma_ops = [i for i, op in enumerate(ops) if op["dma"]]
        sem_tot = [0] * n_dma_sems
        last_on_sem = [None] * n_dma_sems
        qcount = {"sync": 0, "scalar": 0, "gpsimd": 0}
        qrange = {"sync": (0, n_dma_sems - 6), "scalar": (n_dma_sems - 6, n_dma_sems), "gpsimd": (n_dma_sems - 6, n_dma_sems)}
        for i in dma_ops:
            op = ops[i]
            lo_, hi_ = qrange[op["eng"]]
            s = lo_ + qcount[op["eng"]] % (hi_ - lo_)
            qcount[op["eng"]] += 1
            if last_on_sem[s] is not None:
                op["deps"].add(last_on_sem[s])
            sem_tot[s] += 16 * op["dma"]
            op["sig"] = ("dma", s, sem_tot[s])
            last_on_sem[s] = i

        needed = set()
        for op in ops:
            needed.update(op["deps"])
        sig_cnt = {e: 0 for e in self.ENGS}
        for i, op in enumerate(ops):
            if op["dma"]:
                continue
            if i in needed:
                sig_cnt[op["eng"]] += 1
                op["sig"] = ("eng", op["eng"], sig_cnt[op["eng"]])
            else:
                op["sig"] = None

        self.by_eng = {e: [op for op in ops if op["eng"] == e] for e in self.ENGS}
        self.final_dma = [(s, sem_tot[s]) for s in range(n_dma_sems) if sem_tot[s] > 0]
        self.n_dma_sems = n_dma_sems

    def emit(self, nc, block, sems, dma_sems):
        ops = self.ops

        def run_engine(e, name):
            known = {}
            for op in self.by_eng[name]:
                waits = {}
                for dix in op["deps"]:
                    sg = ops[dix]["sig"]
                    if sg[0] == "dma":
                        key = ("dma", sg[1])
                    else:
                        key = ("eng", sg[1])
                    waits[key] = max(waits.get(key, 0), sg[2])
                for key, val in waits.items():
                    if known.get(key, 0) >= val:
                        continue
                    known[key] = val
                    sem = dma_sems[key[1]] if key[0] == "dma" else sems[key[1]]
                    e.wait_ge(sem, val)
                inst = op["fn"](e)
                sg = op["sig"]
                if sg is not None:
                    if sg[0] == "dma":
                        insts = inst if isinstance(inst, (list, tuple)) else [inst]
                        assert len(insts) == op["dma"], (len(insts), op["dma"])
                        for ins in insts:
                            ins.then_inc(dma_sems[sg[1]], 16)
                    else:
                        inst.then_inc(sems[name], 1)
            if name in ("sync", "scalar"):
                for s, tot in self.final_dma:
                    if known.get(("dma", s), 0) < tot:
                        e.wait_ge(dma_sems[s], tot)

        @block.tensor
        def _(e):
            run_engine(e, "tensor")

        @block.scalar
        def _(e):
            run_engine(e, "scalar")

        @block.vector
        def _(e):
            run_engine(e, "vector")

        @block.gpsimd
        def _(e):
            run_engine(e, "gpsimd")

        @block.sync
        def _(e):
            run_engine(e, "sync")


NEG_BIG = -1.0e30
IDX_SCALE = float((8 ** -0.5) * (64 ** -0.5))
TWO_PI = float(2 * np.pi)
CW1 = 6.28125
CW2 = float(2 * np.pi - 6.28125)
NIT = 22
BIS_LO = -64.0
BIS_STEP0 = 64.0

CB_ID = 0
CB_SWAP = 128
CB_BAND = 256
CB_ONES = 512
CB_BAND2 = 640
CB_N = 1152
CF_INVF = 0
CF_SIGN = 1
CF_CAUS = 2
CF_ONES = 130
CF_N = 258


def make_consts():
    cb = np.zeros((128, CB_N), np.float32)
    cb[:, CB_ID:CB_ID + 128] = np.eye(128, dtype=np.float32)
    for m in range(128):
        k = m + 32 if (m % 64) < 32 else m - 32
        cb[k, CB_SWAP + m] = 1.0
    kj = np.arange(128)[:, None]
    c = np.arange(128)[None, :]
    cb[:, CB_BAND:CB_BAND + 128] = (kj <= c)
    cb[:, CB_BAND + 128:CB_BAND + 256] = (kj >= c)
    cb[:, CB_ONES:CB_ONES + 128] = 1.0
    cb[:, CB_BAND2:CB_BAND2 + 256] = cb[:, CB_BAND:CB_BAND + 256]
    cb[:, CB_BAND2 + 256:CB_BAND2 + 512] = cb[:, CB_BAND:CB_BAND + 256]
    cf = np.zeros((128, CF_N), np.float32)
    half = 32
    inv_freq = (10000.0 ** (-np.arange(half, dtype=np.float32) / half)).astype(np.float32)
    p = np.arange(128)
    cf[:, CF_INVF] = inv_freq[p % 32]
    cf[:, CF_SIGN] = np.where((p % 64) < 32, -1.0, 1.0)
    t = np.arange(128)[:, None]
    s = np.arange(128)[None, :]
    cf[:, CF_CAUS:CF_CAUS + 128] = np.where(s <= t, 0.0, NEG_BIG)
    cf[:, CF_ONES:CF_ONES + 128] = 1.0
    return cb.astype(ml_dtypes.bfloat16), cf


def a_piece_cols():
    out = []
    for hp in range(8):
        pcs = []
        heads = (2 * hp, 2 * hp + 1)
        for g in range(3):
            for t in range(3):
                pcs.append([g * 3072 + t * 1024 + h * 64 + d for h in heads for d in range(64)])
        pcs.append([9216 + h * 64 + d for h in heads for d in range(64)])
        out.append(pcs)
    return np.array(out, np.int64)


B_OFF = dict(q=0, k=1024, v=1280, qi=1536, ki=2048, wi=2112, gate=2120)
BP_Q = 0
BP_K2 = 8
BP_V = 12
BP_QI = 14
BP_WREP = 18
BP_KI2 = 22
BP_GATE = 23
BP_WI = 31
BP_N = 32


def b_piece_cols():
    pcs = []
    for p in range(8):
        pcs.append([B_OFF["q"] + p * 128 + j for j in range(128)])
    for g in range(4):
        pcs.append([B_OFF["k"] + g * 64 + d for _ in range(2) for d in range(64)])
    for p in range(2):
        pcs.append([B_OFF["v"] + p * 128 + j for j in range(128)])
    for p in range(4):
        pcs.append([B_OFF["qi"] + p * 128 + j for j in range(128)])
    for p in range(4):
        pcs.append([B_OFF["wi"] + 2 * p + e for e in range(2) for _ in range(64)])
    pcs.append([B_OFF["ki"] + d for _ in range(2) for d in range(64)])
    for p in range(8):
        pcs.append([B_OFF["gate"] + p * 128 + j for j in range(128)])
    pcs.append([B_OFF["wi"] + (j % 8) for j in range(128)])
    return np.array(pcs, np.int64)


def pieces_layout(w, cols):
    g = w[:, cols.reshape(-1)]
    npc = cols.size // 128
    g = g.reshape(8, 128, npc, 128)
    g = np.ascontiguousarray(g.transpose(2, 1, 0, 3))
    return g.reshape(cols.shape[:-1] + (128, 8, 128))


from contextlib import ExitStack


def pk(bank, sub="*"):
    return ("ps%d" % bank, sub)


def f32v(ap):
    return ap.bitcast(F32)


class Prog:
    def __init__(self, layers=(0, 1, 2, 3), final_norm=True, debug_out=None):
        self.layers = tuple(layers)
        self.final_norm = final_norm
        self.debug_out = debug_out
        self.nc = bass.Bass("TRN2", target_bir_lowering=False)
        self.sc = Sched()
        self.rr = {}

    def ring(self, name, n):
        i = self.rr.get(name, 0)
        self.rr[name] = i + 1
        return i % n

    def build(self):
        nc = self.nc
        sc = self.sc
        dt = nc.dram_tensor
        self.x = dt("x", [S, D], F32, kind="ExternalInput").ap()
        self.cT = dt("cT", [128, 8], F32, kind="ExternalInput").ap()
        self.pos = dt("pos", [1, S], I32, kind="ExternalInput").ap()
        self.ngT = dt("ngT", [128, 32], F32, kind="ExternalInput").ap()
        self.fg = dt("fg", [1, D], F32, kind="ExternalInput").ap()
        self.ada_w = dt("ada_w", [4, D, 3 * D], F32, kind="ExternalInput").ap()
        self.adabT = dt("adabT", [128, 96], F32, kind="ExternalInput").ap()
        self.adab = dt("adab", [4, 3 * D], F32, kind="ExternalInput").ap()
        self.awin = dt("awin", [2, 8, 10, 128, 8, 128], F32, kind="ExternalInput").ap()
        self.awout = dt("awout", [2, D, D], F32, kind="ExternalInput").ap()
        self.bwin = dt("bwin", [2, BP_N, 128, 8, 128], F32, kind="ExternalInput").ap()
        self.bwout = dt("bwout", [2, D, D], F32, kind="ExternalInput").ap()
        self.cbd = dt("cb", [128, CB_N], BF16, kind="ExternalInput").ap()
        self.cfd = dt("cf", [128, CF_N], F32, kind="ExternalInput").ap()
        self.out = dt("out", [S, D], F32, kind="ExternalOutput").ap()
        self.yTd = dt("yT_scr", [D, S], BF16, kind="Internal").ap()
        self.gbd = dt("gb_scr", [4, 128, D], F32, kind="Internal").ap()
        self.QTd = dt("QT_scr", [D, S], BF16, kind="Internal").ap()
        self.QiTd = dt("QiT_scr", [512, S], BF16, kind="Internal").ap()
        self.GTd = dt("GT_scr", [D, S], BF16, kind="Internal").ap()

        with ExitStack() as es:
            def sb(name, shape, dtype):
                return es.enter_context(nc.sbuf_tensor(name, shape, dtype))

            self.UT = sb("UT", [128, 32768], BF16)
            self.TAB = sb("TAB", [128, 8192], BF16)
            self.RX = sb("RX", [128, 16384], BF16)
            self.RY = sb("RY", [128, 8704], BF16)
            self.RZ = sb("RZ", [128, 6656], BF16)
            self.RW = sb("RW", [128, 12288], BF16)
            self.RT = sb("RT", [128, 12288], BF16)
            self.cb = sb("cbs", [128, CB_N], BF16)
            self.cf = sb("cfs", [128, CF_N], F32)
            self.sm = sb("small", [128, 512], F32)
            self.wtok = sb("wtok", [128, 3 * 256], F32)
            self.dummy = sb("dummyt", [128, 8], F32)
            self.ps = [es.enter_context(nc.psum_tensor(f"ps{i}", [128, 512], F32)) for i in range(8)]
            self.sems = {e: es.enter_context(nc.semaphore(f"s_{e}")) for e in Sched.ENGS}
            NDS = 18
            self.dsems = [es.enter_context(nc.semaphore(f"d_{i}")) for i in range(NDS)]
            block = es.enter_context(nc.Block())

            self.define()
            import os
            if os.environ.get("KTRUNC"):
                sc.ops = sc.ops[:int(os.environ["KTRUNC"])]
            sc.finalize(nc, n_dma_sems=NDS)
            sc.emit(nc, block, self.sems, self.dsems)
        return nc

    def smv(self, a, b):
        return self.sm[:, a:b]

    def fence(self, regions):
        d = self.dummy
        self.sc.dve(lambda e: e.memset(d[:, 0:1], 0.0), w=[(r, "*") for r in regions] + [("dummy", 0)])

    def define(self):
        sc = self.sc
        cb, cf = self.cb, self.cf
        sc.dma(lambda e: e.dma_start(out=cb[:, :], in_=self.cbd[:, :]), w=[("cb", 0)])
        sc.dma(lambda e: e.dma_start(out=cf[:, :], in_=self.cfd[:, :]), w=[("cf", 0)])
        self.p0_tables()
        self.p0_mods()
        src = self.x
        for l in self.layers:
            self.p1_norm(l, src)
            if l % 2 == 0:
                self.p2_a(l)
            else:
                self.p2_b(l)
            self.p3_out(l, src)
            src = self.out
        if self.final_norm:
            self.p4_final(src)

    def p0_tables(self):
        sc = self.sc
        UT, cf = self.UT, self.cf
        posi = UT[:, 0:8192].bitcast(I32)
        ang = f32v(UT[:, 8192:16384])
        tk = f32v(UT[:, 16384:24576])
        rr_ = f32v(UT[:, 24576:32768])
        tki = UT[:, 16384:24576].bitcast(I32)
        C = self.TAB[:, 0:4096]
        Sg = self.TAB[:, 4096:8192]
        K = [("UT", "p0")]
        sc.dma(lambda e: e.dma_start(out=posi, in_=self.pos.partition_broadcast(128)), w=K)
        sc.dve(lambda e: e.tensor_copy(out=rr_, in_=posi), r=K, w=K)
        sc.dve(lambda e: e.tensor_scalar(out=ang, in0=rr_, scalar1=cf[:, CF_INVF:CF_INVF + 1], scalar2=None,
                                         op0=ALU.mult), r=K + [("cf", 0)], w=K)

        def reduce_and_sin(shift, dst, mul_sign):
            sc.dve(lambda e: e.tensor_scalar(out=tk, in0=ang, scalar1=shift, scalar2=1.0 / TWO_PI,
                                             op0=ALU.add, op1=ALU.mult), r=K, w=K)
            sc.dve(lambda e: e.tensor_copy(out=tki, in_=tk), r=K, w=K)
            sc.dve(lambda e: e.tensor_copy(out=tk, in_=tki), r=K, w=K)
            sc.dve(lambda e: e.tensor_scalar(out=rr_, in0=ang, scalar1=shift, scalar2=None, op0=ALU.add), r=K, w=K)
            sc.dve(lambda e: e.scalar_tensor_tensor(out=rr_, in0=tk, scalar=-CW1, in1=rr_, op0=ALU.mult, op1=ALU.add),
                   r=K, w=K)
            sc.dve(lambda e: e.scalar_tensor_tensor(out=rr_, in0=tk, scalar=-CW2, in1=rr_, op0=ALU.mult, op1=ALU.add),
                   r=K, w=K)
            sc.dve(lambda e: e.tensor_scalar(out=tk, in0=rr_, scalar1=float(np.pi), scalar2=-TWO_PI,
                                             op0=ALU.is_gt, op1=ALU.mult), r=K, w=K)
            sc.dve(lambda e: e.tensor_tensor(out=rr_, in0=rr_, in1=tk, op=ALU.add), r=K, w=K)
            sc.dve(lambda e: e.tensor_scalar(out=tk, in0=rr_, scalar1=float(-np.pi), scalar2=TWO_PI,
                                             op0=ALU.is_lt, op1=ALU.mult), r=K, w=K)
            sc.dve(lambda e: e.tensor_tensor(out=rr_, in0=rr_, in1=tk, op=ALU.add), r=K, w=K)
            sc.dve(lambda e: e.tensor_scalar(out=rr_, in0=rr_, scalar1=3.1415925, scalar2=-3.1415925,
                                             op0=ALU.min, op1=ALU.max), r=K, w=K)
            if mul_sign:
                sc.act(lambda e: e.activation(out=tk, in_=rr_, func=AF.Sin), r=K, w=K)
                sc.dve(lambda e: e.tensor_scalar(out=dst, in0=tk, scalar1=cf[:, CF_SIGN:CF_SIGN + 1], scalar2=None,
                                                 op0=ALU.mult), r=K, w=[("TAB", 0)])
            else:
                sc.act(lambda e: e.activation(out=dst, in_=rr_, func=AF.Sin), r=K, w=[("TAB", 0)])

        reduce_and_sin(0.0, Sg, True)
        reduce_and_sin(float(np.pi / 2), C, False)
        self.fence(["UT"])

    def p0_mods(self):
        sc = self.sc
        UT, RX, cf, cb, sm = self.UT, self.RX, self.cf, self.cb, self.sm
        c_sb = sm[:, 392:400]
        cact = sm[:, 384:392]
        modT = sm[:, 192:288]
        adabT = sm[:, 288:384]
        ng = sm[:, 160:192]
        gs = sm[:, 96:128]
        sh = sm[:, 128:160]
        stage = [f32v(UT[:, 0:8192]).rearrange("p (k n) -> p k n", k=8),
                 f32v(UT[:, 8192:16384]).rearrange("p (k n) -> p k n", k=8)]
        cbc = f32v(UT[:, 16384:18432]).rearrange("p (k n) -> p k n", k=8)
        gbt = f32v(UT[:, 18432:20480])
        abb = f32v(UT[:, 20480:22528])
        cbh = UT[:, 22528:23552].rearrange("p (k n) -> p k n", k=8)
        cbl = UT[:, 23552:24576].rearrange("p (k n) -> p k n", k=8)
        tmp = f32v(UT[:, 24576:25600]).rearrange("p (j n) -> p j n", j=4)
        whi = [RX[:, 0:4096].rearrange("p (k n) -> p k n", k=8), RX[:, 4096:8192].rearrange("p (k n) -> p k n", k=8)]
        wlo = [RX[:, 8192:12288].rearrange("p (k n) -> p k n", k=8),
               RX[:, 12288:16384].rearrange("p (k n) -> p k n", k=8)]
        ident = cb[:, CB_ID:CB_ID + 128]
        sc.dma(lambda e: e.dma_start(out=c_sb, in_=self.cT[:, :]), w=[("sm", "c")])
        sc.dma(lambda e: e.dma_start(out=adabT, in_=self.adabT[:, :]), w=[("sm", "adabT")])
        sc.dma(lambda e: e.dma_start(out=ng, in_=self.ngT[:, :]), w=[("sm", "ng")])
        sc.act(lambda e: e.activation(out=cact, in_=c_sb, func=AF.Silu), r=[("sm", "c")], w=[("sm", "cact")])
        for kc in range(8):
            sc.dve(lambda e, kc=kc: e.tensor_scalar(out=cbc[:, kc, :], in0=cf[:, CF_ONES:CF_ONES + 128],
                                                    scalar1=cact[:, kc:kc + 1], scalar2=None, op0=ALU.mult),
                   r=[("sm", "cact"), ("cf", 0)], w=[("UT", "cbc")])
        sc.dve(lambda e: e.tensor_copy(out=cbh, in_=cbc), r=[("UT", "cbc")], w=[("UT", "cbh")])
        sc.dve(lambda e: e.tensor_tensor(out=cbl, in0=cbc, in1=cbh, op=ALU.subtract),
               r=[("UT", "cbc"), ("UT", "cbh")], w=[("UT", "cbl")])
        for l in range(4):
            if l not in self.layers:
                continue
            sc.dma(lambda e, l=l: e.dma_start(out=abb, in_=self.adab[l:l + 1, 2048:3072].partition_broadcast(128)),
                   w=[("UT", "abb")])
            for cc in range(6):
                si = self.ring("adast", 2)
                st = stage[si]
                src_ = self.ada_w[l].rearrange("(k p) n -> p k n", p=128)[:, :, cc * 512:(cc + 1) * 512]
                sc.dma(lambda e, st=st, src_=src_: e.dma_start(out=st, in_=src_), w=[("UT", ("adast", si))])
                wh, wl = whi[si], wlo[si]
                sc.act(lambda e, st=st, wh=wh: e.activation(out=wh, in_=st, func=AF.Copy),
                       r=[("UT", ("adast", si))], w=[("RX", ("whi", si))])
                sc.dve(lambda e, st=st, wh=wh, wl=wl: e.tensor_tensor(out=wl, in0=st, in1=wh, op=ALU.subtract),
                       r=[("UT", ("adast", si)), ("RX", ("whi", si))], w=[("RX", ("wlo", si))])
                pi = self.ring("p0ps", 2)
                pp = self.ps[pi]
                combos = [(cbh, wh, "cbh", "whi"), (cbh, wl, "cbh", "wlo"), (cbl, wh, "cbl", "whi")]
                n = 0
                for (la_, ra_, lk, rk) in combos:
                    for kc in range(8):
                        sc.pe(lambda e, la_=la_, ra_=ra_, kc=kc, pp=pp, n=n: e.matmul(
                            pp[:, :], lhsT=la_[:, kc, :], rhs=ra_[:, kc, :], start=(n == 0), stop=(n == 23)),
                            r=[("UT", lk), ("RX", (rk, si))], w=[pk(pi)])
                        n += 1
                sc.dve(lambda e, pp=pp: e.tensor_tensor(
                    out=tmp, in0=pp[:, :].rearrange("p (j n) -> p j n", j=4),
                    in1=ident.unsqueeze(1).to_broadcast([128, 4, 128]), op=ALU.mult),
                    r=[pk(pi), ("cb", 0)], w=[("UT", "tmp")])
                c0 = l * 24 + cc * 4
                sc.dve(lambda e, c0=c0: e.tensor_reduce(out=modT[:, c0:c0 + 4], in_=tmp, axis=mybir.AxisListType.X,
                                                        op=ALU.add),
                       r=[("UT", "tmp")], w=[("sm", ("modTraw", l, cc))])
                if cc >= 4:
                    half = cc - 4
                    sc.dve(lambda e, half=half, pp=pp: e.tensor_tensor(
                        out=gbt[:, half * 512:(half + 1) * 512], in0=pp[:, :],
                        in1=abb[:, half * 512:(half + 1) * 512], op=ALU.add),
                        r=[pk(pi), ("UT", "abb")], w=[("UT", ("gbt", half))])
            sc.dma(lambda e, l=l: e.dma_start(out=self.gbd[l], in_=gbt), r=[("UT", ("gbt", 0)), ("UT", ("gbt", 1))],
                   w=[("gbd", l)], q="scalar")
        for l in self.layers:
            sc.dve(lambda e, l=l: e.tensor_tensor(out=modT[:, l * 24:(l + 1) * 24], in0=modT[:, l * 24:(l + 1) * 24],
                                                  in1=adabT[:, l * 24:(l + 1) * 24], op=ALU.add),
                   r=[("sm", ("modTraw", l, cc)) for cc in range(6)] + [("sm", "adabT")], w=[("sm", "modT")])
        for l in self.layers:
            sc.dve(lambda e, l=l: e.scalar_tensor_tensor(out=gs[:, l * 8:(l + 1) * 8], in0=modT[:, l * 24 + 8:l * 24 + 16],
                                                         scalar=1.0, in1=ng[:, l * 8:(l + 1) * 8], op0=ALU.add,
                                                         op1=ALU.mult),
                   r=[("sm", "modT"), ("sm", "ng")], w=[("sm", "gs")])
            sc.dve(lambda e, l=l: e.tensor_copy(out=sh[:, l * 8:(l + 1) * 8], in_=modT[:, l * 24:l * 24 + 8]),
                   r=[("sm", "modT")], w=[("sm", "sh")])
        self.fence(["UT", "RX"])

    def p1_norm(self, l, src):
        sc = self.sc
        RW, sm, cb = self.RW, self.sm, self.cb
        UT3 = self.UT[:, :].rearrange("p (k t) -> p k t", k=8)
        ss, rstd, ms = sm[:, 0:32], sm[:, 32:64], sm[:, 64:96]
        gs, sh = sm[:, 96:128], sm[:, 128:160]
        ht = [f32v(RW[:, 0:2048]), f32v(RW[:, 2048:4096])]
        xh = [RW[:, 4096:5120], RW[:, 5120:6144]]
        ident = cb[:, CB_ID:CB_ID + 128]
        self.fence(["RW"])
        for tt in range(NT):
            bi = self.ring("p1ht", 2)
            h_, x_ = ht[bi], xh[bi]
            hk = [("hd", tt)] if src is self.out else []
            sc.dma(lambda e, h_=h_, tt=tt: e.dma_start(out=h_, in_=src[tt * 128:(tt + 1) * 128, :]),
                   r=hk, w=[("RW", ("ht", bi))])
            sc.act(lambda e, h_=h_, x_=x_, tt=tt: e.activation(out=x_, in_=h_, func=AF.Square,
                                                               accum_out=ss[:, tt:tt + 1]),
                   r=[("RW", ("ht", bi))], w=[("RW", ("xh", bi)), ("sm", ("ss", tt))])
            sc.dve(lambda e, tt=tt: e.tensor_scalar(out=ms[:, tt:tt + 1], in0=ss[:, tt:tt + 1], scalar1=1.0 / D,
                                                    scalar2=EPS, op0=ALU.mult, op1=ALU.add),
                   r=[("sm", ("ss", tt))], w=[("sm", ("ms", tt))])
            sc.act(lambda e, tt=tt: e.activation(out=ms[:, tt:tt + 1], in_=ms[:, tt:tt + 1], func=AF.Sqrt),
                   r=[("sm", ("ms", tt))], w=[("sm", ("ms", tt))])
            sc.dve(lambda e, tt=tt: e.reciprocal(out=rstd[:, tt:tt + 1], in_=ms[:, tt:tt + 1]),
                   r=[("sm", ("ms", tt))], w=[("sm", ("rstd", tt))])
            sc.dve(lambda e, h_=h_, x_=x_, tt=tt: e.tensor_scalar(out=x_, in0=h_, scalar1=rstd[:, tt:tt + 1],
                                                                  scalar2=None, op0=ALU.mult),
                   r=[("RW", ("ht", bi)), ("sm", ("rstd", tt))], w=[("RW", ("xh", bi))])
            pstA = self.ps[6][:, :].bitcast(BF16).rearrange("p (k t) -> p k t", k=8)
            pstB = self.ps[7][:, :].bitcast(BF16).rearrange("p (k t) -> p k t", k=8)
            for kc in range(8):
                pst, bank = (pstA, 6) if kc < 4 else (pstB, 7)
                sc.pe(lambda e, x_=x_, kc=kc, pst=pst: e.transpose(pst[:, kc % 4, :], x_[:, kc * 128:(kc + 1) * 128], ident),
                      r=[("RW", ("xh", bi)), ("cb", 0)], w=[pk(bank)])
            for k4 in range(4):
                for (pst, bank, kc) in ((pstA, 6, k4), (pstB, 7, 4 + k4)):
                    o = UT3[:, kc, tt * 128:(tt + 1) * 128]
                    col = l * 8 + kc
                    if bank == 6:
                        sc.act(lambda e, o=o, k4=k4, pst=pst, col=col: e.activation(
                            out=o, in_=pst[:, k4, :], func=AF.Identity, bias=sh[:, col:col + 1], scale=gs[:, col:col + 1]),
                            r=[pk(6), ("sm", "gs"), ("sm", "sh")], w=[("UT", (tt, kc))])
                    else:
                        sc.dve(lambda e, o=o, k4=k4, pst=pst, col=col: e.tensor_scalar(
                            out=o, in0=pst[:, k4, :], scalar1=gs[:, col:col + 1], scalar2=sh[:, col:col + 1],
                            op0=ALU.mult, op1=ALU.add),
                            r=[pk(7), ("sm", "gs"), ("sm", "sh")], w=[("UT", (tt, kc))])

    def ut_keys(self, tiles, kc):
        return [("UT", (t, kc)) for t in tiles]

    def p4_final(self, src):
        sc = self.sc
        self.fence(["UT", "RW"])
        UT, sm = self.UT, self.sm
        fgb = f32v(UT[:, 0:2048])
        ht = [f32v(UT[:, 2048:4096]), f32v(UT[:, 4096:6144])]
        ho = [f32v(UT[:, 6144:8192]), f32v(UT[:, 8192:10240])]
        junk = f32v(UT[:, 10240:12288])
        ss, rstd, ms = sm[:, 0:32], sm[:, 32:64], sm[:, 64:96]
        sc.dma(lambda e: e.dma_start(out=fgb, in_=self.fg.partition_broadcast(128)), w=[("UT", "fgb")])
        for tt in range(NT):
            bi = self.ring("p4", 2)
            h_, o_ = ht[bi], ho[bi]
            hk = [("hd", tt)] if src is self.out else []
            sc.dma(lambda e, h_=h_, tt=tt: e.dma_start(out=h_, in_=src[tt * 128:(tt + 1) * 128, :]),
                   r=hk, w=[("UT", ("ht", bi))])
            sc.act(lambda e, h_=h_, tt=tt: e.activation(out=junk, in_=h_, func=AF.Square, accum_out=ss[:, tt:tt + 1]),
                   r=[("UT", ("ht", bi))], w=[("UT", "junk"), ("sm", ("ss", tt))])
            sc.dve(lambda e, tt=tt: e.tensor_scalar(out=ms[:, tt:tt + 1], in0=ss[:, tt:tt + 1], scalar1=1.0 / D,
                                                    scalar2=EPS, op0=ALU.mult, op1=ALU.add),
                   r=[("sm", ("ss", tt))], w=[("sm", ("ms", tt))])
            sc.act(lambda e, tt=tt: e.activation(out=ms[:, tt:tt + 1], in_=ms[:, tt:tt + 1], func=AF.Sqrt),
                   r=[("sm", ("ms", tt))], w=[("sm", ("ms", tt))])
            sc.dve(lambda e, tt=tt: e.reciprocal(out=rstd[:, tt:tt + 1], in_=ms[:, tt:tt + 1]),
                   r=[("sm", ("ms", tt))], w=[("sm", ("rstd", tt))])
            sc.dve(lambda e, h_=h_, o_=o_, tt=tt: e.scalar_tensor_tensor(
                out=o_, in0=h_, scalar=rstd[:, tt:tt + 1], in1=fgb, op0=ALU.mult, op1=ALU.mult),
                r=[("UT", ("ht", bi)), ("sm", ("rstd", tt)), ("UT", "fgb")], w=[("UT", ("ho", bi))])
            sc.dma(lambda e, o_=o_, tt=tt: e.dma_start(out=self.out[tt * 128:(tt + 1) * 128, :], in_=o_),
                   r=[("UT", ("ho", bi))], w=[("hd", tt)], q="scalar")

    def dump(self, name, ap, keys):
        shape = list(ap.shape)
        d = self.nc.dram_tensor("dbg_" + name, shape, ap.dtype, kind="ExternalOutput").ap()
        self.sc.dma(lambda e: e.dma_start(out=d, in_=ap), r=keys, w=[("dbg", name)], q="scalar")

    def p3_out(self, l, src):
        sc = self.sc
        self.fence(["UT"])
        UT = self.UT
        wd = (self.awout if l % 2 == 0 else self.bwout)[l // 2].rearrange("(k p) n -> p k n", p=128)
        wst = [f32v(UT[:, 0:4096]).rearrange("p (k n) -> p k n", k=2)]
        wob = UT[:, 4096:12288].rearrange("p (k n) -> p k n", k=8)
        gb = f32v(UT[:, 12288:14336])
        ych = [UT[:, 14336:18432].rearrange("p (k t) -> p k t", k=8),
               UT[:, 18432:22528].rearrange("p (k t) -> p k t", k=8)]
        ht = [f32v(UT[:, 22528:24576]), f32v(UT[:, 24576:26624])]
        hn = [f32v(UT[:, 26624:28672]), f32v(UT[:, 28672:30720])]
        sc.dma(lambda e: e.dma_start(out=gb, in_=self.gbd[l]), r=[("gbd", l)], w=[("UT", "gb")])
        for q4 in range(4):
            sc.dma(lambda e, q4=q4: e.dma_start(out=wst[0], in_=wd[:, 2 * q4:2 * q4 + 2, :]), w=[("UT", "wst")])
            for k2 in range(2):
                kc = 2 * q4 + k2
                sc.dve(lambda e, kc=kc, k2=k2: e.tensor_tensor(out=wob[:, kc, :], in0=wst[0][:, k2, :], in1=gb, op=ALU.mult),
                       r=[("UT", "wst"), ("UT", "gb")], w=[("UT", ("wob", kc))])
        for tc in range(8):
            yi = self.ring("p3y", 2)
            yc = ych[yi]
            srcy = self.yTd.rearrange("(k p) t -> p k t", p=128)[:, :, tc * 512:(tc + 1) * 512]
            sc.dma(lambda e, yc=yc, srcy=srcy: e.dma_start(out=yc, in_=srcy),
                   r=[("yT", (h, tc)) for h in range(16)], w=[("UT", ("ych", yi))])
            for t4 in range(4):
                tt = tc * 4 + t4
                bi = self.ring("p3h", 2)
                h_, n_ = ht[bi], hn[bi]
                hk = [("hd", tt)] if src is self.out else []
                sc.dma(lambda e, h_=h_, tt=tt: e.dma_start(out=h_, in_=src[tt * 128:(tt + 1) * 128, :]),
                       r=hk, w=[("UT", ("ht", bi))])
                for half in range(2):
                    pi = 4 + self.ring("p3ps", 4)
                    pp = self.ps[pi]
                    for kc in range(8):
                        sc.pe(lambda e, yc=yc, kc=kc, t4=t4, half=half, pp=pp: e.matmul(
                            pp[:, :], lhsT=yc[:, kc, t4 * 128:(t4 + 1) * 128], rhs=wob[:, kc, half * 512:(half + 1) * 512],
                            start=(kc == 0), stop=(kc == 7)),
                            r=[("UT", ("ych", yi)), ("UT", ("wob", kc))], w=[pk(pi)])
                    sc.dve(lambda e, h_=h_, n_=n_, half=half, pp=pp: e.tensor_tensor(
                        out=n_[:, half * 512:(half + 1) * 512], in0=pp[:, :], in1=h_[:, half * 512:(half + 1) * 512],
                        op=ALU.add),
                        r=[pk(pi), ("UT", ("ht", bi))], w=[("UT", ("hn", bi, half))])
                sc.dma(lambda e, n_=n_, tt=tt: e.dma_start(out=self.out[tt * 128:(tt + 1) * 128, :], in_=n_),
                       r=[("UT", ("hn", bi, 0)), ("UT", ("hn", bi, 1))], w=[("hd", tt)], q="scalar")
        self.fence(["UT"])

    def load_piece(self, dram_ap):
        sc = self.sc
        RW = self.RW
        si = self.ring("wst", 3)
        bi = self.ring("wbf", 4)
        st = f32v(RW[:, si * 2048:(si + 1) * 2048]).rearrange("p (k n) -> p k n", k=8)
        wb = RW[:, 6144 + bi * 1024:6144 + (bi + 1) * 1024].rearrange("p (k n) -> p k n", k=8)
        sc.dma(lambda e: e.dma_start(out=st, in_=dram_ap), w=[("RW", ("wst", si))])
        sc.pool(lambda e: e.tensor_copy(out=wb, in_=st), r=[("RW", ("wst", si))], w=[("RW", ("wbf", bi))])
        return wb, ("RW", ("wbf", bi))

    def proj_rope(self, wb, wkey, sink, pre_chunk=None):
        sc = self.sc
        RT, cb = self.RT, self.cb
        UT3 = self.UT[:, :].rearrange("p (k t) -> p k t", k=8)
        C = self.TAB[:, 0:4096]
        Sg = self.TAB[:, 4096:8192]
        swap = cb[:, CB_SWAP:CB_SWAP + 128]
        qb = [RT[:, 0:512], RT[:, 512:1024]]
        t1 = [f32v(RT[:, 1024:2048]), f32v(RT[:, 2048:3072])]
        t2 = [f32v(RT[:, 3072:4096]), f32v(RT[:, 4096:5120])]
        pend = None

        def do_swap(p):
            tc, bi, pa = p
            pb = 2 + self.ring("psB", 2)
            sc.pe(lambda e: e.matmul(self.ps[pb][:, :], lhsT=swap, rhs=qb[bi], start=True, stop=True),
                  r=[("RT", ("qb", bi)), ("cb", 0)], w=[pk(pb)])
            sl = slice(tc * 512, (tc + 1) * 512)
            sc.dve(lambda e: e.tensor_tensor(out=t1[bi], in0=self.ps[pb][:, :], in1=Sg[:, sl], op=ALU.mult),
                   r=[pk(pb), ("TAB", 0)], w=[("RT", ("t1", bi))])
            sc.pool(lambda e: e.tensor_tensor(out=t2[bi], in0=qb[bi], in1=C[:, sl], op=ALU.mult),
                    r=[("RT", ("qb", bi)), ("TAB", 0)], w=[("RT", ("t2", bi))])
            sink(tc, t1[bi], t2[bi], [("RT", ("t1", bi)), ("RT", ("t2", bi))])

        for tc in range(8):
            if pre_chunk is not None:
                pre_chunk(tc)
            pa = self.ring("psA", 2)
            bi = self.ring("qb", 2)
            for kc in range(8):
                sc.pe(lambda e, kc=kc, tc=tc, pa=pa: e.matmul(
                    self.ps[pa][:, :], lhsT=wb[:, kc, :], rhs=UT3[:, kc, tc * 512:(tc + 1) * 512],
                    start=(kc == 0), stop=(kc == 7)),
                    r=[wkey] + self.ut_keys(range(tc * 4, tc * 4 + 4), kc), w=[pk(pa)])
            sc.act(lambda e, pa=pa, bi=bi: e.activation(out=qb[bi], in_=self.ps[pa][:, :], func=AF.Copy),
                   r=[pk(pa)], w=[("RT", ("qb", bi))])
            if pend is not None:
                do_swap(pend)
            pend = (tc, bi, pa)
        do_swap(pend)

    def p2_a(self, l):
        sc = self.sc
        la = l // 2
        RX, RY, RZ, RT, cb, cf = self.RX, self.RY, self.RZ, self.RT, self.cb, self.cf
        UT3 = self.UT[:, :].rearrange("p (k t) -> p k t", k=8)
        acc = [f32v(RX[:, 0:8192]), f32v(RX[:, 8192:16384])]
        acc3 = f32v(RX[:, :]).rearrange("p (e t) -> p e t", e=2)
        QT, KT = RY[:, 0:4096], RY[:, 4096:8192]
        Vp = RZ[:, 0:4160].rearrange("p (t e c) -> p t e c", t=32, e=2)
        band = cb[:, CB_BAND:CB_BAND + 256]
        band2 = cb[:, CB_BAND2:CB_BAND2 + 512].rearrange("p (e q) -> p e q", e=2)
        self.fence(["RX", "RY", "RZ", "RW", "RT"])
        sc.pool(lambda e: e.memset(RZ[:, 0:4160], 1.0), w=[("RZ", "*")])
        PTs = [RT[:, 5120 + i * 512:5120 + (i + 1) * 512].rearrange("p (e q) -> p e q", e=2) for i in range(3)]
        th = f32v(RT[:, 6656:7680])
        sg = f32v(RT[:, 7680:8704])
        tq = f32v(RT[:, 8704:9728])
        rd = f32v(RT[:, 9728:10752])
        yb = [RT[:, 10752:11264], RT[:, 11264:11776]]

        def sink_to(dst, dkey, d, eng):
            def sink(tc, t1, t2, keys):
                dv = dst.rearrange("p (r i) -> p r i", r=d)[:, :, tc * 512 // d:(tc + 1) * 512 // d]
                a = t1.rearrange("p (i r) -> p r i", r=d)
                b = t2.rearrange("p (i r) -> p r i", r=d)
                sc.add(eng, lambda e: e.tensor_tensor(out=dv, in0=a, in1=b, op=ALU.add), r=keys, w=[dkey])
            return sink

        for hp in range(8):
            for g, (win, d) in enumerate(A_GROUPS):
                nb = 32 // d
                wb, wk = self.load_piece(self.awin[la, hp, 3 * g + 0])
                self.proj_rope(wb, wk, sink_to(QT, ("RY", "QT"), d, "vector"))
                wb, wk = self.load_piece(self.awin[la, hp, 3 * g + 1])
                self.proj_rope(wb, wk, sink_to(KT, ("RY", "KT"), d, "vector"))
                wb, wk = self.load_piece(self.awin[la, hp, 3 * g + 2])
                UTd = UT3.rearrange("p k (i r) -> p k r i", r=d)
                for J0 in range(0, 32, 4):
                    pv = 2 + self.ring("psB", 2)
                    for jj in range(4):
                        J = J0 + jj
                        r_, b_ = J // nb, J % nb
                        tiles = range(d * b_, d * b_ + d)
                        for kc in range(8):
                            sc.pe(lambda e, kc=kc, jj=jj, r_=r_, b_=b_, pv=pv, wb=wb, UTd=UTd: e.matmul(
                                self.ps[pv][:, jj * 128:(jj + 1) * 128], lhsT=UTd[:, kc, r_, b_ * 128:(b_ + 1) * 128],
                                rhs=wb[:, kc, :], start=(kc == 0), stop=(kc == 7)),
                                r=[wk] + self.ut_keys(tiles, kc), w=[pk(pv)])
                    sc.act(lambda e, pv=pv, J0=J0: e.activation(
                        out=Vp[:, J0:J0 + 4, :, 0:64],
                        in_=self.ps[pv][:, :].rearrange("p (j e c) -> p j e c", j=4, e=2), func=AF.Copy),
                        r=[pk(pv)], w=[("RZ", ("V", J0))])
                def blk(J, nb=nb):
                    first = (J % nb == 0)
                    has_off = (J + 1 < 32) and ((J + 1) % nb != 0)
                    return first, has_off, (256 if has_off else 128)

                def sbank(J, e_):
                    return (4 + e_) if J % 2 == 0 else e_

                def emit_S(J):
                    _, _, N = blk(J)
                    for e_ in range(2):
                        bS = sbank(J, e_)
                        sc.pe(lambda e, e_=e_, J=J, N=N, bS=bS: e.matmul(
                            self.ps[bS][:, 0:N], lhsT=KT[64 * e_:64 * e_ + 64, J * 128:(J + 1) * 128],
                            rhs=QT[64 * e_:64 * e_ + 64, J * 128:J * 128 + N], start=True, stop=True),
                            r=[("RY", "QT"), ("RY", "KT")], w=[pk(bS)])

                emit_S(0)
                for J in range(32):
                    first, has_off, N = blk(J)
                    if J + 1 < 32:
                        emit_S(J + 1)
                    pti = self.ring("PT", 3)
                    PT = PTs[pti]
                    for e_ in range(2):
                        bS = sbank(J, e_)
                        sc.act(lambda e, PT=PT, e_=e_, N=N, bS=bS: e.activation(out=PT[:, e_, 0:N], in_=self.ps[bS][:, 0:N],
                                                                              func=AF.Exp, scale=0.125),
                               r=[pk(bS)], w=[("RT", ("PT", pti))])
                    sc.dve(lambda e, PT=PT, N=N: e.tensor_tensor(
                        out=PT[:, :, 0:N], in0=PT[:, :, 0:N], in1=band2[:, :, 0:N], op=ALU.mult),
                        r=[("RT", ("PT", pti)), ("cb", 0)], w=[("RT", ("PT", pti))])
                    bk = 6 + (J % 2)
                    bk2 = 6 + ((J + 1) % 2)
                    for e_ in range(2):
                        sc.pe(lambda e, e_=e_, J=J, PT=PT, bk=bk, first=first: e.matmul(
                            self.ps[bk][0:65, e_ * 128:(e_ + 1) * 128], lhsT=Vp[:, J, e_, :], rhs=PT[:, e_, 0:128],
                            start=(first and e_ == 0), stop=True, skip_group_check=True),
                            r=[("RZ", ("V", (J // 4) * 4)), ("RT", ("PT", pti))], w=[pk(bk)])
                    if has_off:
                        for e_ in range(2):
                            sc.pe(lambda e, e_=e_, J=J, PT=PT, bk2=bk2: e.matmul(
                                self.ps[bk2][0:65, e_ * 128:(e_ + 1) * 128], lhsT=Vp[:, J, e_, :], rhs=PT[:, e_, 128:256],
                                start=(e_ == 0), stop=False, skip_group_check=True),
                                r=[("RZ", ("V", (J // 4) * 4)), ("RT", ("PT", pti))], w=[pk(bk2)])
                    r_, b_ = J // nb, J % nb
                    tiles = range(d * b_, d * b_ + d)
                    pO = self.ps[bk][0:65, 0:256].rearrange("p (e q) -> p e q", e=2)
                    av = acc3.rearrange("p e (i r) -> p e r i", r=d)[0:65, :, r_, b_ * 128:(b_ + 1) * 128]
                    akeys = [("RX", ("acc", e_, t)) for t in tiles for e_ in range(2)]
                    if g == 0:
                        sc.dve(lambda e, av=av, pO=pO: e.tensor_copy(out=av, in_=pO), r=[pk(bk)], w=akeys)
                    else:
                        sc.dve(lambda e, av=av, pO=pO: e.tensor_tensor(out=av, in0=pO, in1=av, op=ALU.add),
                               r=[pk(bk)] + akeys, w=akeys)
            wbg, wkg = self.load_piece(self.awin[la, hp, 9])
            units = []
            for e_ in range(2):
                for tc in range(8):
                    units.append(dict(h=2 * hp + e_, tc=tc, acc=acc[e_][:, tc * 512:(tc + 1) * 512],
                                      akeys=[("RX", ("acc", e_, t)) for t in range(tc * 4, tc * 4 + 4)],
                                      gate=("proj", wbg, wkg, e_)))
            self.finalize_units(units)

    def finalize_units(self, units):
        sc = self.sc
        RT = self.RT
        UT3 = self.UT[:, :].rearrange("p (k t) -> p k t", k=8)
        th = f32v(RT[:, 6656:7680])
        sg = f32v(RT[:, 7680:8704])
        tq = f32v(RT[:, 8704:9728])
        yb = [RT[:, 10752:11264], RT[:, 11264:11776]]
        rdh = [RT[64:65, 11776:12288], RT[64:65, 7680:8192]]
        rdl = [RT[64:65, 6656:7168], RT[64:65, 8704:9216]]
        onesb = self.cb[64:65, CB_ONES:CB_ONES + 64]
        st = {}

        rb = f32v(RT[:, 9728:10752])

        def stage1(i):
            u = units[i]
            ri = i % 2
            acc_ap, akeys = u["acc"], u["akeys"]
            sc.act(lambda e: e.activation(out=rdh[ri], in_=acc_ap[64:65, :], func=AF.Copy, scale=2.0),
                   r=akeys, w=[("RT", ("rdh", ri))])
            sc.dve(lambda e: e.scalar_tensor_tensor(out=rdl[ri], in0=acc_ap[64:65, :], scalar=2.0, in1=rdh[ri],
                                                    op0=ALU.mult, op1=ALU.subtract),
                   r=akeys + [("RT", ("rdh", ri))], w=[("RT", ("rdl", ri))])
            if u["gate"][0] == "dram":
                u["gate"][3]()
            if u["gate"][0] == "proj":
                _, wbg, wkg, e_ = u["gate"]
                tc = u["tc"]
                pg = 2 + ri
                for kc in range(8):
                    sc.pe(lambda e, kc=kc: e.matmul(
                        self.ps[pg][0:64, :], lhsT=wbg[:, kc, 64 * e_:64 * e_ + 64],
                        rhs=UT3[:, kc, tc * 512:(tc + 1) * 512], start=(kc == 0), stop=(kc == 7)),
                        r=[wkg] + self.ut_keys(range(tc * 4, tc * 4 + 4), kc), w=[pk(pg)])

        def stage2(i):
            u = units[i]
            ri = i % 2
            acc_ap, akeys = u["acc"], u["akeys"]
            pb = ri
            sc.pe(lambda e: e.matmul(self.ps[pb][0:64, :], lhsT=onesb, rhs=rdh[ri], start=True, stop=False),
                  r=[("RT", ("rdh", ri)), ("cb", 0)], w=[pk(pb)])
            sc.pe(lambda e: e.matmul(self.ps[pb][0:64, :], lhsT=onesb, rhs=rdl[ri], start=False, stop=True),
                  r=[("RT", ("rdl", ri)), ("cb", 0)], w=[pk(pb)])
            if u["gate"][0] == "proj":
                pg = 2 + ri
                sc.act(lambda e: e.activation(out=th[0:64, :], in_=self.ps[pg][0:64, :], func=AF.Tanh, scale=0.5),
                       r=[pk(pg)], w=[("RT", "th")])
                sc.dve(lambda e: e.scalar_tensor_tensor(out=sg[0:64, :], in0=th[0:64, :], scalar=1.0,
                                                        in1=self.ps[pg][0:64, :], op0=ALU.add, op1=ALU.mult),
                       r=[pk(pg), ("RT", "th")], w=[("RT", "sg")])
                gsrc, gkeys = sg[0:64, :], [("RT", "sg")]
            else:
                gsrc, gkeys = u["gate"][1], u["gate"][2]
            sc.dve(lambda e: e.reciprocal(out=rb[0:64, :], in_=self.ps[pb][0:64, :]), r=[pk(pb)], w=[("RT", "rb")])
            sc.dve(lambda e: e.tensor_tensor(out=tq[0:64, :], in0=acc_ap[0:64, :], in1=rb[0:64, :], op=ALU.mult),
                   r=akeys + [("RT", "rb")], w=[("RT", "tq")])
            yi = self.ring("yb", 2)
            y_ = yb[yi]
            sc.pool(lambda e: e.tensor_tensor(out=y_[0:64, :], in0=tq[0:64, :], in1=gsrc, op=ALU.mult),
                    r=[("RT", "tq")] + gkeys, w=[("RT", ("yb", yi))])
            h, tc = u["h"], u["tc"]
            sc.dma(lambda e: e.dma_start(out=self.yTd[h * 64:(h + 1) * 64, tc * 512:(tc + 1) * 512], in_=y_[0:64, :]),
                   r=[("RT", ("yb", yi))], w=[("yT", (h, tc))], q="scalar")

        stage1(0)
        for i in range(len(units)):
            if i + 1 < len(units):
                stage1(i + 1)
            stage2(i)

    def p2_b(self, l):
        sc = self.sc
        lb = l // 2
        UT, RX, RY, RZ, RW, RT, cb, cf, sm = self.UT, self.RX, self.RY, self.RZ, self.RW, self.RT, self.cb, self.cf, self.sm
        UT3 = UT[:, :].rearrange("p (k t) -> p k t", k=8)
        KT2 = RX[:, :].rearrange("p (g t) -> p g t", g=4)
        Vp = RY[:, 0:8320].rearrange("p (t g c) -> p t g c", t=32, g=4)
        KiT2 = RZ[:, 0:4096]
        wtok = self.wtok[:, 0:256]
        lo_t = self.wtok[:, 256:512]
        hi_t = self.wtok[:, 512:768]
        bw = self.bwin[lb]
        self.fence(["RX", "RY", "RZ", "RW", "RT"])
        sc.pool(lambda e: e.memset(RY[:, 0:8320], 1.0), w=[("RY", "*")])
        ob = [RT[:, 5120:5632], RT[:, 5632:6144]]
        th = f32v(RT[:, 6656:7680])
        sg = f32v(RT[:, 7680:8704])
        tq = f32v(RT[:, 8704:9728])
        rd = f32v(RT[:, 9728:10752])
        yb = [RT[:, 10752:11264], RT[:, 11264:11776]]
        wr = [th, sg]

        def sink_sb(dst3, g, dkey):
            def sink(tc, t1, t2, keys):
                sc.dve(lambda e: e.tensor_tensor(out=dst3[:, g, tc * 512:(tc + 1) * 512], in0=t1, in1=t2, op=ALU.add),
                       r=keys, w=[dkey])
            return sink

        def sink_dram(dram_rows, tag, mul=None):
            def sink(tc, t1, t2, keys):
                oi = self.ring("ob", 2)
                o_ = ob[oi]
                if mul is None:
                    sc.dve(lambda e: e.tensor_tensor(out=o_, in0=t1, in1=t2, op=ALU.add), r=keys, w=[("RT", ("ob", oi))])
                else:
                    wv, wkeys = mul(tc)
                    sc.dve(lambda e: e.tensor_tensor(out=t1, in0=t1, in1=t2, op=ALU.add), r=keys, w=[keys[0]])
                    sc.pool(lambda e: e.tensor_tensor(out=o_, in0=t1, in1=wv, op=ALU.mult),
                            r=[keys[0]] + wkeys, w=[("RT", ("ob", oi))])
                sc.dma(lambda e: e.dma_start(out=dram_rows[:, tc * 512:(tc + 1) * 512], in_=o_),
                       r=[("RT", ("ob", oi))], w=[(tag, tc)], q="scalar")
            return sink

        for g in range(4):
            wb, wk = self.load_piece(bw[BP_K2 + g])
            self.proj_rope(wb, wk, sink_sb(KT2, g, ("RX", ("KT2", g))))
        wb, wk = self.load_piece(bw[BP_KI2])
        KiT3 = RZ[:, 0:4096].rearrange("p (g t) -> p g t", g=1)
        self.proj_rope(wb, wk, sink_sb(KiT3, 0, ("RZ", "KiT2")))
        for p in range(2):
            wb, wk = self.load_piece(bw[BP_V + p])
            for J0 in range(0, 32, 4):
                pv = 2 + self.ring("psB", 2)
                for jj in range(4):
                    J = J0 + jj
                    for kc in range(8):
                        sc.pe(lambda e, kc=kc, jj=jj, J=J, pv=pv, wb=wb: e.matmul(
                            self.ps[pv][:, jj * 128:(jj + 1) * 128], lhsT=UT3[:, kc, J * 128:(J + 1) * 128],
                            rhs=wb[:, kc, :], start=(kc == 0), stop=(kc == 7)),
                            r=[wk] + self.ut_keys([J], kc), w=[pk(pv)])
                sc.act(lambda e, pv=pv, J0=J0, p=p: e.activation(
                    out=Vp[:, J0:J0 + 4, 2 * p:2 * p + 2, 0:64],
                    in_=self.ps[pv][:, :].rearrange("p (j e c) -> p j e c", j=4, e=2), func=AF.Copy),
                    r=[pk(pv)], w=[("RY", ("V", J0, p))])
        wb, wk = self.load_piece(bw[BP_WI])
        pw = 2 + self.ring("psB", 2)
        for J in range(32):
            for kc in range(8):
                sc.pe(lambda e, kc=kc, J=J, wb=wb: e.matmul(
                    self.ps[pw][:, J * 8:(J + 1) * 8], lhsT=UT3[:, kc, J * 128:(J + 1) * 128], rhs=wb[:, kc, 0:8],
                    start=(kc == 0), stop=(kc == 7)), r=[wk] + self.ut_keys([J], kc), w=[pk(pw)])
        sc.act(lambda e: e.activation(out=wtok, in_=self.ps[pw][:, 0:256], func=AF.Copy), r=[pk(pw)], w=[("wtok", "w")])
        sc.dve(lambda e: e.tensor_scalar(out=lo_t, in0=wtok, scalar1=0.0, scalar2=2.0, op0=ALU.is_ge, op1=ALU.mult),
               r=[("wtok", "w")], w=[("wtok", "lo")])
        sc.dve(lambda e: e.tensor_scalar(out=lo_t, in0=lo_t, scalar1=-1.0, scalar2=None, op0=ALU.add),
               r=[("wtok", "lo")], w=[("wtok", "lo")])
        for p in range(4):
            wbr, wkr = self.load_piece(bw[BP_WREP + p])
            wbq, wkq = self.load_piece(bw[BP_QI + p])

            def pre_chunk(tc, wbr=wbr, wkr=wkr):
                pa = self.ring("psA", 2)
                wi_ = tc % 2
                for kc in range(8):
                    sc.pe(lambda e, kc=kc, pa=pa: e.matmul(
                        self.ps[pa][:, :], lhsT=wbr[:, kc, :], rhs=UT3[:, kc, tc * 512:(tc + 1) * 512],
                        start=(kc == 0), stop=(kc == 7)),
                        r=[wkr] + self.ut_keys(range(tc * 4, tc * 4 + 4), kc), w=[pk(pa)])
                sc.act(lambda e, pa=pa, wi_=wi_: e.activation(out=wr[wi_], in_=self.ps[pa][:, :], func=AF.Abs,
                                                             scale=IDX_SCALE),
                       r=[pk(pa)], w=[("RT", ("wr", wi_))])

            def mul(tc):
                return wr[tc % 2], [("RT", ("wr", tc % 2))]

            self.proj_rope(wbq, wkq, sink_dram(self.QiTd[p * 128:(p + 1) * 128, :], ("QiT", p), mul=mul),
                           pre_chunk=pre_chunk)
        for p in range(8):
            wb, wk = self.load_piece(bw[BP_Q + p])
            self.proj_rope(wb, wk, sink_dram(self.QTd[p * 128:(p + 1) * 128, :], ("QT", p)))
        for p in range(8):
            wb, wk = self.load_piece(bw[BP_GATE + p])
            for e_ in range(2):
                h = 2 * p + e_
                for tc in range(8):
                    pg = 2 + self.ring("psB", 2)
                    for kc in range(8):
                        sc.pe(lambda e, kc=kc, pg=pg, wb=wb, e_=e_, tc=tc: e.matmul(
                            self.ps[pg][0:64, :], lhsT=wb[:, kc, 64 * e_:64 * e_ + 64],
                            rhs=UT3[:, kc, tc * 512:(tc + 1) * 512], start=(kc == 0), stop=(kc == 7)),
                            r=[wk] + self.ut_keys(range(tc * 4, tc * 4 + 4), kc), w=[pk(pg)])
                    sc.act(lambda e, pg=pg: e.activation(out=tq[0:64, :], in_=self.ps[pg][0:64, :], func=AF.Tanh, scale=0.5),
                           r=[pk(pg)], w=[("RT", "tq")])
                    oi = self.ring("ob", 2)
                    o_ = ob[oi]
                    sc.dve(lambda e, pg=pg, o_=o_: e.scalar_tensor_tensor(
                        out=o_[0:64, :], in0=tq[0:64, :], scalar=1.0, in1=self.ps[pg][0:64, :], op0=ALU.add, op1=ALU.mult),
                        r=[pk(pg), ("RT", "tq")], w=[("RT", ("ob", oi))])
                    sc.dma(lambda e, o_=o_, h=h, tc=tc: e.dma_start(
                        out=self.GTd[h * 64:(h + 1) * 64, tc * 512:(tc + 1) * 512], in_=o_[0:64, :]),
                        r=[("RT", ("ob", oi))], w=[(("GT", h), tc)], q="scalar")

        self.fence(["UT", "RW", "RT"])
        accB = f32v(UT[:, 0:16384]).rearrange("p (h q) -> p h q", h=16)
        score = f32v(UT[:, 16384:24576])
        maskb = UT[:, 24576:28672]
        maskT = UT[:, 28672:32768].rearrange("p (j q) -> p j q", j=32)
        QTc = RW[:, 0:4096].rearrange("p (a q) -> p a q", a=8)
        QiTc = RW[:, 4096:6144].rearrange("p (a q) -> p a q", a=4)
        tmpS = [f32v(RW[:, 6144:7168]), f32v(RW[:, 7168:8192])]
        GTc = [RW[:, 8192:8704], RW[:, 8704:9216]]
        PTs = [RT[:, i * 1024:(i + 1) * 1024] for i in range(3)]
        ident = cb[:, CB_ID:CB_ID + 128]
        caus = cf[:, CF_CAUS:CF_CAUS + 128]
        for qt in range(32):
            sc4, q0 = qt // 4, (qt % 4) * 128
            csl = slice(sc4 * 512, (sc4 + 1) * 512)
            if qt % 4 == 0:
                sc.dma(lambda e, csl=csl: e.dma_start(out=QTc, in_=self.QTd.rearrange("(a p) t -> p a t", p=128)[:, :, csl]),
                       r=[(("QT", p), sc4) for p in range(8)], w=[("RW", "QTc")])
                sc.dma(lambda e, csl=csl: e.dma_start(out=QiTc, in_=self.QiTd.rearrange("(a p) t -> p a t", p=128)[:, :, csl]),
                       r=[(("QiT", p), sc4) for p in range(4)], w=[("RW", "QiTc")])
            nk = qt + 1
            ncols = nk * 128
            for c0 in range(0, ncols, 512):
                cw = min(512, ncols - c0)
                for hh in range(8):
                    par = hh % 2
                    sc.pe(lambda e, hh=hh, par=par, c0=c0, cw=cw, q0=q0: e.matmul(
                        self.ps[2 + par][:, 0:cw], lhsT=QiTc[64 * par:64 * par + 64, hh // 2, q0:q0 + 128],
                        rhs=KiT2[64 * par:64 * par + 64, c0:c0 + cw], start=True, stop=True),
                        r=[("RW", "QiTc"), ("RZ", "KiT2")], w=[pk(2 + par)])
                    col = qt * 8 + hh
                    ti = self.ring("tmpS", 2)
                    t_ = tmpS[ti]
                    sc.act(lambda e, par=par, cw=cw, t_=t_: e.activation(out=t_[:, 0:cw], in_=self.ps[2 + par][:, 0:cw],
                                                                       func=AF.Relu),
                           r=[pk(2 + par)], w=[("RW", ("tmpS", ti))])
                    if hh == 0:
                        sc.dve(lambda e, c0=c0, cw=cw, col=col, t_=t_: e.tensor_scalar(
                            out=score[:, c0:c0 + cw], in0=t_[:, 0:cw], scalar1=lo_t[:, col:col + 1], scalar2=None,
                            op0=ALU.mult),
                            r=[("RW", ("tmpS", ti)), ("wtok", "lo")], w=[("UT", "score")])
                    else:
                        sc.dve(lambda e, c0=c0, cw=cw, col=col, t_=t_: e.scalar_tensor_tensor(
                            out=score[:, c0:c0 + cw], in0=t_[:, 0:cw], scalar=lo_t[:, col:col + 1],
                            in1=score[:, c0:c0 + cw], op0=ALU.mult, op1=ALU.add),
                            r=[("RW", ("tmpS", ti)), ("wtok", "lo"), ("UT", "score")], w=[("UT", "score")])
            sc.dve(lambda e, qt=qt: e.tensor_tensor(out=score[:, qt * 128:(qt + 1) * 128],
                                                    in0=score[:, qt * 128:(qt + 1) * 128], in1=caus, op=ALU.add),
                   r=[("UT", "score"), ("cf", 0)], w=[("UT", "score")])
            if qt >= 2:
                mid = sm[:, 400:401]
                cnt = sm[:, 401:402]
                ind = sm[:, 402:403]
                thr = sm[:, 403:404]
                sc.dve(lambda e: e.memset(mid, BIS_LO + BIS_STEP0), w=[("sm", "mid")])
                sgn = sm[:, 404:405]
                tot = sm[:, 405:406]
                nD = (nk // 2) * 128 if nk >= 8 else ncols
                nA = ncols - nD
                for it in range(NIT):
                    step = BIS_STEP0 / (2.0 ** it)
                    sc.dve(lambda e, nD=nD: e.tensor_scalar(
                        out=maskb[:, 0:nD], in0=score[:, 0:nD], scalar1=mid, scalar2=None, op0=ALU.is_gt,
                        op1=ALU.add, accum_out=cnt),
                        r=[("UT", "score"), ("sm", "mid")], w=[("UT", "maskb"), ("sm", "cnt")])
                    if nA > 0:
                        sc.act(lambda e, nD=nD, ncols=ncols: e.activation(
                            out=maskb[:, nD:ncols], in_=score[:, nD:ncols], func=AF.Sign, bias=mid, scale=-1.0,
                            accum_out=sgn),
                            r=[("UT", "score"), ("sm", "mid")], w=[("UT", "maskbA"), ("sm", ("sgn", True))])
                        sc.dve(lambda e: e.scalar_tensor_tensor(out=tot, in0=sgn, scalar=-0.5, in1=cnt, op0=ALU.mult,
                                                                op1=ALU.add),
                               r=[("sm", ("sgn", True)), ("sm", "cnt")], w=[("sm", "tot")])
                        thr_c = TOPK - 0.5 - nA / 2.0
                        sc.dve(lambda e, step=step, thr_c=thr_c: e.tensor_scalar(out=ind, in0=tot, scalar1=thr_c, scalar2=step,
                                                                                 op0=ALU.is_gt, op1=ALU.mult),
                               r=[("sm", "tot")], w=[("sm", "ind")])
                    else:
                        sc.dve(lambda e, step=step: e.tensor_scalar(out=ind, in0=cnt, scalar1=TOPK - 0.5, scalar2=step,
                                                                   op0=ALU.is_gt, op1=ALU.mult),
                               r=[("sm", "cnt")], w=[("sm", "ind")])
                    sc.dve(lambda e, step=step: e.scalar_tensor_tensor(out=mid, in0=ind, scalar=-step / 2.0, in1=mid,
                                                                       op0=ALU.add, op1=ALU.add),
                           r=[("sm", "ind"), ("sm", "mid")], w=[("sm", "mid")])
                fstep = BIS_STEP0 / (2.0 ** NIT)
                sc.dve(lambda e: e.tensor_scalar(out=thr, in0=mid, scalar1=-fstep, scalar2=None, op0=ALU.add),
                       r=[("sm", "mid")], w=[("sm", "thr")])
                sc.dve(lambda e, ncols=ncols: e.tensor_scalar(out=maskb[:, 0:ncols], in0=score[:, 0:ncols], scalar1=thr,
                                                              scalar2=None, op0=ALU.is_gt),
                       r=[("UT", "score"), ("sm", "thr")], w=[("UT", "maskb"), ("UT", "maskbA")])
            else:
                sc.dve(lambda e, ncols=ncols: e.tensor_scalar(out=maskb[:, 0:ncols], in0=score[:, 0:ncols],
                                                              scalar1=-1.0e29, scalar2=None, op0=ALU.is_gt),
                       r=[("UT", "score")], w=[("UT", "maskb")])
            for j0 in range(0, nk, 8):
                m = min(8, nk - j0)
                pt_ = 2 + self.ring("psB", 2)
                ptv = self.ps[pt_][:, :].bitcast(BF16).rearrange("p (j q) -> p j q", j=8)
                for jj in range(m):
                    sc.pe(lambda e, jj=jj, j0=j0, ptv=ptv: e.transpose(
                        ptv[:, jj, :], maskb[:, (j0 + jj) * 128:(j0 + jj + 1) * 128], ident),
                        r=[("UT", "maskb"), ("UT", "maskbA"), ("cb", 0)], w=[pk(pt_)])
                sc.act(lambda e, ptv=ptv, j0=j0, m=m: e.activation(out=maskT[:, j0:j0 + m, :], in_=ptv[:, 0:m, :],
                                                                  func=AF.Copy),
                       r=[pk(pt_)], w=[("UT", "maskT")])
            steps = [(gk, j0, min(2, nk - j0)) for gk in range(4) for j0 in range(0, nk, 2)]
            pos_ = {}
            for gk in range(4):
                pos_[gk] = 6 + self.ring("psO", 2)

            def sbankb(i, par):
                return (4 + par) if i % 2 == 0 else par

            def emit_Sb(i, q0=q0):
                gk, j0, m = steps[i]
                for t_ in range(m):
                    j = j0 + t_
                    for r_ in range(4):
                        par, r2 = r_ % 2, r_ // 2
                        bS = sbankb(i, par)
                        c_ = (t_ * 2 + r2) * 128
                        sc.pe(lambda e, par=par, r2=r2, gk=gk, j=j, q0=q0, bS=bS, c_=c_: e.matmul(
                            self.ps[bS][:, c_:c_ + 128],
                            lhsT=KT2[64 * par:64 * par + 64, gk, j * 128:(j + 1) * 128],
                            rhs=QTc[64 * par:64 * par + 64, 2 * gk + r2, q0:q0 + 128], start=True, stop=True),
                            r=[("RX", ("KT2", gk)), ("RW", "QTc")], w=[pk(bS)])

            emit_Sb(0)
            for i, (gk, j0, m) in enumerate(steps):
                po = pos_[gk]
                if i + 1 < len(steps):
                    emit_Sb(i + 1)
                pti = self.ring("PT", 3)
                PT4 = PTs[pti].rearrange("p (t h q) -> p t h q", t=2, h=4)
                for par in range(2):
                    bS = sbankb(i, par)
                    sc.act(lambda e, par=par, PT4=PT4, bS=bS, m=m: e.activation(
                        out=PT4[:, 0:m, 2 * par:2 * par + 2, :],
                        in_=self.ps[bS][:, 0:m * 256].rearrange("p (t b q) -> p t b q", t=m, b=2),
                        func=AF.Exp, scale=0.125), r=[pk(bS)], w=[("RT", ("PT", pti))])
                sc.dve(lambda e, PT4=PT4, j0=j0, m=m: e.tensor_tensor(
                    out=PT4[:, 0:m, :, :], in0=PT4[:, 0:m, :, :],
                    in1=maskT[:, j0:j0 + m, :].unsqueeze(2).to_broadcast([128, m, 4, 128]), op=ALU.mult),
                    r=[("RT", ("PT", pti)), ("UT", "maskT")], w=[("RT", ("PT", pti))])
                for t_ in range(m):
                    j = j0 + t_
                    for r_ in range(4):
                        par, r2 = r_ % 2, r_ // 2
                        sc.pe(lambda e, par=par, r2=r2, r_=r_, gk=gk, j=j, t_=t_, PT4=PT4, po=po: e.matmul(
                            self.ps[po][0:65, r_ * 128:(r_ + 1) * 128], lhsT=Vp[:, j, gk, :], rhs=PT4[:, t_, par * 2 + r2, :],
                            start=(j == 0 and r_ == 0), stop=(j == nk - 1), skip_group_check=True),
                            r=[("RY", ("V", (j // 4) * 4, gk // 2)), ("RT", ("PT", pti))], w=[pk(po)])
                if j0 + m == nk:
                    sc.act(lambda e, gk=gk, po=po, q0=q0: e.activation(
                        out=accB[0:65, 4 * gk:4 * gk + 4, q0:q0 + 128],
                        in_=self.ps[po][0:65, :].rearrange("p (r q) -> p r q", r=4), func=AF.Copy),
                        r=[pk(po)], w=[("UT", ("accB", gk))])
            if qt % 4 == 3:
                units = []
                for h in range(16):
                    gi = h % 2
                    g_ = GTc[gi]

                    def loader(g_=g_, h=h, csl=csl, gi=gi, sc4=sc4):
                        sc.dma(lambda e: e.dma_start(out=g_[0:64, :], in_=self.GTd[h * 64:(h + 1) * 64, csl]),
                               r=[(("GT", h), sc4)], w=[("RW", ("GTc", gi))])

                    units.append(dict(h=h, tc=sc4, acc=accB[:, h, :], akeys=[("UT", ("accB", h // 4))],
                                      gate=("dram", g_[0:64, :], [("RW", ("GTc", gi))], loader)))
                self.finalize_units(units)


_SHARED = {}


def prep_shared(inputs):
    cbv, cfv = make_consts()
    norm_g = np.asarray(inputs["norm_g"], np.float32)
    ada_b = np.asarray(inputs["ada_b"], np.float32)
    sh = dict(
        ngT=np.ascontiguousarray(norm_g.reshape(4, 8, 128).transpose(2, 0, 1).reshape(128, 32)),
        fg=np.ascontiguousarray(np.asarray(inputs["final_g"], np.float32).reshape(1, D)),
        ada_w=np.ascontiguousarray(np.asarray(inputs["ada_w"], np.float32)),
        adabT=np.ascontiguousarray(ada_b.reshape(4, 24, 128).transpose(2, 0, 1).reshape(128, 96)),
        adab=np.ascontiguousarray(ada_b),
        awout=np.ascontiguousarray(np.asarray(inputs["a_w_out"], np.float32)),
        bwout=np.ascontiguousarray(np.asarray(inputs["b_w_out"], np.float32)),
        cb=cbv, cf=cfv,
    )
    acols = a_piece_cols()
    a_w_in = np.asarray(inputs["a_w_in"], np.float32)
    sh["awin"] = np.stack([pieces_layout(a_w_in[i], acols) for i in range(2)])
    bcols = b_piece_cols()
    b_w_in = np.asarray(inputs["b_w_in"], np.float32)
    sh["bwin"] = np.stack([pieces_layout(b_w_in[i], bcols) for i in range(2)])
    return sh


def core_inputs(inputs, b, shared):
    m = dict(shared)
    m["x"] = np.ascontiguousarray(np.asarray(inputs["x"][b], np.float32))
    m["cT"] = np.ascontiguousarray(np.asarray(inputs["c"][b], np.float32).reshape(8, 128).T)
    m["pos"] = np.ascontiguousarray(np.asarray(inputs["positions"][b], np.int32).reshape(1, S))
    return m


_NC_CACHE = {}


def kernel(x, c, positions, norm_g, ada_w, ada_b, a_w_in, a_w_out, b_w_in, b_w_out, final_g):
    inputs = dict(x=x, c=c, positions=positions, norm_g=norm_g, ada_w=ada_w, ada_b=ada_b, a_w_in=a_w_in,
                  a_w_out=a_w_out, b_w_in=b_w_in, b_w_out=b_w_out, final_g=final_g)
    shared = prep_shared(inputs)
    nc = Prog().build()
    in_maps = [core_inputs(inputs, b, shared) for b in range(8)]
    res = run_bass_kernel_spmd(nc, in_maps, core_ids=list(range(8)))
    return np.stack([np.asarray(r["out"], np.float32) for r in res.results], axis=0)
```

```python
import numpy as np
import ml_dtypes
import concourse.bass as bass
import concourse.mybir as mybir
from concourse.bass_utils import run_bass_kernel_spmd

F32 = mybir.dt.float32
BF16 = mybir.dt.bfloat16
I32 = mybir.dt.int32
ALU = mybir.AluOpType
AF = mybir.ActivationFunctionType

S = 4096
D = 1024
NT = S // 128
EPS = 1e-6
A_GROUPS = ((128, 1), (512, 4), (2048, 16))
TOPK = 256


class Sched:
    ENGS = ("tensor", "scalar", "vector", "gpsimd", "sync")

    def __init__(self):
        self.ops = []

    def add(self, eng, fn, r=(), w=(), dma=0):
        self.ops.append(dict(eng=eng, fn=fn, r=list(r), w=list(w), dma=dma))

    def pe(self, fn, r=(), w=()):
        self.add("tensor", fn, r, w)

    def act(self, fn, r=(), w=()):
        self.add("scalar", fn, r, w)

    def dve(self, fn, r=(), w=()):
        self.add("vector", fn, r, w)

    def pool(self, fn, r=(), w=()):
        import os
        self.add(os.environ.get("KPOOL", "gpsimd"), fn, r, w)

    def dma(self, fn, r=(), w=(), n=1, q="sync"):
        self.add(q, fn, r, w, dma=n)

    def finalize(self, nc, n_dma_sems=16):
        ops = self.ops
        state = {}

        def touch(key):
            name, sub = key
            d = state.setdefault(name, {})
            if sub == "*":
                d.setdefault("*", [None, []])
                return list(d.keys())
            d.setdefault(sub, [None, []])
            return [sub, "*"] if "*" in d else [sub]

        for i, op in enumerate(ops):
            deps = set()
            for key in op["r"]:
                d = state.setdefault(key[0], {})
                for sub in touch(key):
                    wv = d[sub][0]
                    if wv is not None:
                        deps.add(wv)
            for key in op["w"]:
                d = state.setdefault(key[0], {})
                for sub in touch(key):
                    wv, rd = d[sub]
                    if wv is not None:
                        deps.add(wv)
                    deps.update(rd)
            for key in op["r"]:
                d = state[key[0]]
                subs = [key[1]] if key[1] != "*" else list(d.keys())
                for sub in subs:
                    rl = d[sub][1]
                    if not op["dma"]:
                        rl[:] = [j for j in rl if ops[j]["dma"] or ops[j]["eng"] != op["eng"]]
                    rl.append(i)
            for key in op["w"]:
                d = state[key[0]]
                subs = [key[1]] if key[1] != "*" else list(d.keys())
                for sub in subs:
                    d[sub][0] = i
                    d[sub][1] = []
            deps.discard(i)
            op["deps"] = deps

        pos = {}
        cnt = {e: 0 for e in self.ENGS}
        for i, op in enumerate(ops):
            pos[i] = cnt[op["eng"]]
            cnt[op["eng"]] += 1

        for i, op in enumerate(ops):
            keep = set()
            for dix in op["deps"]:
                p = ops[dix]
                if p["eng"] == op["eng"] and not p["dma"]:
                    if op["eng"] == "tensor" and not op["dma"]:
                        continue
                keep.add(dix)
            op["deps"] = keep

        dma_ops = [i for i, op in enumerate(ops) if op["dma"]]
        sem_tot = [0] * n_dma_sems
        last_on_sem = [None] * n_dma_sems
        qcount = {"sync": 0, "scalar": 0, "gpsimd": 0}
        qrange = {"sync": (0, n_dma_sems - 6), "scalar": (n_dma_sems - 6, n_dma_sems), "gpsimd": (n_dma_sems - 6, n_dma_sems)}
        for i in dma_ops:
            op = ops[i]
            lo_, hi_ = qrange[op["eng"]]
            s = lo_ + qcount[op["eng"]] % (hi_ - lo_)
            qcount[op["eng"]] += 1
            if last_on_sem[s] is not None:
                op["deps"].add(last_on_sem[s])
            sem_tot[s] += 16 * op["dma"]
            op["sig"] = ("dma", s, sem_tot[s])
            last_on_sem[s] = i

        needed = set()
        for op in ops:
            needed.update(op["deps"])
        sig_cnt = {e: 0 for e in self.ENGS}
        for i, op in enumerate(ops):
            if op["dma"]:
                continue
            if i in needed:
                sig_cnt[op["eng"]] += 1
                op["sig"] = ("eng", op["eng"], sig_cnt[op["eng"]])
            else:
                op["sig"] = None

        self.by_eng = {e: [op for op in ops if op["eng"] == e] for e in self.ENGS}
        self.final_dma = [(s, sem_tot[s]) for s in range(n_dma_sems) if sem_tot[s] > 0]
        self.n_dma_sems = n_dma_sems

    def emit(self, nc, block, sems, dma_sems):
        ops = self.ops

        def run_engine(e, name):
            known = {}
            for op in self.by_eng[name]:
                waits = {}
                for dix in op["deps"]:
                    sg = ops[dix]["sig"]
                    if sg[0] == "dma":
                        key = ("dma", sg[1])
                    else:
                        key = ("eng", sg[1])
                    waits[key] = max(waits.get(key, 0), sg[2])
                for key, val in waits.items():
                    if known.get(key, 0) >= val:
                        continue
                    known[key] = val
                    sem = dma_sems[key[1]] if key[0] == "dma" else sems[key[1]]
                    e.wait_ge(sem, val)
                inst = op["fn"](e)
                sg = op["sig"]
                if sg is not None:
                    if sg[0] == "dma":
                        insts = inst if isinstance(inst, (list, tuple)) else [inst]
                        assert len(insts) == op["dma"], (len(insts), op["dma"])
                        for ins in insts:
                            ins.then_inc(dma_sems[sg[1]], 16)
                    else:
                        inst.then_inc(sems[name], 1)
            if name in ("sync", "scalar"):
                for s, tot in self.final_dma:
                    if known.get(("dma", s), 0) < tot:
                        e.wait_ge(dma_sems[s], tot)

        @block.tensor
        def _(e):
            run_engine(e, "tensor")

        @block.scalar
        def _(e):
            run_engine(e, "scalar")

        @block.vector
        def _(e):
            run_engine(e, "vector")

        @block.gpsimd
        def _(e):
            run_engine(e, "gpsimd")

        @block.sync
        def _(e):
            run_engine(e, "sync")


NEG_BIG = -1.0e30
IDX_SCALE = float((8 ** -0.5) * (64 ** -0.5))
TWO_PI = float(2 * np.pi)
CW1 = 6.28125
CW2 = float(2 * np.pi - 6.28125)
NIT = 22
BIS_LO = -64.0
BIS_STEP0 = 64.0

CB_ID = 0
CB_SWAP = 128
CB_BAND = 256
CB_ONES = 512
CB_BAND2 = 640
CB_N = 1152
CF_INVF = 0
CF_SIGN = 1
CF_CAUS = 2
CF_ONES = 130
CF_N = 258


def make_consts():
    cb = np.zeros((128, CB_N), np.float32)
    cb[:, CB_ID:CB_ID + 128] = np.eye(128, dtype=np.float32)
    for m in range(128):
        k = m + 32 if (m % 64) < 32 else m - 32
        cb[k, CB_SWAP + m] = 1.0
    kj = np.arange(128)[:, None]
    c = np.arange(128)[None, :]
    cb[:, CB_BAND:CB_BAND + 128] = (kj <= c)
    cb[:, CB_BAND + 128:CB_BAND + 256] = (kj >= c)
    cb[:, CB_ONES:CB_ONES + 128] = 1.0
    cb[:, CB_BAND2:CB_BAND2 + 256] = cb[:, CB_BAND:CB_BAND + 256]
    cb[:, CB_BAND2 + 256:CB_BAND2 + 512] = cb[:, CB_BAND:CB_BAND + 256]
    cf = np.zeros((128, CF_N), np.float32)
    half = 32
    inv_freq = (10000.0 ** (-np.arange(half, dtype=np.float32) / half)).astype(np.float32)
    p = np.arange(128)
    cf[:, CF_INVF] = inv_freq[p % 32]
    cf[:, CF_SIGN] = np.where((p % 64) < 32, -1.0, 1.0)
    t = np.arange(128)[:, None]
    s = np.arange(128)[None, :]
    cf[:, CF_CAUS:CF_CAUS + 128] = np.where(s <= t, 0.0, NEG_BIG)
    cf[:, CF_ONES:CF_ONES + 128] = 1.0
    return cb.astype(ml_dtypes.bfloat16), cf


def a_piece_cols():
    out = []
    for hp in range(8):
        pcs = []
        heads = (2 * hp, 2 * hp + 1)
        for g in range(3):
            for t in range(3):
                pcs.append([g * 3072 + t * 1024 + h * 64 + d for h in heads for d in range(64)])
        pcs.append([9216 + h * 64 + d for h in heads for d in range(64)])
        out.append(pcs)
    return np.array(out, np.int64)


B_OFF = dict(q=0, k=1024, v=1280, qi=1536, ki=2048, wi=2112, gate=2120)
BP_Q = 0
BP_K2 = 8
BP_V = 12
BP_QI = 14
BP_WREP = 18
BP_KI2 = 22
BP_GATE = 23
BP_WI = 31
BP_N = 32


def b_piece_cols():
    pcs = []
    for p in range(8):
        pcs.append([B_OFF["q"] + p * 128 + j for j in range(128)])
    for g in range(4):
        pcs.append([B_OFF["k"] + g * 64 + d for _ in range(2) for d in range(64)])
    for p in range(2):
        pcs.append([B_OFF["v"] + p * 128 + j for j in range(128)])
    for p in range(4):
        pcs.append([B_OFF["qi"] + p * 128 + j for j in range(128)])
    for p in range(4):
        pcs.append([B_OFF["wi"] + 2 * p + e for e in range(2) for _ in range(64)])
    pcs.append([B_OFF["ki"] + d for _ in range(2) for d in range(64)])
    for p in range(8):
        pcs.append([B_OFF["gate"] + p * 128 + j for j in range(128)])
    pcs.append([B_OFF["wi"] + (j % 8) for j in range(128)])
    return np.array(pcs, np.int64)


def pieces_layout(w, cols):
    g = w[:, cols.reshape(-1)]
    npc = cols.size // 128
    g = g.reshape(8, 128, npc, 128)
    g = np.ascontiguousarray(g.transpose(2, 1, 0, 3))
    return g.reshape(cols.shape[:-1] + (128, 8, 128))


from contextlib import ExitStack


def pk(bank, sub="*"):
    return ("ps%d" % bank, sub)


def f32v(ap):
    return ap.bitcast(F32)


class Prog:
    def __init__(self, layers=(0, 1, 2, 3), final_norm=True, debug_out=None):
        self.layers = tuple(layers)
        self.final_norm = final_norm
        self.debug_out = debug_out
        self.nc = bass.Bass("TRN2", target_bir_lowering=False)
        self.sc = Sched()
        self.rr = {}

    def ring(self, name, n):
        i = self.rr.get(name, 0)
        self.rr[name] = i + 1
        return i % n

    def build(self):
        nc = self.nc
        sc = self.sc
        dt = nc.dram_tensor
        self.x = dt("x", [S, D], F32, kind="ExternalInput").ap()
        self.cT = dt("cT", [128, 8], F32, kind="ExternalInput").ap()
        self.pos = dt("pos", [1, S], I32, kind="ExternalInput").ap()
        self.ngT = dt("ngT", [128, 32], F32, kind="ExternalInput").ap()
        self.fg = dt("fg", [1, D], F32, kind="ExternalInput").ap()
        self.ada_w = dt("ada_w", [4, D, 3 * D], F32, kind="ExternalInput").ap()
        self.adabT = dt("adabT", [128, 96], F32, kind="ExternalInput").ap()
        self.adab = dt("adab", [4, 3 * D], F32, kind="ExternalInput").ap()
        self.awin = dt("awin", [2, 8, 10, 128, 8, 128], F32, kind="ExternalInput").ap()
        self.awout = dt("awout", [2, D, D], F32, kind="ExternalInput").ap()
        self.bwin = dt("bwin", [2, BP_N, 128, 8, 128], F32, kind="ExternalInput").ap()
        self.bwout = dt("bwout", [2, D, D], F32, kind="ExternalInput").ap()
        self.cbd = dt("cb", [128, CB_N], BF16, kind="ExternalInput").ap()
        self.cfd = dt("cf", [128, CF_N], F32, kind="ExternalInput").ap()
        self.out = dt("out", [S, D], F32, kind="ExternalOutput").ap()
        self.yTd = dt("yT_scr", [D, S], BF16, kind="Internal").ap()
        self.gbd = dt("gb_scr", [4, 128, D], F32, kind="Internal").ap()
        self.QTd = dt("QT_scr", [D, S], BF16, kind="Internal").ap()
        self.QiTd = dt("QiT_scr", [512, S], BF16, kind="Internal").ap()
        self.GTd = dt("GT_scr", [D, S], BF16, kind="Internal").ap()

        with ExitStack() as es:
            def sb(name, shape, dtype):
                return es.enter_context(nc.sbuf_tensor(name, shape, dtype))

            self.UT = sb("UT", [128, 32768], BF16)
            self.TAB = sb("TAB", [128, 8192], BF16)
            self.RX = sb("RX", [128, 16384], BF16)
            self.RY = sb("RY", [128, 8704], BF16)
            self.RZ = sb("RZ", [128, 6656], BF16)
            self.RW = sb("RW", [128, 12288], BF16)
            self.RT = sb("RT", [128, 12288], BF16)
            self.cb = sb("cbs", [128, CB_N], BF16)
            self.cf = sb("cfs", [128, CF_N], F32)
            self.sm = sb("small", [128, 512], F32)
            self.wtok = sb("wtok", [128, 3 * 256], F32)
            self.dummy = sb("dummyt", [128, 8], F32)
            self.ps = [es.enter_context(nc.psum_tensor(f"ps{i}", [128, 512], F32)) for i in range(8)]
            self.sems = {e: es.enter_context(nc.semaphore(f"s_{e}")) for e in Sched.ENGS}
            NDS = 18
            self.dsems = [es.enter_context(nc.semaphore(f"d_{i}")) for i in range(NDS)]
            block = es.enter_context(nc.Block())

            self.define()
            import os
            if os.environ.get("KTRUNC"):
                sc.ops = sc.ops[:int(os.environ["KTRUNC"])]
            sc.finalize(nc, n_dma_sems=NDS)
            sc.emit(nc, block, self.sems, self.dsems)
        return nc

    def smv(self, a, b):
        return self.sm[:, a:b]

    def fence(self, regions):
        d = self.dummy
        self.sc.dve(lambda e: e.memset(d[:, 0:1], 0.0), w=[(r, "*") for r in regions] + [("dummy", 0)])

    def define(self):
        sc = self.sc
        cb, cf = self.cb, self.cf
        sc.dma(lambda e: e.dma_start(out=cb[:, :], in_=self.cbd[:, :]), w=[("cb", 0)])
        sc.dma(lambda e: e.dma_start(out=cf[:, :], in_=self.cfd[:, :]), w=[("cf", 0)])
        self.p0_tables()
        self.p0_mods()
        src = self.x
        for l in self.layers:
            self.p1_norm(l, src)
            if l % 2 == 0:
                self.p2_a(l)
            else:
                self.p2_b(l)
            self.p3_out(l, src)
            src = self.out
        if self.final_norm:
            self.p4_final(src)

    def p0_tables(self):
        sc = self.sc
        UT, cf = self.UT, self.cf
        posi = UT[:, 0:8192].bitcast(I32)
        ang = f32v(UT[:, 8192:16384])
        tk = f32v(UT[:, 16384:24576])
        rr_ = f32v(UT[:, 24576:32768])
        tki = UT[:, 16384:24576].bitcast(I32)
        C = self.TAB[:, 0:4096]
        Sg = self.TAB[:, 4096:8192]
        K = [("UT", "p0")]
        sc.dma(lambda e: e.dma_start(out=posi, in_=self.pos.partition_broadcast(128)), w=K)
        sc.dve(lambda e: e.tensor_copy(out=rr_, in_=posi), r=K, w=K)
        sc.dve(lambda e: e.tensor_scalar(out=ang, in0=rr_, scalar1=cf[:, CF_INVF:CF_INVF + 1], scalar2=None,
                                         op0=ALU.mult), r=K + [("cf", 0)], w=K)

        def reduce_and_sin(shift, dst, mul_sign):
            sc.dve(lambda e: e.tensor_scalar(out=tk, in0=ang, scalar1=shift, scalar2=1.0 / TWO_PI,
                                             op0=ALU.add, op1=ALU.mult), r=K, w=K)
            sc.dve(lambda e: e.tensor_copy(out=tki, in_=tk), r=K, w=K)
            sc.dve(lambda e: e.tensor_copy(out=tk, in_=tki), r=K, w=K)
            sc.dve(lambda e: e.tensor_scalar(out=rr_, in0=ang, scalar1=shift, scalar2=None, op0=ALU.add), r=K, w=K)
            sc.dve(lambda e: e.scalar_tensor_tensor(out=rr_, in0=tk, scalar=-CW1, in1=rr_, op0=ALU.mult, op1=ALU.add),
                   r=K, w=K)
            sc.dve(lambda e: e.scalar_tensor_tensor(out=rr_, in0=tk, scalar=-CW2, in1=rr_, op0=ALU.mult, op1=ALU.add),
                   r=K, w=K)
            sc.dve(lambda e: e.tensor_scalar(out=tk, in0=rr_, scalar1=float(np.pi), scalar2=-TWO_PI,
                                             op0=ALU.is_gt, op1=ALU.mult), r=K, w=K)
            sc.dve(lambda e: e.tensor_tensor(out=rr_, in0=rr_, in1=tk, op=ALU.add), r=K, w=K)
            sc.dve(lambda e: e.tensor_scalar(out=tk, in0=rr_, scalar1=float(-np.pi), scalar2=TWO_PI,
                                             op0=ALU.is_lt, op1=ALU.mult), r=K, w=K)
            sc.dve(lambda e: e.tensor_tensor(out=rr_, in0=rr_, in1=tk, op=ALU.add), r=K, w=K)
            sc.dve(lambda e: e.tensor_scalar(out=rr_, in0=rr_, scalar1=3.1415925, scalar2=-3.1415925,
                                             op0=ALU.min, op1=ALU.max), r=K, w=K)
            if mul_sign:
                sc.act(lambda e: e.activation(out=tk, in_=rr_, func=AF.Sin), r=K, w=K)
                sc.dve(lambda e: e.tensor_scalar(out=dst, in0=tk, scalar1=cf[:, CF_SIGN:CF_SIGN + 1], scalar2=None,
                                                 op0=ALU.mult), r=K, w=[("TAB", 0)])
            else:
                sc.act(lambda e: e.activation(out=dst, in_=rr_, func=AF.Sin), r=K, w=[("TAB", 0)])

        reduce_and_sin(0.0, Sg, True)
        reduce_and_sin(float(np.pi / 2), C, False)
        self.fence(["UT"])

    def p0_mods(self):
        sc = self.sc
        UT, RX, cf, cb, sm = self.UT, self.RX, self.cf, self.cb, self.sm
        c_sb = sm[:, 392:400]
        cact = sm[:, 384:392]
        modT = sm[:, 192:288]
        adabT = sm[:, 288:384]
        ng = sm[:, 160:192]
        gs = sm[:, 96:128]
        sh = sm[:, 128:160]
        stage = [f32v(UT[:, 0:8192]).rearrange("p (k n) -> p k n", k=8),
                 f32v(UT[:, 8192:16384]).rearrange("p (k n) -> p k n", k=8)]
        cbc = f32v(UT[:, 16384:18432]).rearrange("p (k n) -> p k n", k=8)
        gbt = f32v(UT[:, 18432:20480])
        abb = f32v(UT[:, 20480:22528])
        cbh = UT[:, 22528:23552].rearrange("p (k n) -> p k n", k=8)
        cbl = UT[:, 23552:24576].rearrange("p (k n) -> p k n", k=8)
        tmp = f32v(UT[:, 24576:25600]).rearrange("p (j n) -> p j n", j=4)
        whi = [RX[:, 0:4096].rearrange("p (k n) -> p k n", k=8), RX[:, 4096:8192].rearrange("p (k n) -> p k n", k=8)]
        wlo = [RX[:, 8192:12288].rearrange("p (k n) -> p k n", k=8),
               RX[:, 12288:16384].rearrange("p (k n) -> p k n", k=8)]
        ident = cb[:, CB_ID:CB_ID + 128]
        sc.dma(lambda e: e.dma_start(out=c_sb, in_=self.cT[:, :]), w=[("sm", "c")])
        sc.dma(lambda e: e.dma_start(out=adabT, in_=self.adabT[:, :]), w=[("sm", "adabT")])
        sc.dma(lambda e: e.dma_start(out=ng, in_=self.ngT[:, :]), w=[("sm", "ng")])
        sc.act(lambda e: e.activation(out=cact, in_=c_sb, func=AF.Silu), r=[("sm", "c")], w=[("sm", "cact")])
        for kc in range(8):
            sc.dve(lambda e, kc=kc: e.tensor_scalar(out=cbc[:, kc, :], in0=cf[:, CF_ONES:CF_ONES + 128],
                                                    scalar1=cact[:, kc:kc + 1], scalar2=None, op0=ALU.mult),
                   r=[("sm", "cact"), ("cf", 0)], w=[("UT", "cbc")])
        sc.dve(lambda e: e.tensor_copy(out=cbh, in_=cbc), r=[("UT", "cbc")], w=[("UT", "cbh")])
        sc.dve(lambda e: e.tensor_tensor(out=cbl, in0=cbc, in1=cbh, op=ALU.subtract),
               r=[("UT", "cbc"), ("UT", "cbh")], w=[("UT", "cbl")])
        for l in range(4):
            if l not in self.layers:
                continue
            sc.dma(lambda e, l=l: e.dma_start(out=abb, in_=self.adab[l:l + 1, 2048:3072].partition_broadcast(128)),
                   w=[("UT", "abb")])
            for cc in range(6):
                si = self.ring("adast", 2)
                st = stage[si]
                src_ = self.ada_w[l].rearrange("(k p) n -> p k n", p=128)[:, :, cc * 512:(cc + 1) * 512]
                sc.dma(lambda e, st=st, src_=src_: e.dma_start(out=st, in_=src_), w=[("UT", ("adast", si))])
                wh, wl = whi[si], wlo[si]
                sc.act(lambda e, st=st, wh=wh: e.activation(out=wh, in_=st, func=AF.Copy),
                       r=[("UT", ("adast", si))], w=[("RX", ("whi", si))])
                sc.dve(lambda e, st=st, wh=wh, wl=wl: e.tensor_tensor(out=wl, in0=st, in1=wh, op=ALU.subtract),
                       r=[("UT", ("adast", si)), ("RX", ("whi", si))], w=[("RX", ("wlo", si))])
                pi = self.ring("p0ps", 2)
                pp = self.ps[pi]
                combos = [(cbh, wh, "cbh", "whi"), (cbh, wl, "cbh", "wlo"), (cbl, wh, "cbl", "whi")]
                n = 0
                for (la_, ra_, lk, rk) in combos:
                    for kc in range(8):
                        sc.pe(lambda e, la_=la_, ra_=ra_, kc=kc, pp=pp, n=n: e.matmul(
                            pp[:, :], lhsT=la_[:, kc, :], rhs=ra_[:, kc, :], start=(n == 0), stop=(n == 23)),
                            r=[("UT", lk), ("RX", (rk, si))], w=[pk(pi)])
                        n += 1
                sc.dve(lambda e, pp=pp: e.tensor_tensor(
                    out=tmp, in0=pp[:, :].rearrange("p (j n) -> p j n", j=4),
                    in1=ident.unsqueeze(1).to_broadcast([128, 4, 128]), op=ALU.mult),
                    r=[pk(pi), ("cb", 0)], w=[("UT", "tmp")])
                c0 = l * 24 + cc * 4
                sc.dve(lambda e, c0=c0: e.tensor_reduce(out=modT[:, c0:c0 + 4], in_=tmp, axis=mybir.AxisListType.X,
                                                        op=ALU.add),
                       r=[("UT", "tmp")], w=[("sm", ("modTraw", l, cc))])
                if cc >= 4:
                    half = cc - 4
                    sc.dve(lambda e, half=half, pp=pp: e.tensor_tensor(
                        out=gbt[:, half * 512:(half + 1) * 512], in0=pp[:, :],
                        in1=abb[:, half * 512:(half + 1) * 512], op=ALU.add),
                        r=[pk(pi), ("UT", "abb")], w=[("UT", ("gbt", half))])
            sc.dma(lambda e, l=l: e.dma_start(out=self.gbd[l], in_=gbt), r=[("UT", ("gbt", 0)), ("UT", ("gbt", 1))],
                   w=[("gbd", l)], q="scalar")
        for l in self.layers:
            sc.dve(lambda e, l=l: e.tensor_tensor(out=modT[:, l * 24:(l + 1) * 24], in0=modT[:, l * 24:(l + 1) * 24],
                                                  in1=adabT[:, l * 24:(l + 1) * 24], op=ALU.add),
                   r=[("sm", ("modTraw", l, cc)) for cc in range(6)] + [("sm", "adabT")], w=[("sm", "modT")])
        for l in self.layers:
            sc.dve(lambda e, l=l: e.scalar_tensor_tensor(out=gs[:, l * 8:(l + 1) * 8], in0=modT[:, l * 24 + 8:l * 24 + 16],
                                                         scalar=1.0, in1=ng[:, l * 8:(l + 1) * 8], op0=ALU.add,
                                                         op1=ALU.mult),
                   r=[("sm", "modT"), ("sm", "ng")], w=[("sm", "gs")])
            sc.dve(lambda e, l=l: e.tensor_copy(out=sh[:, l * 8:(l + 1) * 8], in_=modT[:, l * 24:l * 24 + 8]),
                   r=[("sm", "modT")], w=[("sm", "sh")])
        self.fence(["UT", "RX"])

    def p1_norm(self, l, src):
        sc = self.sc
        RW, sm, cb = self.RW, self.sm, self.cb
        UT3 = self.UT[:, :].rearrange("p (k t) -> p k t", k=8)
        ss, rstd, ms = sm[:, 0:32], sm[:, 32:64], sm[:, 64:96]
        gs, sh = sm[:, 96:128], sm[:, 128:160]
        ht = [f32v(RW[:, 0:2048]), f32v(RW[:, 2048:4096])]
        xh = [RW[:, 4096:5120], RW[:, 5120:6144]]
        ident = cb[:, CB_ID:CB_ID + 128]
        self.fence(["RW"])
        for tt in range(NT):
            bi = self.ring("p1ht", 2)
            h_, x_ = ht[bi], xh[bi]
            hk = [("hd", tt)] if src is self.out else []
            sc.dma(lambda e, h_=h_, tt=tt: e.dma_start(out=h_, in_=src[tt * 128:(tt + 1) * 128, :]),
                   r=hk, w=[("RW", ("ht", bi))])
            sc.act(lambda e, h_=h_, x_=x_, tt=tt: e.activation(out=x_, in_=h_, func=AF.Square,
                                                               accum_out=ss[:, tt:tt + 1]),
                   r=[("RW", ("ht", bi))], w=[("RW", ("xh", bi)), ("sm", ("ss", tt))])
            sc.dve(lambda e, tt=tt: e.tensor_scalar(out=ms[:, tt:tt + 1], in0=ss[:, tt:tt + 1], scalar1=1.0 / D,
                                                    scalar2=EPS, op0=ALU.mult, op1=ALU.add),
                   r=[("sm", ("ss", tt))], w=[("sm", ("ms", tt))])
            sc.act(lambda e, tt=tt: e.activation(out=ms[:, tt:tt + 1], in_=ms[:, tt:tt + 1], func=AF.Sqrt),
                   r=[("sm", ("ms", tt))], w=[("sm", ("ms", tt))])
            sc.dve(lambda e, tt=tt: e.reciprocal(out=rstd[:, tt:tt + 1], in_=ms[:, tt:tt + 1]),
                   r=[("sm", ("ms", tt))], w=[("sm", ("rstd", tt))])
            sc.dve(lambda e, h_=h_, x_=x_, tt=tt: e.tensor_scalar(out=x_, in0=h_, scalar1=rstd[:, tt:tt + 1],
                                                                  scalar2=None, op0=ALU.mult),
                   r=[("RW", ("ht", bi)), ("sm", ("rstd", tt))], w=[("RW", ("xh", bi))])
            pstA = self.ps[6][:, :].bitcast(BF16).rearrange("p (k t) -> p k t", k=8)
            pstB = self.ps[7][:, :].bitcast(BF16).rearrange("p (k t) -> p k t", k=8)
            for kc in range(8):
                pst, bank = (pstA, 6) if kc < 4 else (pstB, 7)
                sc.pe(lambda e, x_=x_, kc=kc, pst=pst: e.transpose(pst[:, kc % 4, :], x_[:, kc * 128:(kc + 1) * 128], ident),
                      r=[("RW", ("xh", bi)), ("cb", 0)], w=[pk(bank)])
            for k4 in range(4):
                for (pst, bank, kc) in ((pstA, 6, k4), (pstB, 7, 4 + k4)):
                    o = UT3[:, kc, tt * 128:(tt + 1) * 128]
                    col = l * 8 + kc
                    if bank == 6:
                        sc.act(lambda e, o=o, k4=k4, pst=pst, col=col: e.activation(
                            out=o, in_=pst[:, k4, :], func=AF.Identity, bias=sh[:, col:col + 1], scale=gs[:, col:col + 1]),
                            r=[pk(6), ("sm", "gs"), ("sm", "sh")], w=[("UT", (tt, kc))])
                    else:
                        sc.dve(lambda e, o=o, k4=k4, pst=pst, col=col: e.tensor_scalar(
                            out=o, in0=pst[:, k4, :], scalar1=gs[:, col:col + 1], scalar2=sh[:, col:col + 1],
                            op0=ALU.mult, op1=ALU.add),
                            r=[pk(7), ("sm", "gs"), ("sm", "sh")], w=[("UT", (tt, kc))])

    def ut_keys(self, tiles, kc):
        return [("UT", (t, kc)) for t in tiles]

    def p4_final(self, src):
        sc = self.sc
        self.fence(["UT", "RW"])
        UT, sm = self.UT, self.sm
        fgb = f32v(UT[:, 0:2048])
        ht = [f32v(UT[:, 2048:4096]), f32v(UT[:, 4096:6144])]
        ho = [f32v(UT[:, 6144:8192]), f32v(UT[:, 8192:10240])]
        junk = f32v(UT[:, 10240:12288])
        ss, rstd, ms = sm[:, 0:32], sm[:, 32:64], sm[:, 64:96]
        sc.dma(lambda e: e.dma_start(out=fgb, in_=self.fg.partition_broadcast(128)), w=[("UT", "fgb")])
        for tt in range(NT):
            bi = self.ring("p4", 2)
            h_, o_ = ht[bi], ho[bi]
            hk = [("hd", tt)] if src is self.out else []
            sc.dma(lambda e, h_=h_, tt=tt: e.dma_start(out=h_, in_=src[tt * 128:(tt + 1) * 128, :]),
                   r=hk, w=[("UT", ("ht", bi))])
            sc.act(lambda e, h_=h_, tt=tt: e.activation(out=junk, in_=h_, func=AF.Square, accum_out=ss[:, tt:tt + 1]),
                   r=[("UT", ("ht", bi))], w=[("UT", "junk"), ("sm", ("ss", tt))])
            sc.dve(lambda e, tt=tt: e.tensor_scalar(out=ms[:, tt:tt + 1], in0=ss[:, tt:tt + 1], scalar1=1.0 / D,
                                                    scalar2=EPS, op0=ALU.mult, op1=ALU.add),
                   r=[("sm", ("ss", tt))], w=[("sm", ("ms", tt))])
            sc.act(lambda e, tt=tt: e.activation(out=ms[:, tt:tt + 1], in_=ms[:, tt:tt + 1], func=AF.Sqrt),
                   r=[("sm", ("ms", tt))], w=[("sm", ("ms", tt))])
            sc.dve(lambda e, tt=tt: e.reciprocal(out=rstd[:, tt:tt + 1], in_=ms[:, tt:tt + 1]),
                   r=[("sm", ("ms", tt))], w=[("sm", ("rstd", tt))])
            sc.dve(lambda e, h_=h_, o_=o_, tt=tt: e.scalar_tensor_tensor(
                out=o_, in0=h_, scalar=rstd[:, tt:tt + 1], in1=fgb, op0=ALU.mult, op1=ALU.mult),
                r=[("UT", ("ht", bi)), ("sm", ("rstd", tt)), ("UT", "fgb")], w=[("UT", ("ho", bi))])
            sc.dma(lambda e, o_=o_, tt=tt: e.dma_start(out=self.out[tt * 128:(tt + 1) * 128, :], in_=o_),
                   r=[("UT", ("ho", bi))], w=[("hd", tt)], q="scalar")

    def dump(self, name, ap, keys):
        shape = list(ap.shape)
        d = self.nc.dram_tensor("dbg_" + name, shape, ap.dtype, kind="ExternalOutput").ap()
        self.sc.dma(lambda e: e.dma_start(out=d, in_=ap), r=keys, w=[("dbg", name)], q="scalar")

    def p3_out(self, l, src):
        sc = self.sc
        self.fence(["UT"])
        UT = self.UT
        wd = (self.awout if l % 2 == 0 else self.bwout)[l // 2].rearrange("(k p) n -> p k n", p=128)
        wst = [f32v(UT[:, 0:4096]).rearrange("p (k n) -> p k n", k=2)]
        wob = UT[:, 4096:12288].rearrange("p (k n) -> p k n", k=8)
        gb = f32v(UT[:, 12288:14336])
        ych = [UT[:, 14336:18432].rearrange("p (k t) -> p k t", k=8),
               UT[:, 18432:22528].rearrange("p (k t) -> p k t", k=8)]
        ht = [f32v(UT[:, 22528:24576]), f32v(UT[:, 24576:26624])]
        hn = [f32v(UT[:, 26624:28672]), f32v(UT[:, 28672:30720])]
        sc.dma(lambda e: e.dma_start(out=gb, in_=self.gbd[l]), r=[("gbd", l)], w=[("UT", "gb")])
        for q4 in range(4):
            sc.dma(lambda e, q4=q4: e.dma_start(out=wst[0], in_=wd[:, 2 * q4:2 * q4 + 2, :]), w=[("UT", "wst")])
            for k2 in range(2):
                kc = 2 * q4 + k2
                sc.dve(lambda e, kc=kc, k2=k2: e.tensor_tensor(out=wob[:, kc, :], in0=wst[0][:, k2, :], in1=gb, op=ALU.mult),
                       r=[("UT", "wst"), ("UT", "gb")], w=[("UT", ("wob", kc))])
        for tc in range(8):
            yi = self.ring("p3y", 2)
            yc = ych[yi]
            srcy = self.yTd.rearrange("(k p) t -> p k t", p=128)[:, :, tc * 512:(tc + 1) * 512]
            sc.dma(lambda e, yc=yc, srcy=srcy: e.dma_start(out=yc, in_=srcy),
                   r=[("yT", (h, tc)) for h in range(16)], w=[("UT", ("ych", yi))])
            for t4 in range(4):
                tt = tc * 4 + t4
                bi = self.ring("p3h", 2)
                h_, n_ = ht[bi], hn[bi]
                hk = [("hd", tt)] if src is self.out else []
                sc.dma(lambda e, h_=h_, tt=tt: e.dma_start(out=h_, in_=src[tt * 128:(tt + 1) * 128, :]),
                       r=hk, w=[("UT", ("ht", bi))])
                for half in range(2):
                    pi = 4 + self.ring("p3ps", 4)
                    pp = self.ps[pi]
                    for kc in range(8):
                        sc.pe(lambda e, yc=yc, kc=kc, t4=t4, half=half, pp=pp: e.matmul(
                            pp[:, :], lhsT=yc[:, kc, t4 * 128:(t4 + 1) * 128], rhs=wob[:, kc, half * 512:(half + 1) * 512],
                            start=(kc == 0), stop=(kc == 7)),
                            r=[("UT", ("ych", yi)), ("UT", ("wob", kc))], w=[pk(pi)])
                    sc.dve(lambda e, h_=h_, n_=n_, half=half, pp=pp: e.tensor_tensor(
                        out=n_[:, half * 512:(half + 1) * 512], in0=pp[:, :], in1=h_[:, half * 512:(half + 1) * 512],
                        op=ALU.add),
                        r=[pk(pi), ("UT", ("ht", bi))], w=[("UT", ("hn", bi, half))])
                sc.dma(lambda e, n_=n_, tt=tt: e.dma_start(out=self.out[tt * 128:(tt + 1) * 128, :], in_=n_),
                       r=[("UT", ("hn", bi, 0)), ("UT", ("hn", bi, 1))], w=[("hd", tt)], q="scalar")
        self.fence(["UT"])

    def load_piece(self, dram_ap):
        sc = self.sc
        RW = self.RW
        si = self.ring("wst", 3)
        bi = self.ring("wbf", 4)
        st = f32v(RW[:, si * 2048:(si + 1) * 2048]).rearrange("p (k n) -> p k n", k=8)
        wb = RW[:, 6144 + bi * 1024:6144 + (bi + 1) * 1024].rearrange("p (k n) -> p k n", k=8)
        sc.dma(lambda e: e.dma_start(out=st, in_=dram_ap), w=[("RW", ("wst", si))])
        sc.pool(lambda e: e.tensor_copy(out=wb, in_=st), r=[("RW", ("wst", si))], w=[("RW", ("wbf", bi))])
        return wb, ("RW", ("wbf", bi))

    def proj_rope(self, wb, wkey, sink, pre_chunk=None):
        sc = self.sc
        RT, cb = self.RT, self.cb
        UT3 = self.UT[:, :].rearrange("p (k t) -> p k t", k=8)
        C = self.TAB[:, 0:4096]
        Sg = self.TAB[:, 4096:8192]
        swap = cb[:, CB_SWAP:CB_SWAP + 128]
        qb = [RT[:, 0:512], RT[:, 512:1024]]
        t1 = [f32v(RT[:, 1024:2048]), f32v(RT[:, 2048:3072])]
        t2 = [f32v(RT[:, 3072:4096]), f32v(RT[:, 4096:5120])]
        pend = None

        def do_swap(p):
            tc, bi, pa = p
            pb = 2 + self.ring("psB", 2)
            sc.pe(lambda e: e.matmul(self.ps[pb][:, :], lhsT=swap, rhs=qb[bi], start=True, stop=True),
                  r=[("RT", ("qb", bi)), ("cb", 0)], w=[pk(pb)])
            sl = slice(tc * 512, (tc + 1) * 512)
            sc.dve(lambda e: e.tensor_tensor(out=t1[bi], in0=self.ps[pb][:, :], in1=Sg[:, sl], op=ALU.mult),
                   r=[pk(pb), ("TAB", 0)], w=[("RT", ("t1", bi))])
            sc.pool(lambda e: e.tensor_tensor(out=t2[bi], in0=qb[bi], in1=C[:, sl], op=ALU.mult),
                    r=[("RT", ("qb", bi)), ("TAB", 0)], w=[("RT", ("t2", bi))])
            sink(tc, t1[bi], t2[bi], [("RT", ("t1", bi)), ("RT", ("t2", bi))])

        for tc in range(8):
            if pre_chunk is not None:
                pre_chunk(tc)
            pa = self.ring("psA", 2)
            bi = self.ring("qb", 2)
            for kc in range(8):
                sc.pe(lambda e, kc=kc, tc=tc, pa=pa: e.matmul(
                    self.ps[pa][:, :], lhsT=wb[:, kc, :], rhs=UT3[:, kc, tc * 512:(tc + 1) * 512],
                    start=(kc == 0), stop=(kc == 7)),
                    r=[wkey] + self.ut_keys(range(tc * 4, tc * 4 + 4), kc), w=[pk(pa)])
            sc.act(lambda e, pa=pa, bi=bi: e.activation(out=qb[bi], in_=self.ps[pa][:, :], func=AF.Copy),
                   r=[pk(pa)], w=[("RT", ("qb", bi))])
            if pend is not None:
                do_swap(pend)
            pend = (tc, bi, pa)
        do_swap(pend)

    def p2_a(self, l):
        sc = self.sc
        la = l // 2
        RX, RY, RZ, RT, cb, cf = self.RX, self.RY, self.RZ, self.RT, self.cb, self.cf
        UT3 = self.UT[:, :].rearrange("p (k t) -> p k t", k=8)
        acc = [f32v(RX[:, 0:8192]), f32v(RX[:, 8192:16384])]
        acc3 = f32v(RX[:, :]).rearrange("p (e t) -> p e t", e=2)
        QT, KT = RY[:, 0:4096], RY[:, 4096:8192]
        Vp = RZ[:, 0:4160].rearrange("p (t e c) -> p t e c", t=32, e=2)
        band = cb[:, CB_BAND:CB_BAND + 256]
        band2 = cb[:, CB_BAND2:CB_BAND2 + 512].rearrange("p (e q) -> p e q", e=2)
        self.fence(["RX", "RY", "RZ", "RW", "RT"])
        sc.pool(lambda e: e.memset(RZ[:, 0:4160], 1.0), w=[("RZ", "*")])
        PTs = [RT[:, 5120 + i * 512:5120 + (i + 1) * 512].rearrange("p (e q) -> p e q", e=2) for i in range(3)]
        th = f32v(RT[:, 6656:7680])
        sg = f32v(RT[:, 7680:8704])
        tq = f32v(RT[:, 8704:9728])
        rd = f32v(RT[:, 9728:10752])
        yb = [RT[:, 10752:11264], RT[:, 11264:11776]]

        def sink_to(dst, dkey, d, eng):
            def sink(tc, t1, t2, keys):
                dv = dst.rearrange("p (r i) -> p r i", r=d)[:, :, tc * 512 // d:(tc + 1) * 512 // d]
                a = t1.rearrange("p (i r) -> p r i", r=d)
                b = t2.rearrange("p (i r) -> p r i", r=d)
                sc.add(eng, lambda e: e.tensor_tensor(out=dv, in0=a, in1=b, op=ALU.add), r=keys, w=[dkey])
            return sink

        for hp in range(8):
            for g, (win, d) in enumerate(A_GROUPS):
                nb = 32 // d
                wb, wk = self.load_piece(self.awin[la, hp, 3 * g + 0])
                self.proj_rope(wb, wk, sink_to(QT, ("RY", "QT"), d, "vector"))
                wb, wk = self.load_piece(self.awin[la, hp, 3 * g + 1])
                self.proj_rope(wb, wk, sink_to(KT, ("RY", "KT"), d, "vector"))
                wb, wk = self.load_piece(self.awin[la, hp, 3 * g + 2])
                UTd = UT3.rearrange("p k (i r) -> p k r i", r=d)
                for J0 in range(0, 32, 4):
                    pv = 2 + self.ring("psB", 2)
                    for jj in range(4):
                        J = J0 + jj
                        r_, b_ = J // nb, J % nb
                        tiles = range(d * b_, d * b_ + d)
                        for kc in range(8):
                            sc.pe(lambda e, kc=kc, jj=jj, r_=r_, b_=b_, pv=pv, wb=wb, UTd=UTd: e.matmul(
                                self.ps[pv][:, jj * 128:(jj + 1) * 128], lhsT=UTd[:, kc, r_, b_ * 128:(b_ + 1) * 128],
                                rhs=wb[:, kc, :], start=(kc == 0), stop=(kc == 7)),
                                r=[wk] + self.ut_keys(tiles, kc), w=[pk(pv)])
                    sc.act(lambda e, pv=pv, J0=J0: e.activation(
                        out=Vp[:, J0:J0 + 4, :, 0:64],
                        in_=self.ps[pv][:, :].rearrange("p (j e c) -> p j e c", j=4, e=2), func=AF.Copy),
                        r=[pk(pv)], w=[("RZ", ("V", J0))])
                def blk(J, nb=nb):
                    first = (J % nb == 0)
                    has_off = (J + 1 < 32) and ((J + 1) % nb != 0)
                    return first, has_off, (256 if has_off else 128)

                def sbank(J, e_):
                    return (4 + e_) if J % 2 == 0 else e_

                def emit_S(J):
                    _, _, N = blk(J)
                    for e_ in range(2):
                        bS = sbank(J, e_)
                        sc.pe(lambda e, e_=e_, J=J, N=N, bS=bS: e.matmul(
                            self.ps[bS][:, 0:N], lhsT=KT[64 * e_:64 * e_ + 64, J * 128:(J + 1) * 128],
                            rhs=QT[64 * e_:64 * e_ + 64, J * 128:J * 128 + N], start=True, stop=True),
                            r=[("RY", "QT"), ("RY", "KT")], w=[pk(bS)])

                emit_S(0)
                for J in range(32):
                    first, has_off, N = blk(J)
                    if J + 1 < 32:
                        emit_S(J + 1)
                    pti = self.ring("PT", 3)
                    PT = PTs[pti]
                    for e_ in range(2):
                        bS = sbank(J, e_)
                        sc.act(lambda e, PT=PT, e_=e_, N=N, bS=bS: e.activation(out=PT[:, e_, 0:N], in_=self.ps[bS][:, 0:N],
                                                                              func=AF.Exp, scale=0.125),
                               r=[pk(bS)], w=[("RT", ("PT", pti))])
                    sc.dve(lambda e, PT=PT, N=N: e.tensor_tensor(
                        out=PT[:, :, 0:N], in0=PT[:, :, 0:N], in1=band2[:, :, 0:N], op=ALU.mult),
                        r=[("RT", ("PT", pti)), ("cb", 0)], w=[("RT", ("PT", pti))])
                    bk = 6 + (J % 2)
                    bk2 = 6 + ((J + 1) % 2)
                    for e_ in range(2):
                        sc.pe(lambda e, e_=e_, J=J, PT=PT, bk=bk, first=first: e.matmul(
                            self.ps[bk][0:65, e_ * 128:(e_ + 1) * 128], lhsT=Vp[:, J, e_, :], rhs=PT[:, e_, 0:128],
                            start=(first and e_ == 0), stop=True, skip_group_check=True),
                            r=[("RZ", ("V", (J // 4) * 4)), ("RT", ("PT", pti))], w=[pk(bk)])
                    if has_off:
                        for e_ in range(2):
                            sc.pe(lambda e, e_=e_, J=J, PT=PT, bk2=bk2: e.matmul(
                                self.ps[bk2][0:65, e_ * 128:(e_ + 1) * 128], lhsT=Vp[:, J, e_, :], rhs=PT[:, e_, 128:256],
                                start=(e_ == 0), stop=False, skip_group_check=True),
                                r=[("RZ", ("V", (J // 4) * 4)), ("RT", ("PT", pti))], w=[pk(bk2)])
                    r_, b_ = J // nb, J % nb
                    tiles = range(d * b_, d * b_ + d)
                    pO = self.ps[bk][0:65, 0:256].rearrange("p (e q) -> p e q", e=2)
                    av = acc3.rearrange("p e (i r) -> p e r i", r=d)[0:65, :, r_, b_ * 128:(b_ + 1) * 128]
                    akeys = [("RX", ("acc", e_, t)) for t in tiles for e_ in range(2)]
                    if g == 0:
                        sc.dve(lambda e, av=av, pO=pO: e.tensor_copy(out=av, in_=pO), r=[pk(bk)], w=akeys)
                    else:
                        sc.dve(lambda e, av=av, pO=pO: e.tensor_tensor(out=av, in0=pO, in1=av, op=ALU.add),
                               r=[pk(bk)] + akeys, w=akeys)
            wbg, wkg = self.load_piece(self.awin[la, hp, 9])
            units = []
            for e_ in range(2):
                for tc in range(8):
                    units.append(dict(h=2 * hp + e_, tc=tc, acc=acc[e_][:, tc * 512:(tc + 1) * 512],
                                      akeys=[("RX", ("acc", e_, t)) for t in range(tc * 4, tc * 4 + 4)],
                                      gate=("proj", wbg, wkg, e_)))
            self.finalize_units(units)

    def finalize_units(self, units):
        sc = self.sc
        RT = self.RT
        UT3 = self.UT[:, :].rearrange("p (k t) -> p k t", k=8)
        th = f32v(RT[:, 6656:7680])
        sg = f32v(RT[:, 7680:8704])
        tq = f32v(RT[:, 8704:9728])
        yb = [RT[:, 10752:11264], RT[:, 11264:11776]]
        rdh = [RT[64:65, 11776:12288], RT[64:65, 7680:8192]]
        rdl = [RT[64:65, 7168:7680], RT[64:65, 8704:9216]]
        onesb = self.cb[64:65, CB_ONES:CB_ONES + 64]
        st = {}

        rb = f32v(RT[:, 9728:10752])

        def stage1(i):
            u = units[i]
            ri = i % 2
            acc_ap, akeys = u["acc"], u["akeys"]
            sc.act(lambda e: e.activation(out=rdh[ri], in_=acc_ap[64:65, :], func=AF.Copy, scale=2.0),
                   r=akeys, w=[("RT", ("rdh", ri))])
            sc.dve(lambda e: e.scalar_tensor_tensor(out=rdl[ri], in0=acc_ap[64:65, :], scalar=2.0, in1=rdh[ri],
                                                    op0=ALU.mult, op1=ALU.subtract),
                   r=akeys + [("RT", ("rdh", ri))], w=[("RT", ("rdl", ri))])
            if u["gate"][0] == "dram":
                u["gate"][3]()
            if u["gate"][0] == "proj":
                _, wbg, wkg, e_ = u["gate"]
                tc = u["tc"]
                pg = 2 + ri
                for kc in range(8):
                    sc.pe(lambda e, kc=kc: e.matmul(
                        self.ps[pg][0:64, :], lhsT=wbg[:, kc, 64 * e_:64 * e_ + 64],
                        rhs=UT3[:, kc, tc * 512:(tc + 1) * 512], start=(kc == 0), stop=(kc == 7)),
                        r=[wkg] + self.ut_keys(range(tc * 4, tc * 4 + 4), kc), w=[pk(pg)])

        def stage2(i):
            u = units[i]
            ri = i % 2
            acc_ap, akeys = u["acc"], u["akeys"]
            pb = ri
            sc.pe(lambda e: e.matmul(self.ps[pb][0:64, :], lhsT=onesb, rhs=rdh[ri], start=True, stop=False),
                  r=[("RT", ("rdh", ri)), ("cb", 0)], w=[pk(pb)])
            sc.pe(lambda e: e.matmul(self.ps[pb][0:64, :], lhsT=onesb, rhs=rdl[ri], start=False, stop=True),
                  r=[("RT", ("rdl", ri)), ("cb", 0)], w=[pk(pb)])
            if u["gate"][0] == "proj":
                pg = 2 + ri
                sc.act(lambda e: e.activation(out=th[0:64, :], in_=self.ps[pg][0:64, :], func=AF.Tanh, scale=0.5),
                       r=[pk(pg)], w=[("RT", "th")])
                sc.dve(lambda e: e.scalar_tensor_tensor(out=sg[0:64, :], in0=th[0:64, :], scalar=1.0,
                                                        in1=self.ps[pg][0:64, :], op0=ALU.add, op1=ALU.mult),
                       r=[pk(pg), ("RT", "th")], w=[("RT", "sg")])
                gsrc, gkeys = sg[0:64, :], [("RT", "sg")]
            else:
                gsrc, gkeys = u["gate"][1], u["gate"][2]
            sc.dve(lambda e: e.reciprocal(out=rb[0:64, :], in_=self.ps[pb][0:64, :]), r=[pk(pb)], w=[("RT", "rb")])
            sc.dve(lambda e: e.tensor_tensor(out=tq[0:64, :], in0=acc_ap[0:64, :], in1=rb[0:64, :], op=ALU.mult),
                   r=akeys + [("RT", "rb")], w=[("RT", "tq")])
            yi = self.ring("yb", 2)
            y_ = yb[yi]
            sc.pool(lambda e: e.tensor_tensor(out=y_[0:64, :], in0=tq[0:64, :], in1=gsrc, op=ALU.mult),
                    r=[("RT", "tq")] + gkeys, w=[("RT", ("yb", yi))])
            h, tc = u["h"], u["tc"]
            sc.dma(lambda e: e.dma_start(out=self.yTd[h * 64:(h + 1) * 64, tc * 512:(tc + 1) * 512], in_=y_[0:64, :]),
                   r=[("RT", ("yb", yi))], w=[("yT", (h, tc))], q="scalar")

        stage1(0)
        for i in range(len(units)):
            if i + 1 < len(units):
                stage1(i + 1)
            stage2(i)

    def p2_b(self, l):
        sc = self.sc
        lb = l // 2
        UT, RX, RY, RZ, RW, RT, cb, cf, sm = self.UT, self.RX, self.RY, self.RZ, self.RW, self.RT, self.cb, self.cf, self.sm
        UT3 = UT[:, :].rearrange("p (k t) -> p k t", k=8)
        KT2 = RX[:, :].rearrange("p (g t) -> p g t", g=4)
        Vp = RY[:, 0:8320].rearrange("p (t g c) -> p t g c", t=32, g=4)
        KiT2 = RZ[:, 0:4096]
        wtok = self.wtok[:, 0:256]
        lo_t = self.wtok[:, 256:512]
        hi_t = self.wtok[:, 512:768]
        bw = self.bwin[lb]
        self.fence(["RX", "RY", "RZ", "RW", "RT"])
        sc.pool(lambda e: e.memset(RY[:, 0:8320], 1.0), w=[("RY", "*")])
        ob = [RT[:, 5120:5632], RT[:, 5632:6144]]
        th = f32v(RT[:, 6656:7680])
        sg = f32v(RT[:, 7680:8704])
        tq = f32v(RT[:, 8704:9728])
        rd = f32v(RT[:, 9728:10752])
        yb = [RT[:, 10752:11264], RT[:, 11264:11776]]
        wr = [th, sg]

        def sink_sb(dst3, g, dkey):
            def sink(tc, t1, t2, keys):
                sc.dve(lambda e: e.tensor_tensor(out=dst3[:, g, tc * 512:(tc + 1) * 512], in0=t1, in1=t2, op=ALU.add),
                       r=keys, w=[dkey])
            return sink

        def sink_dram(dram_rows, tag, mul=None):
            def sink(tc, t1, t2, keys):
                oi = self.ring("ob", 2)
                o_ = ob[oi]
                if mul is None:
                    sc.dve(lambda e: e.tensor_tensor(out=o_, in0=t1, in1=t2, op=ALU.add), r=keys, w=[("RT", ("ob", oi))])
                else:
                    wv, wkeys = mul(tc)
                    sc.dve(lambda e: e.tensor_tensor(out=t1, in0=t1, in1=t2, op=ALU.add), r=keys, w=[keys[0]])
                    sc.pool(lambda e: e.tensor_tensor(out=o_, in0=t1, in1=wv, op=ALU.mult),
                            r=[keys[0]] + wkeys, w=[("RT", ("ob", oi))])
                sc.dma(lambda e: e.dma_start(out=dram_rows[:, tc * 512:(tc + 1) * 512], in_=o_),
                       r=[("RT", ("ob", oi))], w=[(tag, tc)], q="scalar")
            return sink

        for g in range(4):
            wb, wk = self.load_piece(bw[BP_K2 + g])
            self.proj_rope(wb, wk, sink_sb(KT2, g, ("RX", ("KT2", g))))
        wb, wk = self.load_piece(bw[BP_KI2])
        KiT3 = RZ[:, 0:4096].rearrange("p (g t) -> p g t", g=1)
        self.proj_rope(wb, wk, sink_sb(KiT3, 0, ("RZ", "KiT2")))
        for p in range(2):
            wb, wk = self.load_piece(bw[BP_V + p])
            for J0 in range(0, 32, 4):
                pv = 2 + self.ring("psB", 2)
                for jj in range(4):
                    J = J0 + jj
                    for kc in range(8):
                        sc.pe(lambda e, kc=kc, jj=jj, J=J, pv=pv, wb=wb: e.matmul(
                            self.ps[pv][:, jj * 128:(jj + 1) * 128], lhsT=UT3[:, kc, J * 128:(J + 1) * 128],
                            rhs=wb[:, kc, :], start=(kc == 0), stop=(kc == 7)),
                            r=[wk] + self.ut_keys([J], kc), w=[pk(pv)])
                sc.act(lambda e, pv=pv, J0=J0, p=p: e.activation(
                    out=Vp[:, J0:J0 + 4, 2 * p:2 * p + 2, 0:64],
                    in_=self.ps[pv][:, :].rearrange("p (j e c) -> p j e c", j=4, e=2), func=AF.Copy),
                    r=[pk(pv)], w=[("RY", ("V", J0, p))])
        wb, wk = self.load_piece(bw[BP_WI])
        pw = 2 + self.ring("psB", 2)
        for J in range(32):
            for kc in range(8):
                sc.pe(lambda e, kc=kc, J=J, wb=wb: e.matmul(
                    self.ps[pw][:, J * 8:(J + 1) * 8], lhsT=UT3[:, kc, J * 128:(J + 1) * 128], rhs=wb[:, kc, 0:8],
                    start=(kc == 0), stop=(kc == 7)), r=[wk] + self.ut_keys([J], kc), w=[pk(pw)])
        sc.act(lambda e: e.activation(out=wtok, in_=self.ps[pw][:, 0:256], func=AF.Copy), r=[pk(pw)], w=[("wtok", "w")])
        sc.dve(lambda e: e.tensor_scalar(out=lo_t, in0=wtok, scalar1=0.0, scalar2=2.0, op0=ALU.is_ge, op1=ALU.mult),
               r=[("wtok", "w")], w=[("wtok", "lo")])
        sc.dve(lambda e: e.tensor_scalar(out=lo_t, in0=lo_t, scalar1=-1.0, scalar2=None, op0=ALU.add),
               r=[("wtok", "lo")], w=[("wtok", "lo")])
        for p in range(4):
            wbr, wkr = self.load_piece(bw[BP_WREP + p])
            wbq, wkq = self.load_piece(bw[BP_QI + p])

            def pre_chunk(tc, wbr=wbr, wkr=wkr):
                pa = self.ring("psA", 2)
                wi_ = tc % 2
                for kc in range(8):
                    sc.pe(lambda e, kc=kc, pa=pa: e.matmul(
                        self.ps[pa][:, :], lhsT=wbr[:, kc, :], rhs=UT3[:, kc, tc * 512:(tc + 1) * 512],
                        start=(kc == 0), stop=(kc == 7)),
                        r=[wkr] + self.ut_keys(range(tc * 4, tc * 4 + 4), kc), w=[pk(pa)])
                sc.act(lambda e, pa=pa, wi_=wi_: e.activation(out=wr[wi_], in_=self.ps[pa][:, :], func=AF.Abs,
                                                             scale=IDX_SCALE),
                       r=[pk(pa)], w=[("RT", ("wr", wi_))])

            def mul(tc):
                return wr[tc % 2], [("RT", ("wr", tc % 2))]

            self.proj_rope(wbq, wkq, sink_dram(self.QiTd[p * 128:(p + 1) * 128, :], ("QiT", p), mul=mul),
                           pre_chunk=pre_chunk)
        for p in range(8):
            wb, wk = self.load_piece(bw[BP_Q + p])
            self.proj_rope(wb, wk, sink_dram(self.QTd[p * 128:(p + 1) * 128, :], ("QT", p)))
        for p in range(8):
            wb, wk = self.load_piece(bw[BP_GATE + p])
            for e_ in range(2):
                h = 2 * p + e_
                for tc in range(8):
                    pg = 2 + self.ring("psB", 2)
                    for kc in range(8):
                        sc.pe(lambda e, kc=kc, pg=pg, wb=wb, e_=e_, tc=tc: e.matmul(
                            self.ps[pg][0:64, :], lhsT=wb[:, kc, 64 * e_:64 * e_ + 64],
                            rhs=UT3[:, kc, tc * 512:(tc + 1) * 512], start=(kc == 0), stop=(kc == 7)),
                            r=[wk] + self.ut_keys(range(tc * 4, tc * 4 + 4), kc), w=[pk(pg)])
                    sc.act(lambda e, pg=pg: e.activation(out=tq[0:64, :], in_=self.ps[pg][0:64, :], func=AF.Tanh, scale=0.5),
                           r=[pk(pg)], w=[("RT", "tq")])
                    oi = self.ring("ob", 2)
                    o_ = ob[oi]
                    sc.dve(lambda e, pg=pg, o_=o_: e.scalar_tensor_tensor(
                        out=o_[0:64, :], in0=tq[0:64, :], scalar=1.0, in1=self.ps[pg][0:64, :], op0=ALU.add, op1=ALU.mult),
                        r=[pk(pg), ("RT", "tq")], w=[("RT", ("ob", oi))])
                    sc.dma(lambda e, o_=o_, h=h, tc=tc: e.dma_start(
                        out=self.GTd[h * 64:(h + 1) * 64, tc * 512:(tc + 1) * 512], in_=o_[0:64, :]),
                        r=[("RT", ("ob", oi))], w=[(("GT", h), tc)], q="scalar")

        self.fence(["UT", "RW", "RT"])
        accB = f32v(UT[:, 0:16384]).rearrange("p (h q) -> p h q", h=16)
        score = f32v(UT[:, 16384:24576])
        maskb = UT[:, 24576:28672]
        maskTs = [UT[:, 28672:32768].rearrange("p (j q) -> p j q", j=32),
                  RT[:, 3072:7168].rearrange("p (j q) -> p j q", j=32)]
        mTkey = [("UT", ("maskT", 0)), ("RT", ("maskT", 1))]
        QTc = RW[:, 0:4096].rearrange("p (a q) -> p a q", a=8)
        QiTcs = [RW[:, 4096:6144].rearrange("p (a q) -> p a q", a=4),
                 RW[:, 9216:11264].rearrange("p (a q) -> p a q", a=4)]
        tmpS = [f32v(RW[:, 6144:7168]), f32v(RW[:, 7168:8192])]
        GTc = [RW[:, 8192:8704], RW[:, 8704:9216]]
        PTs = [RT[:, i * 1024:(i + 1) * 1024] for i in range(3)]
        ident = cb[:, CB_ID:CB_ID + 128]
        caus = cf[:, CF_CAUS:CF_CAUS + 128]
        mid = sm[:, 400:401]
        cnt = sm[:, 401:402]
        ind = sm[:, 402:403]
        thr = sm[:, 403:404]
        sgn = sm[:, 404:405]
        tot = sm[:, 405:406]

        def prep_chunks(qt):
            ch = []
            sc4, q0 = qt // 4, (qt % 4) * 128
            csl = slice(sc4 * 512, (sc4 + 1) * 512)
            qi_i = sc4 % 2
            QiTc = QiTcs[qi_i]
            qikey = ("RW", ("QiTc", qi_i))
            maskT = maskTs[qt % 2]
            mk = mTkey[qt % 2]
            nk = qt + 1
            ncols = nk * 128
            if qt % 4 == 0:
                def c_load():
                    sc.dma(lambda e: e.dma_start(out=QiTc, in_=self.QiTd.rearrange("(a p) t -> p a t", p=128)[:, :, csl]),
                           r=[(("QiT", p), sc4) for p in range(4)], w=[qikey])
                ch.append(c_load)
            for c0 in range(0, ncols, 512):
                cw = min(512, ncols - c0)
                for hh in range(8):
                    def c_score(c0=c0, cw=cw, hh=hh):
                        par = hh % 2
                        sc.pe(lambda e: e.matmul(
                            self.ps[2 + par][:, 0:cw], lhsT=QiTc[64 * par:64 * par + 64, hh // 2, q0:q0 + 128],
                            rhs=KiT2[64 * par:64 * par + 64, c0:c0 + cw], start=True, stop=True),
                            r=[qikey, ("RZ", "KiT2")], w=[pk(2 + par)])
                        col = qt * 8 + hh
                        ti = self.ring("tmpS", 2)
                        t_ = tmpS[ti]
                        sc.act(lambda e: e.activation(out=t_[:, 0:cw], in_=self.ps[2 + par][:, 0:cw], func=AF.Relu),
                               r=[pk(2 + par)], w=[("RW", ("tmpS", ti))])
                        if hh == 0:
                            sc.dve(lambda e: e.tensor_scalar(
                                out=score[:, c0:c0 + cw], in0=t_[:, 0:cw], scalar1=lo_t[:, col:col + 1], scalar2=None,
                                op0=ALU.mult),
                                r=[("RW", ("tmpS", ti)), ("wtok", "lo")], w=[("UT", "score")])
                        else:
                            sc.dve(lambda e: e.scalar_tensor_tensor(
                                out=score[:, c0:c0 + cw], in0=t_[:, 0:cw], scalar=lo_t[:, col:col + 1],
                                in1=score[:, c0:c0 + cw], op0=ALU.mult, op1=ALU.add),
                                r=[("RW", ("tmpS", ti)), ("wtok", "lo"), ("UT", "score")], w=[("UT", "score")])
                    c_score.cost = 0.75
                    ch.append(c_score)

            def c_caus():
                sc.dve(lambda e: e.tensor_tensor(out=score[:, qt * 128:(qt + 1) * 128],
                                                 in0=score[:, qt * 128:(qt + 1) * 128], in1=caus, op=ALU.add),
                       r=[("UT", "score"), ("cf", 0)], w=[("UT", "score")])
                if qt >= 2:
                    sc.dve(lambda e: e.memset(mid, BIS_LO + BIS_STEP0), w=[("sm", "mid")])
            ch.append(c_caus)
            if qt >= 2:
                nD = (nk // 2) * 128 if nk >= 8 else ncols
                nA = ncols - nD
                for it in range(NIT):
                    def c_bis(it=it):
                        step = BIS_STEP0 / (2.0 ** it)
                        sc.dve(lambda e: e.tensor_scalar(
                            out=maskb[:, 0:nD], in0=score[:, 0:nD], scalar1=mid, scalar2=None, op0=ALU.is_gt,
                            op1=ALU.add, accum_out=cnt),
                            r=[("UT", "score"), ("sm", "mid")], w=[("UT", "maskb"), ("sm", "cnt")])
                        if nA > 0:
                            sc.act(lambda e: e.activation(
                                out=maskb[:, nD:ncols], in_=score[:, nD:ncols], func=AF.Sign, bias=mid, scale=-1.0,
                                accum_out=sgn),
                                r=[("UT", "score"), ("sm", "mid")], w=[("UT", "maskbA"), ("sm", "sgn")])
                            sc.dve(lambda e: e.scalar_tensor_tensor(out=tot, in0=sgn, scalar=-0.5, in1=cnt, op0=ALU.mult,
                                                                    op1=ALU.add),
                                   r=[("sm", "sgn"), ("sm", "cnt")], w=[("sm", "tot")])
                            thr_c = TOPK - 0.5 - nA / 2.0
                            sc.dve(lambda e: e.tensor_scalar(out=ind, in0=tot, scalar1=thr_c, scalar2=step,
                                                             op0=ALU.is_gt, op1=ALU.mult),
                                   r=[("sm", "tot")], w=[("sm", "ind")])
                        else:
                            sc.dve(lambda e: e.tensor_scalar(out=ind, in0=cnt, scalar1=TOPK - 0.5, scalar2=step,
                                                             op0=ALU.is_gt, op1=ALU.mult),
                                   r=[("sm", "cnt")], w=[("sm", "ind")])
                        sc.dve(lambda e: e.scalar_tensor_tensor(out=mid, in0=ind, scalar=-step / 2.0, in1=mid,
                                                                op0=ALU.add, op1=ALU.add),
                               r=[("sm", "ind"), ("sm", "mid")], w=[("sm", "mid")])
                    c_bis.cost = 1.1 + (nD / 960.0)
                    ch.append(c_bis)

            def c_mask():
                if qt >= 2:
                    fstep = BIS_STEP0 / (2.0 ** NIT)
                    sc.dve(lambda e: e.tensor_scalar(out=thr, in0=mid, scalar1=-fstep, scalar2=None, op0=ALU.add),
                           r=[("sm", "mid")], w=[("sm", "thr")])
                    sc.dve(lambda e: e.tensor_scalar(out=maskb[:, 0:ncols], in0=score[:, 0:ncols], scalar1=thr,
                                                     scalar2=None, op0=ALU.is_gt),
                           r=[("UT", "score"), ("sm", "thr")], w=[("UT", "maskb"), ("UT", "maskbA")])
                else:
                    sc.dve(lambda e: e.tensor_scalar(out=maskb[:, 0:ncols], in0=score[:, 0:ncols],
                                                     scalar1=-1.0e29, scalar2=None, op0=ALU.is_gt),
                           r=[("UT", "score")], w=[("UT", "maskb"), ("UT", "maskbA")])
            c_mask.cost = 0.5 + ncols / 960.0
            ch.append(c_mask)
            for j0 in range(0, nk, 8):
                def c_tr(j0=j0):
                    m = min(8, nk - j0)
                    pt_ = 2 + self.ring("psB", 2)
                    ptv = self.ps[pt_][:, :].bitcast(BF16).rearrange("p (j q) -> p j q", j=8)
                    for jj in range(m):
                        sc.pe(lambda e, jj=jj: e.transpose(
                            ptv[:, jj, :], maskb[:, (j0 + jj) * 128:(j0 + jj + 1) * 128], ident),
                            r=[("UT", "maskb"), ("UT", "maskbA"), ("cb", 0)], w=[pk(pt_)])
                    sc.act(lambda e: e.activation(out=maskT[:, j0:j0 + m, :], in_=ptv[:, 0:m, :], func=AF.Copy),
                           r=[pk(pt_)], w=[mk])
                c_tr.cost = 1.5
                ch.append(c_tr)
            return ch

        def attn_chunks(qt):
            ch = []
            sc4, q0 = qt // 4, (qt % 4) * 128
            csl = slice(sc4 * 512, (sc4 + 1) * 512)
            maskT = maskTs[qt % 2]
            mk = mTkey[qt % 2]
            nk = qt + 1
            steps = [(gk, j0, min(2, nk - j0)) for gk in range(4) for j0 in range(0, nk, 2)]
            state = {}

            def sbankb(i, par):
                return (4 + par) if i % 2 == 0 else par

            def emit_Sb(i):
                gk, j0, m = steps[i]
                for t_ in range(m):
                    j = j0 + t_
                    for r_ in range(4):
                        par, r2 = r_ % 2, r_ // 2
                        bS = sbankb(i, par)
                        c_ = (t_ * 2 + r2) * 128
                        sc.pe(lambda e, par=par, r2=r2, gk=gk, j=j, bS=bS, c_=c_: e.matmul(
                            self.ps[bS][:, c_:c_ + 128],
                            lhsT=KT2[64 * par:64 * par + 64, gk, j * 128:(j + 1) * 128],
                            rhs=QTc[64 * par:64 * par + 64, 2 * gk + r2, q0:q0 + 128], start=True, stop=True),
                            r=[("RX", ("KT2", gk)), ("RW", "QTc")], w=[pk(bS)])

            def c_first():
                if qt % 4 == 0:
                    sc.dma(lambda e: e.dma_start(out=QTc, in_=self.QTd.rearrange("(a p) t -> p a t", p=128)[:, :, csl]),
                           r=[(("QT", p), sc4) for p in range(8)], w=[("RW", "QTc")])
                for gk in range(4):
                    state[gk] = 6 + self.ring("psO", 2)
                emit_Sb(0)
            ch.append(c_first)
            for i, (gk, j0, m) in enumerate(steps):
                def c_step(i=i, gk=gk, j0=j0, m=m):
                    po = state[gk]
                    if i + 1 < len(steps):
                        emit_Sb(i + 1)
                    pti = self.ring("PT", 3)
                    PT4 = PTs[pti].rearrange("p (t h q) -> p t h q", t=2, h=4)
                    for par in range(2):
                        bS = sbankb(i, par)
                        sc.act(lambda e, par=par, bS=bS: e.activation(
                            out=PT4[:, 0:m, 2 * par:2 * par + 2, :],
                            in_=self.ps[bS][:, 0:m * 256].rearrange("p (t b q) -> p t b q", t=m, b=2),
                            func=AF.Exp, scale=0.125), r=[pk(bS)], w=[("RT", ("PT", pti))])
                    sc.dve(lambda e: e.tensor_tensor(
                        out=PT4[:, 0:m, :, :], in0=PT4[:, 0:m, :, :],
                        in1=maskT[:, j0:j0 + m, :].unsqueeze(2).to_broadcast([128, m, 4, 128]), op=ALU.mult),
                        r=[("RT", ("PT", pti)), mk], w=[("RT", ("PT", pti))])
                    for t_ in range(m):
                        j = j0 + t_
                        for r_ in range(4):
                            par, r2 = r_ % 2, r_ // 2
                            sc.pe(lambda e, par=par, r2=r2, r_=r_, j=j, t_=t_: e.matmul(
                                self.ps[po][0:65, r_ * 128:(r_ + 1) * 128], lhsT=Vp[:, j, gk, :],
                                rhs=PT4[:, t_, par * 2 + r2, :],
                                start=(j == 0 and r_ == 0), stop=(j == nk - 1), skip_group_check=True),
                                r=[("RY", ("V", (j // 4) * 4, gk // 2)), ("RT", ("PT", pti))], w=[pk(po)])
                    if j0 + m == nk:
                        sc.act(lambda e: e.activation(
                            out=accB[0:65, 4 * gk:4 * gk + 4, q0:q0 + 128],
                            in_=self.ps[po][0:65, :].rearrange("p (r q) -> p r q", r=4), func=AF.Copy),
                            r=[pk(po)], w=[("UT", ("accB", gk))])
                c_step.cost = 1.0 + 0.55 * m
                ch.append(c_step)
            if qt % 4 == 3:
                def c_fin():
                    units = []
                    for h in range(16):
                        gi = h % 2
                        g_ = GTc[gi]

                        def loader(g_=g_, h=h, gi=gi):
                            sc.dma(lambda e: e.dma_start(out=g_[0:64, :], in_=self.GTd[h * 64:(h + 1) * 64, csl]),
                                   r=[(("GT", h), sc4)], w=[("RW", ("GTc", gi))])

                        units.append(dict(h=h, tc=sc4, acc=accB[:, h, :], akeys=[("UT", ("accB", h // 4))],
                                          gate=("dram", g_[0:64, :], [("RW", ("GTc", gi))], loader)))
                    self.finalize_units(units)
                c_fin.cost = 60.0
                ch.append(c_fin)
            return ch

        def cost(fn):
            return getattr(fn, "cost", 1.0)

        def run_merged(A, P):
            ta = sum(cost(a) for a in A) or 1.0
            tp_ = sum(cost(p) for p in P)
            scale = max(1.0, tp_ / ta)
            pi = 0
            acc_a = 0.0
            acc_p = 0.0
            for a in A:
                a()
                acc_a += cost(a)
                while pi < len(P) and acc_p + cost(P[pi]) * 0.5 <= acc_a * scale:
                    P[pi]()
                    acc_p += cost(P[pi])
                    pi += 1
            while pi < len(P):
                P[pi]()
                pi += 1

        for c_ in prep_chunks(0):
            c_()
        for qt in range(32):
            A = attn_chunks(qt)
            P = prep_chunks(qt + 1) if qt + 1 < 32 else []
            run_merged(A, P)


_SHARED = {}


def prep_shared(inputs):
    cbv, cfv = make_consts()
    norm_g = np.asarray(inputs["norm_g"], np.float32)
    ada_b = np.asarray(inputs["ada_b"], np.float32)
    sh = dict(
        ngT=np.ascontiguousarray(norm_g.reshape(4, 8, 128).transpose(2, 0, 1).reshape(128, 32)),
        fg=np.ascontiguousarray(np.asarray(inputs["final_g"], np.float32).reshape(1, D)),
        ada_w=np.ascontiguousarray(np.asarray(inputs["ada_w"], np.float32)),
        adabT=np.ascontiguousarray(ada_b.reshape(4, 24, 128).transpose(2, 0, 1).reshape(128, 96)),
        adab=np.ascontiguousarray(ada_b),
        awout=np.ascontiguousarray(np.asarray(inputs["a_w_out"], np.float32)),
        bwout=np.ascontiguousarray(np.asarray(inputs["b_w_out"], np.float32)),
        cb=cbv, cf=cfv,
    )
    acols = a_piece_cols()
    a_w_in = np.asarray(inputs["a_w_in"], np.float32)
    sh["awin"] = np.stack([pieces_layout(a_w_in[i], acols) for i in range(2)])
    bcols = b_piece_cols()
    b_w_in = np.asarray(inputs["b_w_in"], np.float32)
    sh["bwin"] = np.stack([pieces_layout(b_w_in[i], bcols) for i in range(2)])
    return sh


def core_inputs(inputs, b, shared):
    m = dict(shared)
    m["x"] = np.ascontiguousarray(np.asarray(inputs["x"][b], np.float32))
    m["cT"] = np.ascontiguousarray(np.asarray(inputs["c"][b], np.float32).reshape(8, 128).T)
    m["pos"] = np.ascontiguousarray(np.asarray(inputs["positions"][b], np.int32).reshape(1, S))
    return m


_NC_CACHE = {}


def kernel(x, c, positions, norm_g, ada_w, ada_b, a_w_in, a_w_out, b_w_in, b_w_out, final_g):
    inputs = dict(x=x, c=c, positions=positions, norm_g=norm_g, ada_w=ada_w, ada_b=ada_b, a_w_in=a_w_in,
                  a_w_out=a_w_out, b_w_in=b_w_in, b_w_out=b_w_out, final_g=final_g)
    shared = prep_shared(inputs)
    nc = Prog().build()
    in_maps = [core_inputs(inputs, b, shared) for b in range(8)]
    res = run_bass_kernel_spmd(nc, in_maps, core_ids=list(range(8)))
    return np.stack([np.asarray(r["out"], np.float32) for r in res.results], axis=0)
```
